# Optimizing a Trainium2 kernel written in Bass

```python
import math
import jax
import jax.numpy as jnp
from jax import lax
import numpy as np

D_MODEL = 1024
BATCH = 8
SEQ = 4096
DEPTH = 1

CHUNK = 64
Q_BLOCK = 128
EPS = 1e-6

DA_HEADS = 8
DA_HEAD_DIM = 64
DA_V_DIM = 2 * DA_HEAD_DIM
DA_QK_WIDTH = DA_HEADS * 2 * DA_HEAD_DIM
DA_WIDTH = DA_HEADS * DA_V_DIM

SSD_HEAD_DIM = 64
SSD_WIDTH = D_MODEL
SSD_HEADS = SSD_WIDTH // SSD_HEAD_DIM
SSD_GROUPS = 4
SSD_STATE = 128
SSD_CONV = 4
SSD_XBC = SSD_WIDTH + 2 * SSD_GROUPS * SSD_STATE

N_BRANCH = 2

PEER_HEADS = 8
PEER_NKEYS = 128
PEER_EXPERTS = PEER_NKEYS * PEER_NKEYS
PEER_KEY_DIM = 256
PEER_HALF = PEER_KEY_DIM // 2
PEER_TOPK = 16
PEER_TOKEN_BLOCK = 128

COLS = (DA_QK_WIDTH, DA_QK_WIDTH, DA_WIDTH, SSD_WIDTH, SSD_XBC, SSD_HEADS, N_BRANCH * D_MODEL)
IN_WIDTH = sum(COLS)
SPLITS = tuple(sum(COLS[:i + 1]) for i in range(len(COLS) - 1))

kernel_name = "hybrid_diffattn_ssd_peer_block"


def lambda_init(layer_index):
    return 0.8 - 0.6 * math.exp(-0.3 * (layer_index - 1))


def rms_norm(x, g):
    xf = x.astype(jnp.float32)
    y = xf * lax.rsqrt(jnp.mean(xf * xf, axis=-1, keepdims=True) + EPS)
    return (y * g.astype(jnp.float32)).astype(x.dtype)


def alibi_slopes(n_heads):
    return jnp.exp2(-8.0 * jnp.arange(1, n_heads + 1, dtype=jnp.float32) / n_heads)


def diff_attention(q, k, v, lam):
    b, s, nh, _, d = q.shape
    nq = s // Q_BLOCK
    q_blocks = q.reshape(b, nq, Q_BLOCK, nh, 2, d).transpose(1, 0, 3, 4, 2, 5)
    k_t = k.transpose(0, 2, 3, 1, 4)
    v_t = v.transpose(0, 2, 1, 3)
    slopes = alibi_slopes(nh)
    k_pos = jnp.arange(s)
    scale = d ** -0.5

    def attend_block(args):
        q_blk, blk = args
        q_pos = blk * Q_BLOCK + jnp.arange(Q_BLOCK)
        scores = jnp.einsum('bhmqd,bhmkd->bhmqk', q_blk, k_t).astype(jnp.float32) * scale
        dist = jnp.abs(q_pos[:, None] - k_pos[None, :]).astype(jnp.float32)
        visible = (k_pos[None, :] // CHUNK) <= (q_pos[:, None] // CHUNK)
        bias = jnp.where(visible[None], -slopes[:, None, None] * dist[None], -jnp.inf)
        probs = jax.nn.softmax(scores + bias[None, :, None], axis=-1)
        weights = probs[:, :, 0] - lam * probs[:, :, 1]
        return jnp.einsum('bhqk,bhkv->bhqv', weights.astype(v.dtype), v_t)

    out = lax.map(attend_block, (q_blocks, jnp.arange(nq)))
    return out.transpose(1, 0, 3, 2, 4).reshape(b, s, nh, 2 * d)


def causal_dwconv(x, w, bias):
    y = lax.conv_general_dilated(
        x, w.astype(x.dtype), window_strides=(1,), padding=[(SSD_CONV - 1, 0)],
        dimension_numbers=('NWC', 'WIO', 'NWC'), feature_group_count=x.shape[-1])
    return y + bias.astype(x.dtype)


def ssd_chunked_scan(xdt, a, bm, cm):
    b, s, nh, p = xdt.shape
    g, n = bm.shape[2], bm.shape[3]
    r = nh // g
    nc = s // CHUNK
    X = xdt.reshape(b, nc, CHUNK, g, r, p)
    A = a.reshape(b, nc, CHUNK, g, r).transpose(0, 3, 4, 1, 2)
    Bc = bm.reshape(b, nc, CHUNK, g, n)
    Cc = cm.reshape(b, nc, CHUNK, g, n)
    a_cum = jnp.cumsum(A, axis=-1)
    tril = jnp.tril(jnp.ones((CHUNK, CHUNK), dtype=bool))
    seg = a_cum[..., :, None] - a_cum[..., None, :]
    L = jnp.exp(jnp.where(tril, seg, -jnp.inf))
    cb = jnp.einsum('bclgn,bcsgn->bcgls', Cc, Bc)
    y_diag = jnp.einsum('bcgls,bgrcls,bcsgrp->bclgrp', cb, L, X)
    decay_states = jnp.exp(a_cum[..., -1:] - a_cum)
    states = jnp.einsum('bcsgn,bgrcs,bcsgrp->cbgrpn', Bc, decay_states, X)
    chunk_decay = jnp.exp(a_cum[..., -1]).transpose(3, 0, 1, 2)

    def step(h, inp):
        st, dec = inp
        return h * dec[..., None, None] + st, h

    _, prev = lax.scan(step, jnp.zeros(states.shape[1:], states.dtype), (states, chunk_decay))
    y_off = jnp.einsum('bclgn,cbgrpn,bgrcl->bclgrp', Cc, prev, jnp.exp(a_cum))
    return (y_diag + y_off).reshape(b, s, nh, p)


def ssd_mixer(z, xbc, dt_raw, conv_w, conv_b, dt_bias, a_log, d_skip, g_ssd):
    b, s, _ = z.shape
    xbc = jax.nn.silu(causal_dwconv(xbc, conv_w, conv_b))
    xs, bm, cm = jnp.split(xbc, [SSD_WIDTH, SSD_WIDTH + SSD_GROUPS * SSD_STATE], axis=-1)
    xs = xs.reshape(b, s, SSD_HEADS, SSD_HEAD_DIM).astype(jnp.float32)
    bm = bm.reshape(b, s, SSD_GROUPS, SSD_STATE).astype(jnp.float32)
    cm = cm.reshape(b, s, SSD_GROUPS, SSD_STATE).astype(jnp.float32)
    dt = jax.nn.softplus(dt_raw.astype(jnp.float32) + dt_bias.astype(jnp.float32))
    a = -jnp.exp(a_log.astype(jnp.float32)) * dt
    y = ssd_chunked_scan(xs * dt[..., None], a, bm, cm) + d_skip.astype(jnp.float32)[:, None] * xs
    y = y.reshape(b, s, SSD_WIDTH).astype(z.dtype)
    return rms_norm(y * jax.nn.silu(z), g_ssd)


def peer(h, w_query, sub_keys, expert_u, expert_v):
    b, s, d = h.shape
    qry = (h @ w_query).reshape(b, s, PEER_HEADS, 2, PEER_HALF)
    sc = jnp.einsum('btnhd,nhkd->btnhk', qry, sub_keys)
    v1, i1 = lax.top_k(sc[..., 0, :], PEER_TOPK)
    v2, i2 = lax.top_k(sc[..., 1, :], PEER_TOPK)
    cand = (v1[..., :, None] + v2[..., None, :]).reshape(b, s, PEER_HEADS, PEER_TOPK * PEER_TOPK)
    cid = (i1[..., :, None] * PEER_NKEYS + i2[..., None, :]).reshape(b, s, PEER_HEADS, PEER_TOPK * PEER_TOPK)
    top, pos = lax.top_k(cand, PEER_TOPK)
    eid = jnp.take_along_axis(cid, pos, axis=-1)
    gate = jax.nn.softmax(top.astype(jnp.float32), axis=-1).astype(h.dtype)
    nblk = (b * s) // PEER_TOKEN_BLOCK
    hb = h.reshape(nblk, PEER_TOKEN_BLOCK, d)
    eb = eid.reshape(nblk, PEER_TOKEN_BLOCK, PEER_HEADS, PEER_TOPK)
    gb = gate.reshape(nblk, PEER_TOKEN_BLOCK, PEER_HEADS, PEER_TOPK)

    def expert_block(args):
        ht, et, gt = args
        u = expert_u[et]
        act = jax.nn.gelu(jnp.einsum('tnkd,td->tnk', u, ht), approximate=False) * gt
        return jnp.einsum('tnk,tnkd->td', act, expert_v[et])

    out = lax.map(expert_block, (hb, eb, gb))
    return out.reshape(b, s, d)


def setup_inputs(seed: int = 0) -> dict:
    key = jax.random.key(seed)
    ks = jax.random.split(key, 24)
    L = DEPTH

    def nrm(k, shape, scale):
        return jax.random.normal(k, shape, jnp.float32) * scale

    dt0 = jnp.exp(jax.random.uniform(ks[10], (L, SSD_HEADS), jnp.float32,
                                     minval=math.log(1e-3), maxval=math.log(1e-1)))
    return {
        "x": nrm(ks[0], (BATCH, SEQ, D_MODEL), 1.0),
        "g_mix": 1.0 + nrm(ks[1], (L, D_MODEL), 0.01),
        "w_in": nrm(ks[2], (L, D_MODEL, IN_WIDTH), D_MODEL ** -0.5),
        "lam_q1": nrm(ks[3], (L, DA_HEAD_DIM), 0.1),
        "lam_k1": nrm(ks[4], (L, DA_HEAD_DIM), 0.1),
        "lam_q2": nrm(ks[5], (L, DA_HEAD_DIM), 0.1),
        "lam_k2": nrm(ks[6], (L, DA_HEAD_DIM), 0.1),
        "g_subln": 1.0 + nrm(ks[7], (L, DA_V_DIM), 0.01),
        "conv_w": nrm(ks[8], (L, SSD_CONV, 1, SSD_XBC), SSD_CONV ** -0.5),
        "conv_b": nrm(ks[9], (L, SSD_XBC), 0.01),
        "dt_bias": dt0 + jnp.log(-jnp.expm1(-dt0)),
        "a_log": jnp.log(jax.random.uniform(ks[11], (L, SSD_HEADS), jnp.float32, minval=1.0, maxval=16.0)),
        "d_skip": 1.0 + nrm(ks[12], (L, SSD_HEADS), 0.1),
        "g_ssd": 1.0 + nrm(ks[13], (L, SSD_WIDTH), 0.01),
        "w_branch_a": nrm(ks[14], (L, DA_WIDTH, D_MODEL), DA_WIDTH ** -0.5),
        "w_branch_b": nrm(ks[15], (L, SSD_WIDTH, D_MODEL), SSD_WIDTH ** -0.5),
        "w_out": nrm(ks[16], (L, D_MODEL, D_MODEL), D_MODEL ** -0.5),
        "g_ffn": 1.0 + nrm(ks[17], (L, D_MODEL), 0.01),
        "w_query": nrm(ks[18], (L, D_MODEL, PEER_HEADS * PEER_KEY_DIM), D_MODEL ** -0.5),
        "sub_keys": nrm(ks[19], (L, PEER_HEADS, 2, PEER_NKEYS, PEER_HALF), PEER_HALF ** -0.5),
        "expert_u": nrm(ks[20], (L, PEER_EXPERTS, D_MODEL), D_MODEL ** -0.5),
        "expert_v": nrm(ks[21], (L, PEER_EXPERTS, D_MODEL), PEER_HEADS ** -0.5),
        "g_final": 1.0 + nrm(ks[22], (D_MODEL,), 0.01),
    }


def reference(x, g_mix, w_in, lam_q1, lam_k1, lam_q2, lam_k2, g_subln, conv_w, conv_b,
              dt_bias, a_log, d_skip, g_ssd, w_branch_a, w_branch_b, w_out, g_ffn,
              w_query, sub_keys, expert_u, expert_v, g_final):
    b, s, _ = x.shape
    for layer in range(DEPTH):
        lam0 = lambda_init(layer + 1)
        h = rms_norm(x, g_mix[layer])
        proj = h @ w_in[layer]
        q, k, v, z, xbc, dt_raw, gate_logits = jnp.split(proj, list(SPLITS), axis=-1)

        lam = (jnp.exp(jnp.sum(lam_q1[layer] * lam_k1[layer]).astype(jnp.float32))
               - jnp.exp(jnp.sum(lam_q2[layer] * lam_k2[layer]).astype(jnp.float32)) + lam0)
        att = diff_attention(q.reshape(b, s, DA_HEADS, 2, DA_HEAD_DIM),
                             k.reshape(b, s, DA_HEADS, 2, DA_HEAD_DIM),
                             v.reshape(b, s, DA_HEADS, DA_V_DIM), lam)
        att = rms_norm(att, g_subln[layer]) * (1.0 - lam0)
        y_a = att.reshape(b, s, DA_WIDTH) @ w_branch_a[layer]

        y_b = ssd_mixer(z, xbc, dt_raw, conv_w[layer], conv_b[layer], dt_bias[layer],
                        a_log[layer], d_skip[layer], g_ssd[layer]) @ w_branch_b[layer]

        g_a, g_b = jnp.split(jax.nn.sigmoid(gate_logits), N_BRANCH, axis=-1)
        x = x + (g_a * y_a + g_b * y_b) @ w_out[layer]

        x = x + peer(rms_norm(x, g_ffn[layer]), w_query[layer], sub_keys[layer],
                     expert_u[layer], expert_v[layer])
    return rms_norm(x, g_final)
```

```python
from contextlib import ExitStack
import math
import numpy as np
import ml_dtypes
import concourse.bass as bass
import concourse.mybir as mybir
from concourse.bass_utils import run_bass_kernel_spmd

F32 = mybir.dt.float32
BF16 = mybir.dt.bfloat16
U32 = mybir.dt.uint32
ALU = mybir.AluOpType
AF = mybir.ActivationFunctionType
AX = mybir.AxisListType

S = 4096
D = 1024
NT = S // 128
EPS = 1e-6
INW = 8208
COMPUTE = ("pe", "act", "dve", "pool")
NDMA_SEMS = 24


class SemPool:
    def __init__(self, nc, es):
        self.nc, self.es = nc, es
        self.dsem = [es.enter_context(nc.semaphore(f"dma{i}")) for i in range(NDMA_SEMS)]
        self.dval = [0] * NDMA_SEMS
        self.drr = 0
        self.n = 0

    def new(self, tag):
        self.n += 1
        return self.es.enter_context(self.nc.semaphore(f"c{self.n}_{tag}"))


SEM_ROLL = 30000


class Prog:
    def __init__(self, nc, es, tag):
        self.nc = nc
        self.tag = tag
        self.G = POOLS[id(nc)]
        self.q = {e: [] for e in COMPUTE + ("sp",)}
        self.cnt = {e: 0 for e in COMPUTE}
        self.epoch = {e: 0 for e in COMPUTE}
        self.semobj = {}
        for e in COMPUTE:
            self.semobj[("c", e, 0)] = self.G.new(f"{tag}_{e}")
        for i in range(NDMA_SEMS):
            self.semobj[("d", i)] = self.G.dsem[i]
        self.last_w = {}
        self.readers = {}
        self.known = {e: {} for e in COMPUTE + ("sp",)}
        self.skip_dist = None

    def _deps(self, eng, reads, writes, extra=()):
        deps = {}
        for r in reads:
            for k, v in self.last_w.get(r, {}).items():
                if deps.get(k, 0) < v:
                    deps[k] = v
        for w in writes:
            for k, v in self.last_w.get(w, {}).items():
                if deps.get(k, 0) < v:
                    deps[k] = v
            for k, v in self.readers.get(w, {}).items():
                if deps.get(k, 0) < v:
                    deps[k] = v
        for k, v in extra:
            if deps.get(k, 0) < v:
                deps[k] = v
        waits = []
        kn = self.known[eng]
        for k, v in deps.items():
            if eng == "pe" and k[0] == "c" and k[1] == "pe":
                continue
            if (self.skip_dist is not None and k[0] == "c" and k[1] == eng and k[2] == self.epoch[eng]
                    and self.cnt[eng] + 1 - v >= self.skip_dist):
                continue
            if kn.get(k, 0) >= v:
                continue
            kn[k] = v
            waits.append((k, v))
        return waits

    def _commit(self, tok, reads, writes):
        k, v = tok
        for r in reads:
            d = self.readers.setdefault(r, {})
            if d.get(k, 0) < v:
                d[k] = v
        for w in writes:
            self.last_w[w] = {k: v}
            self.readers[w] = {}

    def op(self, eng, fn, reads=(), writes=()):
        waits = self._deps(eng, reads, writes)
        if self.cnt[eng] >= SEM_ROLL:
            self.epoch[eng] += 1
            self.cnt[eng] = 0
            self.semobj[("c", eng, self.epoch[eng])] = self.G.new(f"{self.tag}_{eng}{self.epoch[eng]}")
        self.cnt[eng] += 1
        key = ("c", eng, self.epoch[eng])
        tok = (key, self.cnt[eng])
        self.q[eng].append((waits, fn, (key, 1)))
        self._commit(tok, reads, writes)

    def dma(self, out, in_, reads=(), writes=(), queue="sp", **kw):
        G = self.G
        i = G.drr
        G.drr = (G.drr + 1) % NDMA_SEMS
        extra = [(("d", i), G.dval[i])] if G.dval[i] else []
        waits = self._deps(queue, reads, writes, extra=extra)
        G.dval[i] += 16
        tok = (("d", i), G.dval[i])
        self.q[queue].append((waits, lambda e: e.dma_start(out=out, in_=in_, **kw), (("d", i), 16)))
        self._commit(tok, reads, writes)

    def merge(self, keys, newkey):
        d = {}
        for key in keys:
            for k, v in self.last_w.get(key, {}).items():
                if d.get(k, 0) < v:
                    d[k] = v
        self.last_w[newkey] = d
        self.readers.setdefault(newkey, {})

    def emit(self):
        nc = self.nc
        toks = []
        for e in COMPUTE:
            for ep in range(self.epoch[e] + 1):
                v = self.cnt[e] if ep == self.epoch[e] else SEM_ROLL
                if v:
                    toks.append((("c", e, ep), v))
        toks += [(("d", i), self.G.dval[i]) for i in range(NDMA_SEMS) if self.G.dval[i]]
        for eng in COMPUTE + ("sp",):
            kn = self.known[eng]
            waits = [(k, v) for k, v in toks if kn.get(k, 0) < v]
            if waits:
                self.q[eng].append((waits, None, None))
        engmap = {"pe": "tensor", "act": "scalar", "dve": "vector", "pool": "gpsimd", "sp": "sync"}
        with nc.Block() as block:
            for e, bname in engmap.items():
                lst = self.q[e]

                def body(eng, lst=lst):
                    for waits, fn, inc in lst:
                        for k, v in waits:
                            eng.wait_ge(self.semobj[k], v)
                        if fn is not None:
                            fn(eng).then_inc(self.semobj[inc[0]], inc[1])

                getattr(block, bname)(body)


def MM(P, out, lhsT, rhs, start, stop, reads, writes, **kw):
    P.op("pe", lambda e: e.matmul(out, lhsT=lhsT, rhs=rhs, start=start, stop=stop, **kw), reads, writes)


def TR(P, out, in_, ident, reads, writes):
    P.op("pe", lambda e: e.transpose(out=out, in_=in_, identity=ident), reads, writes)


def ACT(P, out, in_, func, reads, writes, bias=None, scale=None, eng="act"):
    kw = {}
    if bias is not None:
        kw["bias"] = bias
    if scale is not None:
        kw["scale"] = scale
    P.op("act", lambda e: e.activation(out=out, in_=in_, func=func, **kw), reads, writes)


def TT(P, eng, out, in0, in1, op, reads, writes):
    P.op(eng, lambda e: e.tensor_tensor(out=out, in0=in0, in1=in1, op=op), reads, writes)


def TS(P, eng, out, in0, s1, op0, reads, writes, s2=None, op1=None):
    if op1 is None:
        P.op(eng, lambda e: e.tensor_scalar(out=out, in0=in0, scalar1=s1, scalar2=None, op0=op0), reads, writes)
    else:
        P.op(eng, lambda e: e.tensor_scalar(out=out, in0=in0, scalar1=s1, scalar2=s2, op0=op0, op1=op1), reads, writes)


def STT(P, eng, out, in0, scalar, in1, op0, op1, reads, writes):
    P.op(eng, lambda e: e.scalar_tensor_tensor(out=out, in0=in0, scalar=scalar, in1=in1, op0=op0, op1=op1), reads, writes)


def CP(P, eng, out, in_, reads, writes):
    if eng == "act":
        P.op("act", lambda e: e.copy(out=out, in_=in_), reads, writes)
    else:
        P.op(eng, lambda e: e.tensor_copy(out=out, in_=in_), reads, writes)


def MEMSET(P, eng, ap, val, writes):
    P.op(eng, lambda e: e.memset(ap, val), (), writes)


def RSUM(P, eng, out, in_, reads, writes):
    P.op(eng, lambda e: e.reduce_sum(out=out, in_=in_, axis=AX.X), reads, writes)


class Alloc:
    def __init__(self, nc, es):
        self.nc, self.es = nc, es

    def sb(self, name, shape, dt):
        return self.es.enter_context(self.nc.sbuf_tensor(name, shape, dt))

    def ps(self, name, shape, dt):
        return self.es.enter_context(self.nc.psum_tensor(name, shape, dt))


def phase_a(nc, T):
    with ExitStack() as es:
        P = Prog(nc, es, "A")
        A = Alloc(nc, es)
        hT = A.sb("a_hT", [128, 8, S + 3], BF16)
        ident = A.sb("a_ident", [128, 128], BF16)
        gmt = A.sb("a_gmt", [128, 8], F32)
        ones_row = A.sb("a_ones", [1, 128], BF16)
        cbrow_f = A.sb("a_cbrowf", [1, 2048], F32)
        cbrow = A.sb("a_cbrow", [1, 2048], BF16)
        cbcol = A.sb("a_cbcol", [128, 16], F32)
        xt = [A.sb(f"a_xt{i}", [128, D], F32) for i in range(2)]
        sq = A.sb("a_sq", [128, D], F32)
        hb = [A.sb(f"a_hb{i}", [128, D], BF16) for i in range(2)]
        ss = [A.sb(f"a_ss{i}", [128, 4], F32) for i in range(2)]
        wf = [A.sb(f"a_wf{i}", [128, 8, 256], F32) for i in range(2)]
        wb = [A.sb(f"a_wb{i}", [128, 8, 256], BF16) for i in range(2)]
        pre = [A.sb(f"a_pre{i}", [128, S + 3], BF16) for i in range(2)]
        cacc = A.sb("a_cacc", [128, S], F32)
        cwcol = A.sb("a_cwcol", [128, 16, 4], F32)
        st_tmc = [A.sb(f"a_sttmc{i}", [128, NT, 128], BF16) for i in range(2)]
        st_fm = [A.sb(f"a_stfm{i}", [128, S], BF16) for i in range(2)]
        st_tm = [A.sb(f"a_sttm{i}", [128, 4, 256], BF16) for i in range(2)]
        st_dt = A.sb("a_stdt", [128, NT, 16], F32)
        pt = A.ps("a_pt", [128, 8, 128], BF16)
        pm = [A.ps(f"a_pm{i}", [128, 512], F32) for i in range(4)]

        P.dma(ident[:], T["ident"], writes=["ident"])
        P.dma(gmt[:], T["gm"], writes=["gmt"])
        P.dma(cbrow_f[:], T["conv_b_row"], writes=["cbrow_f"])
        P.dma(cbcol[:], T["conv_b_col"], writes=["cbcol"])
        MEMSET(P, "pool", hT[:, :, 0:3], 0.0, ["hT_pad"])
        P.dma(cwcol[:], T["conv_w_col"], writes=["cwcol"])
        for i in range(2):
            MEMSET(P, "pool", pre[i][:, 0:3], 0.0, [("prepad", i)])
        MEMSET(P, "pool", ones_row[:], 1.0, ["ones"])
        CP(P, "dve", cbrow[:], cbrow_f[:], ["cbrow_f"], ["cbrow"])

        for tt in range(NT):
            b = tt % 2
            P.dma(xt[b][:], T["x"][tt * 128:(tt + 1) * 128, :], writes=[("xt", b)])
            TT(P, "dve", sq[:], xt[b][:], xt[b][:], ALU.mult, [("xt", b)], ["sq"])
            RSUM(P, "dve", ss[b][:, 0:1], sq[:], ["sq"], [("ss", b)])
            ACT(P, ss[b][:, 1:2], ss[b][:, 0:1], AF.Ln, [("ss", b)], [("ss", b)], bias=EPS, scale=1.0 / D)
            ACT(P, ss[b][:, 2:3], ss[b][:, 1:2], AF.Exp, [("ss", b)], [("ss", b)], scale=-0.5)
            TS(P, "dve", hb[b][:], xt[b][:], ss[b][:, 2:3], ALU.mult, [("ss", b), ("xt", b)], [("hb", b)])
            for k in range(8):
                TR(P, pt[:, k, :], hb[b][:, k * 128:(k + 1) * 128], ident[:], [("hb", b), "ident"], ["pt"])
            CP(P, "act" if tt % 2 else "dve", hT[:, :, 3 + tt * 128: 3 + (tt + 1) * 128], pt[:], ["pt"], [("hT", tt)])
        P.merge([("hT", tt) for tt in range(NT)] + ["hT_pad"], "hT")

        gm_bc = gmt[:].unsqueeze(2).to_broadcast([128, 8, 256])
        segs = [("q", 0, 1024), ("k", 1024, 2048), ("v", 2048, 3072), ("z", 3072, 4096),
                ("xs", 4096, 5120), ("B", 5120, 5632), ("C", 5632, 6144), ("dt", 6144, 6160),
                ("g", 6160, 8208)]
        blocks = []
        for name, c0, c1 in segs:
            for c in range(c0, c1, 256):
                blocks.append((name, c, min(256, c1 - c)))
        state = {"pm": 0, "ev": 0, "fm": 0, "tm": 0}

        def next_pm():
            i = state["pm"]
            state["pm"] = (i + 1) % 4
            return i

        def load_w(bi):
            name, c0, ncol = blocks[bi]
            b = bi % 2
            P.dma(wf[b][:, :, 0:ncol], T["w_in"][:, :, c0:c0 + ncol], writes=[("wf", b)])

        load_w(0)
        for bi, (name, c0, ncol) in enumerate(blocks):
            b = bi % 2
            if bi + 1 < len(blocks):
                load_w(bi + 1)
            conv = False
            TT(P, "pool", wb[b][:, :, 0:ncol], wf[b][:, :, 0:ncol], gm_bc[:, :, 0:ncol], ALU.mult,
               [("wf", b), "gmt"], [("wb", b)])
            wkeys = [("wb", b)]

            def w_of(tap):
                return wb[b]

            taps = range(1)
            if name in ("xs", "B", "C"):
                for cc in range(ncol // 128):
                    j = (c0 - 4096) // 128 + cc
                    pr = pre[j % 2]
                    for tb in range(8):
                        pi = next_pm()
                        for k in range(8):
                            MM(P, pm[pi][:, :], wb[b][:, k, cc * 128:(cc + 1) * 128],
                               hT[:, k, 3 + tb * 512: 3 + (tb + 1) * 512], k == 0, k == 7, wkeys + ["hT"], [("pm", pi)])
                        CP(P, "dve" if state["ev"] % 2 else "act", pr[:, 3 + tb * 512: 3 + (tb + 1) * 512], pm[pi][:, :],
                           [("pm", pi)], [("pre", j % 2, tb)])
                        state["ev"] += 1
                    P.merge([("pre", j % 2, tb) for tb in range(8)] + [("prepad", j % 2)], ("preall", j % 2))
                    ce = "dve"
                    TS(P, ce, cacc[:], pr[:, 3:3 + S], cwcol[:, j, 3:4], ALU.mult, [("preall", j % 2), "cwcol"], ["cacc"])
                    for tap in (2, 1, 0):
                        STT(P, ce, cacc[:], pr[:, tap:tap + S], cwcol[:, j, tap:tap + 1], cacc[:], ALU.mult, ALU.add,
                            [("preall", j % 2), "cwcol", "cacc"], ["cacc"])
                    for tb in range(8):
                        P.readers.setdefault(("pre", j % 2, tb), {}).update(P.readers.get(("preall", j % 2), {}))
                    sfi = state["fm"] % 2
                    state["fm"] += 1
                    sf = st_fm[sfi]
                    ACT(P, sf[:, :], cacc[:], AF.Silu, ["cacc", "cbcol"], [("stfm", sfi)], bias=cbcol[:, j:j + 1])
                    if name in ("B", "C"):
                        r0 = (c0 - (5120 if name == "B" else 5632)) + cc * 128
                        dram = (T["BT_s"] if name == "B" else T["CT_s"])[r0:r0 + 128, :]
                        P.dma(dram, sf[:, :], reads=[("stfm", sfi)], writes=[("dram_fm", name, r0)])
                    if name in ("xs", "B"):
                        x = state["tm"] % 2
                        state["tm"] += 1
                        for tt in range(NT):
                            TR(P, pt[:, tt % 8, :], sf[:, tt * 128:(tt + 1) * 128], ident[:], [("stfm", sfi), "ident"], ["pt"])
                            if tt % 8 == 7:
                                CP(P, "dve" if (tt // 8) % 2 else "act", st_tmc[x][:, tt - 7:tt + 1, :], pt[:],
                                   ["pt"], [("sttmc", x)])
                        col0 = (c0 - (4096 if name == "xs" else 5120)) + cc * 128
                        dram = (T["xs_s"] if name == "xs" else T["B_s"])[:, col0:col0 + 128]
                        P.dma(dram.rearrange("(t p) c -> p t c", p=128), st_tmc[x][:], reads=[("sttmc", x)],
                              writes=[("dram_tmc", name, col0)])
                continue
            if name in ("q", "k", "g", "B", "C"):
                for cc in range(ncol // 128):
                    sfi = state["fm"] % 2
                    state["fm"] += 1
                    sf = st_fm[sfi]
                    for tb in range(8):
                        pi = next_pm()
                        n_mm = len(taps) * 8
                        i = 0
                        for tap in taps:
                            sh = tap if conv else 3
                            for k in range(8):
                                MM(P, pm[pi][:, :], w_of(tap)[:, k, cc * 128:(cc + 1) * 128],
                                   hT[:, k, tb * 512 + sh: tb * 512 + sh + 512], i == 0, i == n_mm - 1,
                                   wkeys + ["hT"], [("pm", pi)])
                                i += 1
                        dst = sf[:, tb * 512:(tb + 1) * 512]
                        if name == "q":
                            P.op("act", lambda e, o=dst, i_=pm[pi][:, :]: e.mul(out=o, in_=i_, mul=0.125),
                                 [("pm", pi)], [("stfm", sfi)])
                        elif name == "k":
                            CP(P, "dve" if state["ev"] % 2 else "act", dst, pm[pi][:, :], [("pm", pi)], [("stfm", sfi)])
                            state["ev"] += 1
                        elif name == "g":
                            ACT(P, dst, pm[pi][:, :], AF.Sigmoid, [("pm", pi)], [("stfm", sfi)])
                        else:
                            j = (c0 - 4096 + cc * 128) // 128
                            ACT(P, dst, pm[pi][:, :], AF.Silu, [("pm", pi), "cbcol"], [("stfm", sfi)],
                                bias=cbcol[:, j:j + 1])
                    if name == "q":
                        r0 = c0 + cc * 128
                        dram = T["qk_s"][r0:r0 + 128, :]
                    elif name == "k":
                        r0 = 1024 + (c0 - 1024) + cc * 128
                        dram = T["qk_s"][r0:r0 + 128, :]
                    elif name == "g":
                        r0 = c0 - 6160 + cc * 128
                        dram = T["g_s"][r0:r0 + 128, :]
                    elif name == "B":
                        r0 = c0 - 5120 + cc * 128
                        dram = T["BT_s"][r0:r0 + 128, :]
                    else:
                        r0 = c0 - 5632 + cc * 128
                        dram = T["CT_s"][r0:r0 + 128, :]
                    P.dma(dram, sf[:, :], reads=[("stfm", sfi)], writes=[("dram_fm", name, r0)])
            if name in ("v", "z", "xs", "B", "dt"):
                for tt in range(NT):
                    pi = next_pm()
                    n_mm = len(taps) * 8 + (1 if conv else 0)
                    i = 0
                    for tap in taps:
                        sh = tap if conv else 3
                        for k in range(8):
                            MM(P, pm[pi][:, 0:ncol], hT[:, k, tt * 128 + sh: tt * 128 + sh + 128],
                               w_of(tap)[:, k, 0:ncol], i == 0, i == n_mm - 1, wkeys + ["hT"], [("pm", pi)])
                            i += 1
                    if conv:
                        cx0 = c0 - 4096
                        MM(P, pm[pi][:, 0:ncol], ones_row[0:1, :], cbrow[0:1, cx0:cx0 + ncol], False, True,
                           ["ones", "cbrow"], [("pm", pi)])
                    if name == "dt":
                        CP(P, "dve", st_dt[:, tt, :], pm[pi][:, 0:16], [("pm", pi)], ["stdt"])
                        continue
                    q4 = tt % 4
                    si = (state["tm"] // 4) % 2
                    state["tm"] += 1
                    dst = st_tm[si][:, q4, :]
                    if conv:
                        ACT(P, dst, pm[pi][:, 0:ncol], AF.Silu, [("pm", pi)], [("sttm", si)])
                    else:
                        CP(P, "dve" if state["ev"] % 2 else "act", dst, pm[pi][:, 0:ncol], [("pm", pi)], [("sttm", si)])
                        state["ev"] += 1
                    if q4 == 3:
                        t0 = (tt - 3) * 128
                        if name == "v":
                            dram = T["v_s"][t0:t0 + 512, c0 - 2048:c0 - 2048 + 256]
                        elif name == "z":
                            dram = T["z_s"][t0:t0 + 512, c0 - 3072:c0 - 3072 + 256]
                        elif name == "xs":
                            dram = T["xs_s"][t0:t0 + 512, c0 - 4096:c0 - 4096 + 256]
                        else:
                            dram = T["B_s"][t0:t0 + 512, c0 - 5120:c0 - 5120 + 256]
                        P.dma(dram.rearrange("(a p) c -> p a c", p=128), st_tm[si][:], reads=[("sttm", si)],
                              writes=[("dram_tm", name, c0, tt)])
                if name == "dt":
                    P.dma(T["dt_s"].rearrange("(a p) c -> p a c", p=128), st_dt[:], reads=["stdt"], writes=["dram_dt"])
        P.emit()


POOLS = {}
_SEM_ES = []


def declare_tensors(nc, debug):
    T = {}

    def inp(name, shape, dt):
        T[name] = nc.dram_tensor(name, shape, dt, kind="ExternalInput").ap()

    def scr(name, shape, dt):
        kind = "ExternalOutput" if debug else "Internal"
        T[name] = nc.dram_tensor(name, shape, dt, kind=kind).ap()

    inp("x", [S, D], F32)
    inp("w_in", [128, 8, INW], F32)
    inp("gm", [128, 8], F32)
    inp("conv_w_col", [128, 16, 4], F32)
    inp("conv_b_row", [1, 2048], F32)
    inp("conv_b_col", [128, 16], F32)
    inp("ident", [128, 128], BF16)
    scr("qk_s", [2048, S], BF16)
    scr("v_s", [S, 1024], BF16)
    scr("z_s", [S, 1024], BF16)
    scr("xs_s", [S, 1024], BF16)
    scr("B_s", [S, 512], BF16)
    scr("BT_s", [512, S], BF16)
    scr("CT_s", [512, S], BF16)
    scr("dt_s", [S, 16], F32)
    scr("g_s", [2048, S], BF16)
    inp("qaug", [8, 2, S], BF16)
    inp("kbias", [128, 8, 36], F32)
    inp("dbias", [8, 128, 4, 512], F32)
    for nm in ("lam_q1", "lam_k1", "lam_q2", "lam_k2"):
        inp(nm, [1, 64], F32)
    inp("g_subln", [1, 128], F32)
    scr("attT_s", [1024, S], BF16)
    inp("ssd_cst", [128, 5, 128], F32)
    for nm in ("dt_bias", "a_log", "d_skip"):
        inp(nm, [1, 16], F32)
    inp("g_ssd", [1, D], F32)
    scr("ybT_s", [1024, S], BF16)
    for nm in ("w_a", "w_b", "w_o"):
        inp(nm, [128, 8, D], F32)
    inp("g_ffn", [1, D], F32)
    scr("x1_s", [S, D], F32)
    scr("h2T_s", [1024, S], BF16)
    inp("UT_h", [128, 8, 128, 128], F32)
    inp("V_h", [128, 128, D], F32)
    inp("wq_h", [128, 8, 2048], F32)
    inp("skT_h", [128, 16, 128], F32)
    inp("iota128", [128, 128], F32)
    inp("thr15", [128, 15], F32)
    inp("g_final", [1, D], F32)
    scr("UTb_s", [128, 8, 128, 128], BF16)
    scr("Vb_s", [128, 128, D], BF16)
    scr("route_s", [3, 128, S], F32)
    T["out"] = nc.dram_tensor("out", [S, D], F32, kind="ExternalOutput").ap()
    return T


def build_program(debug=False, phases="ABCD012", **kw):
    nc = bass.Bass("TRN2", target_bir_lowering=False)
    T = declare_tensors(nc, debug)
    _SEM_ES.append(ExitStack())
    POOLS[id(nc)] = SemPool(nc, _SEM_ES[-1])
    if "A" in phases:
        phase_a(nc, T)
    if "B" in phases:
        phase_b(nc, T, **kw.get("b", {}))
    if "C" in phases:
        phase_c(nc, T, **kw.get("c", {}))
    if "D" in phases:
        phase_d(nc, T)
    if "1" in phases:
        phase_e1(nc, T, **kw.get("e1", {}))
    if "2" in phases:
        phase_e2(nc, T, **kw.get("e2", {}))
    return nc


def host_inputs(inp, b):
    f = np.float32
    d = {}
    d["x"] = np.ascontiguousarray(inp["x"][b], dtype=f)
    d["w_in"] = np.ascontiguousarray(inp["w_in"][0].reshape(8, 128, INW).transpose(1, 0, 2), dtype=f)
    d["gm"] = np.ascontiguousarray(inp["g_mix"][0].reshape(8, 128).T, dtype=f)
    d["conv_w_col"] = np.ascontiguousarray(inp["conv_w"][0].reshape(4, 16, 128).transpose(2, 1, 0), dtype=f)
    d["conv_b_row"] = np.ascontiguousarray(inp["conv_b"][0].reshape(1, 2048), dtype=f)
    d["conv_b_col"] = np.ascontiguousarray(inp["conv_b"][0].reshape(16, 128).T, dtype=f)
    d["ident"] = np.eye(128).astype(ml_dtypes.bfloat16)
    qaug, kbias, dbias = attn_consts()
    d["qaug"], d["kbias"], d["dbias"] = qaug, kbias, dbias
    for nm in ("lam_q1", "lam_k1", "lam_q2", "lam_k2"):
        d[nm] = np.ascontiguousarray(inp[nm][0].reshape(1, 64), dtype=f)
    d["g_subln"] = np.ascontiguousarray(inp["g_subln"][0].reshape(1, 128), dtype=f)
    d["ssd_cst"] = ssd_consts()
    for nm in ("dt_bias", "a_log", "d_skip"):
        d[nm] = np.ascontiguousarray(inp[nm][0].reshape(1, 16), dtype=f)
    d["g_ssd"] = np.ascontiguousarray(inp["g_ssd"][0].reshape(1, D), dtype=f)
    for nm, src in (("w_a", "w_branch_a"), ("w_b", "w_branch_b"), ("w_o", "w_out")):
        d[nm] = np.ascontiguousarray(inp[src][0].reshape(8, 128, D).transpose(1, 0, 2), dtype=f)
    d["g_ffn"] = np.ascontiguousarray(inp["g_ffn"][0].reshape(1, D), dtype=f)
    d.update(shared_peer_inputs(inp))
    return d


_SHARED = {}


def shared_peer_inputs(inp):
    key = id(inp["expert_u"])
    if key in _SHARED:
        return _SHARED[key]
    f = np.float32
    d = {}
    U = np.asarray(inp["expert_u"][0], dtype=f)
    d["UT_h"] = np.ascontiguousarray(U.reshape(128, 128, 8, 128).transpose(3, 2, 1, 0))
    d["V_h"] = np.ascontiguousarray(np.asarray(inp["expert_v"][0], dtype=f).reshape(128, 128, D))
    d["wq_h"] = np.ascontiguousarray(inp["w_query"][0].reshape(8, 128, 2048).transpose(1, 0, 2), dtype=f)
    sk = np.asarray(inp["sub_keys"][0], dtype=f)
    d["skT_h"] = np.ascontiguousarray(sk.reshape(16, 128, 128).transpose(2, 0, 1))
    d["iota128"] = np.ascontiguousarray(np.broadcast_to(np.arange(128, dtype=f), (128, 128)))
    d["thr15"] = np.ascontiguousarray(np.broadcast_to(16.0 * np.arange(1, 16, dtype=f), (128, 15)))
    d["g_final"] = np.ascontiguousarray(inp["g_final"].reshape(1, D), dtype=f)
    _SHARED.clear()
    _SHARED[key] = d
    return d


def kernel(**inputs):
    nc = build_program()
    in_maps = [host_inputs(inputs, b) for b in range(8)]
    res = run_bass_kernel_spmd(nc, in_maps, core_ids=list(range(8)))
    return np.stack([r["out"] for r in res.results], axis=0)


LAM0 = 0.8 - 0.6 * math.exp(-0.3 * 0)
SLOPES = [2.0 ** (-(i + 1)) for i in range(8)]
BAND_CUT = 64.0


def phase_b(nc, T, heads=range(8)):
    with ExitStack() as es:
        P = Prog(nc, es, "B")
        A = Alloc(nc, es)
        ident = A.sb("b_ident", [128, 128], BF16)
        qa = [[A.sb(f"b_qa{s}{m}", [66, S], BF16) for m in range(2)] for s in range(2)]
        ka = [[A.sb(f"b_ka{s}{m}", [66, S], BF16) for m in range(2)] for s in range(2)]
        va = [A.sb(f"b_va{s}", [128, NT, 129], BF16) for s in range(2)]
        dbias = [A.sb(f"b_db{s}", [128, 4, 512], F32) for s in range(2)]
        kbias = A.sb("b_kbias", [128, 8, 36], F32)
        lamv = A.sb("b_lamv", [128, 4, 64], F32)
        lamt = A.sb("b_lamt", [128, 8], F32)
        gs = A.sb("b_gs", [128, 128], F32)
        pT = [A.sb(f"b_pT{r}", [128, 512], BF16) for r in range(4)]
        sbias = [A.sb(f"b_sb{r}", [128, 512], F32) for r in range(2)]
        rec = [A.sb(f"b_rec{r}", [128, 8], F32) for r in range(2)]
        o1 = [A.sb(f"b_o1{r}", [128, 128], F32) for r in range(2)]
        oo = [A.sb(f"b_oo{r}", [128, 128], F32) for r in range(2)]
        osq = A.sb("b_osq", [128, 128], F32)
        on = [A.sb(f"b_on{r}", [128, 128], BF16) for r in range(2)]
        stage = [A.sb(f"b_st{r}", [128, 512], BF16) for r in range(2)]
        psS = [A.ps(f"b_ps{i}", [128, 512], F32) for i in range(4)]
        accb = [A.ps(f"b_acc{i}", [128, 512], F32) for i in range(3)]
        ptr = A.ps("b_ptr", [128, 4, 128], BF16)

        def acc(m, i):
            s = m * 4 + i
            return accb[s // 3][:, (s % 3) * 129:(s % 3) * 129 + 129]

        P.dma(ident[:], T["ident"], writes=["ident"])
        P.dma(kbias[:], T["kbias"], writes=["kbias"])
        for j, nm in enumerate(["lam_q1", "lam_k1", "lam_q2", "lam_k2"]):
            P.dma(lamv[:, j, :], T[nm].partition_broadcast(128), writes=[("lamv", j)])
        P.dma(gs[:], T["g_subln"].partition_broadcast(128), writes=["gs"])
        TT(P, "dve", lamv[:, 0, :], lamv[:, 0, :], lamv[:, 1, :], ALU.mult, [("lamv", 0), ("lamv", 1)], [("lamv", 0)])
        TT(P, "dve", lamv[:, 2, :], lamv[:, 2, :], lamv[:, 3, :], ALU.mult, [("lamv", 2), ("lamv", 3)], [("lamv", 2)])
        RSUM(P, "dve", lamt[:, 0:1], lamv[:, 0, :], [("lamv", 0)], ["lamt"])
        RSUM(P, "dve", lamt[:, 1:2], lamv[:, 2, :], [("lamv", 2)], ["lamt"])
        ACT(P, lamt[:, 2:4], lamt[:, 0:2], AF.Exp, ["lamt"], ["lamt"])
        TT(P, "dve", lamt[:, 4:5], lamt[:, 3:4], lamt[:, 2:3], ALU.subtract, ["lamt"], ["lamt"])
        TS(P, "dve", lamt[:, 4:5], lamt[:, 4:5], -LAM0, ALU.add, ["lamt"], ["lamt"])
        TS(P, "dve", gs[:], gs[:], 1.0 - LAM0, ALU.mult, ["gs"], ["gs"])
        for s in range(2):
            for m in range(2):
                MEMSET(P, "pool", ka[s][m][64:66, :], 1.0, [("ka1", s, m)])
            MEMSET(P, "pool", va[s][:, :, 128:129], 1.0, [("va1", s)])

        heads = list(heads)

        def load_head(idx):
            h = heads[idx]
            s = idx % 2
            for m in range(2):
                r0 = (h * 2 + m) * 64
                P.dma(qa[s][m][0:64, :], T["qk_s"][r0:r0 + 64, :], writes=[("qa", s, m)])
                P.dma(qa[s][m][64:66, :], T["qaug"][h], writes=[("qa", s, m)])
                P.dma(ka[s][m][0:64, :], T["qk_s"][1024 + r0:1024 + r0 + 64, :], writes=[("ka", s, m)])
            P.dma(va[s][:, :, 0:128], T["v_s"][:, h * 128:(h + 1) * 128].rearrange("(t p) c -> p t c", p=128),
                  writes=[("va", s)])
            P.dma(dbias[s][:], T["dbias"][h], writes=[("db", s)])

        load_head(0)
        st = {"ps": 0, "pt": 0, "fin": 0, "stg": 0}
        for idx, h in enumerate(heads):
            s = idx % 2
            if idx + 1 < len(heads):
                load_head(idx + 1)
            slope = SLOPES[h]
            for qb in range(8):
                for b3 in range(3):
                    MEMSET(P, "dve", accb[b3][:, 0:387], 0.0, [("acc", b3)])
                units = []
                for kt in range(4 * qb + 4):
                    j = kt - 4 * qb
                    if j < 0:
                        mind = qb * 512 - (kt * 128 + 127)
                        if slope * mind > BAND_CUT:
                            continue
                    for m in range(2):
                        units.append((kt, j, m))

                def stage1(u, kt, j, m):
                    c0 = 128 * j if j > 0 else 0
                    dd = (4 * qb - kt) + 3
                    pi = u % 4
                    MM(P, psS[pi][:, c0:512], ka[s][m][0:66, kt * 128:(kt + 1) * 128],
                       qa[s][m][0:66, qb * 512 + c0:(qb + 1) * 512], True, True,
                       [("ka", s, m), ("ka1", s, m), ("qa", s, m)], [("ps", pi)])
                    if j >= 0:
                        TT(P, "dve", sbias[m][:, c0:512], psS[pi][:, c0:512], dbias[s][:, j, c0:512], ALU.add,
                           [("ps", pi), ("db", s)], [("sbias", m)])
                        ACT(P, pT[pi][:, c0:512], sbias[m][:, c0:512], AF.Exp, [("sbias", m), "kbias"],
                            [("pT", pi)], bias=kbias[:, h, dd:dd + 1])
                    else:
                        ACT(P, pT[pi][:, :], psS[pi][:, :], AF.Exp, [("ps", pi), "kbias"], [("pT", pi)],
                            bias=kbias[:, h, dd:dd + 1])

                def stage2(u, kt, j, m):
                    pi = u % 4
                    for i in range(max(j, 0), 4):
                        sl = m * 4 + i
                        MM(P, acc(m, i), pT[pi][:, i * 128:(i + 1) * 128], va[s][:, kt, :], False, False,
                           [("pT", pi), ("va", s), ("va1", s)], [("acc", sl // 3)], skip_group_check=True)

                LOOK = 2
                ub = st["ps"]
                for x in range(len(units) + LOOK):
                    if x < len(units):
                        stage1(ub + x, *units[x])
                    if x >= LOOK:
                        stage2(ub + x - LOOK, *units[x - LOOK])
                st["ps"] = ub + len(units)
                sg = st["stg"] % 2
                st["stg"] += 1
                for i in range(4):
                    f = st["fin"] % 2
                    st["fin"] += 1
                    a0, a1 = acc(0, i), acc(1, i)
                    k0, k1 = ("acc", (0 * 4 + i) // 3), ("acc", (4 + i) // 3)
                    P.op("dve", lambda e, o=rec[f][:, 0:1], i_=a0[:, 128:129]: e.reciprocal(out=o, in_=i_), [k0], [("rec", f)])
                    P.op("dve", lambda e, o=rec[f][:, 1:2], i_=a1[:, 128:129]: e.reciprocal(out=o, in_=i_), [k1], [("rec", f)])
                    TS(P, "dve", rec[f][:, 2:3], rec[f][:, 1:2], lamt[:, 4:5], ALU.mult, [("rec", f), "lamt"], [("rec", f)])
                    TS(P, "dve", o1[f][:], a0[:, 0:128], rec[f][:, 0:1], ALU.mult, [k0, ("rec", f)], [("o1", f)])
                    STT(P, "dve", oo[f][:], a1[:, 0:128], rec[f][:, 2:3], o1[f][:], ALU.mult, ALU.add,
                        [k1, ("rec", f), ("o1", f)], [("oo", f)])
                    TT(P, "dve", osq[:], oo[f][:], oo[f][:], ALU.mult, [("oo", f)], ["osq"])
                    RSUM(P, "dve", rec[f][:, 3:4], osq[:], ["osq"], [("rec", f)])
                    ACT(P, rec[f][:, 4:5], rec[f][:, 3:4], AF.Ln, [("rec", f)], [("rec", f)], bias=EPS, scale=1.0 / 128)
                    ACT(P, rec[f][:, 5:6], rec[f][:, 4:5], AF.Exp, [("rec", f)], [("rec", f)], scale=-0.5)
                    STT(P, "dve", on[f][:], oo[f][:], rec[f][:, 5:6], gs[:], ALU.mult, ALU.mult,
                        [("oo", f), ("rec", f), "gs"], [("on", f)])
                    TR(P, ptr[:, i, :], on[f][:], ident[:], [("on", f), "ident"], [("ptr", i)])
                    CP(P, "act", stage[sg][:, i * 128:(i + 1) * 128], ptr[:, i, :], [("ptr", i)], [("stage", sg)])
                P.dma(T["attT_s"][h * 128:(h + 1) * 128, qb * 512:(qb + 1) * 512], stage[sg][:],
                      reads=[("stage", sg)], writes=[("attT", h, qb)])
        P.emit()


def attn_consts():
    bf = ml_dtypes.bfloat16
    pos = np.arange(S)
    qrel = pos % 512
    qaug = np.zeros((8, 2, S), np.float32)
    kbias = np.zeros((128, 8, 36), np.float32)
    dbias = np.zeros((8, 128, 4, 512), np.float32)
    ki = np.arange(128)
    qi = np.arange(512)
    for h in range(8):
        sl = SLOPES[h]
        qaug[h, 0] = -sl * (qrel % 256)
        qaug[h, 1] = -sl * 256.0 * (qrel // 256)
        for dd in range(36):
            kbias[:, h, dd] = sl * (ki - 128.0 * (dd - 3))
        for j in range(4):
            k = (128 * j + ki)[:, None]
            q = qi[None, :]
            masked = (k // 64) > (q // 64)
            fut = (k > q) & ~masked
            dbias[h, :, j, :] = np.where(masked, -30000.0, np.where(fut, -2.0 * sl * (k - q), 0.0))
    return qaug.astype(bf), kbias, dbias


def phase_c(nc, T, ntiles=NT):
    with ExitStack() as es:
        P = Prog(nc, es, "C")
        A = Alloc(nc, es)
        identb = A.sb("c_identb", [128, 128], BF16)
        cst = A.sb("c_cst", [128, 5, 128], F32)
        BT = A.sb("c_BT", [128, 4, S], BF16)
        CT = A.sb("c_CT", [128, 4, S], BF16)
        gssd = A.sb("c_gssd", [128, D], F32)
        pv = A.sb("c_pv", [128, 3, 16], F32)
        dtr = [A.sb(f"c_dtr{i}", [128, 16], F32) for i in range(2)]
        xs = [A.sb(f"c_xs{i}", [128, 16, 64], BF16) for i in range(2)]
        zt = [A.sb(f"c_zt{i}", [128, D], BF16) for i in range(2)]
        Bt = [A.sb(f"c_Bt{i}", [128, 512], BF16) for i in range(2)]
        sc_ = [A.sb(f"c_sc{i}", [128, 12, 16], F32) for i in range(2)]
        sm_ = [A.sb(f"c_sm{i}", [128, 32], F32) for i in range(2)]
        ead_ = [A.sb(f"c_ead{i}", [128, 32], F32) for i in range(2)]
        atri_ = [A.sb(f"c_atri{i}", [128, 16, 128], F32) for i in range(2)]
        LT_ = [A.sb(f"c_LT{i}", [128, 16, 128], BF16) for i in range(2)]
        cbL_ = [A.sb(f"c_cbL{i}", [128, 16, 128], BF16) for i in range(2)]
        Xdt_ = [A.sb(f"c_Xdt{i}", [128, 16, 64], BF16) for i in range(2)]
        Xdd_ = [A.sb(f"c_Xdd{i}", [128, 16, 64], BF16) for i in range(2)]
        t1_ = [A.sb(f"c_t1{i}", [128, 16, 64], F32) for i in range(2)]
        y1_ = [A.sb(f"c_y1{i}", [128, 16, 64], F32) for i in range(2)]
        t2_ = [A.sb(f"c_t2{i}", [128, 16, 64], F32) for i in range(2)]
        h32 = A.sb("c_h32", [128, 16, 64], F32)
        hbf = A.sb("c_hbf", [128, D], BF16)
        sz_ = [A.sb(f"c_sz{i}", [128, D], F32) for i in range(2)]
        yz_ = [A.sb(f"c_yz{i}", [128, D], F32) for i in range(2)]
        sq_ = [A.sb(f"c_sq{i}", [128, D], F32) for i in range(2)]
        rs_ = [A.sb(f"c_rs{i}", [128, 4], F32) for i in range(2)]
        yn_ = [A.sb(f"c_yn{i}", [128, D], BF16) for i in range(2)]
        stage = [A.sb(f"c_st{i}", [128, 8, 512], BF16) for i in range(2)]
        big = [A.ps(f"c_big{i}", [128, 1024], F32) for i in range(2)]
        cb = A.ps("c_cb", [128, 4, 128], F32)
        small = A.ps("c_small", [128, 32], F32)
        ptr = A.ps("c_ptr", [128, 8, 128], BF16)

        tri, ones, negtri, identF, maskT = (cst[:, i, :] for i in range(5))
        P.dma(identb[:], T["ident"], writes=["identb"])
        P.dma(cst[:], T["ssd_cst"], writes=["cst"])
        P.dma(BT[:], T["BT_s"].rearrange("(g n) t -> n g t", n=128), writes=["BT"])
        P.dma(CT[:], T["CT_s"].rearrange("(g n) t -> n g t", n=128), writes=["CT"])
        P.dma(gssd[:], T["g_ssd"].partition_broadcast(128), writes=["gssd"])
        for j, nm in enumerate(["dt_bias", "a_log", "d_skip"]):
            P.dma(pv[:, j, :], T[nm].partition_broadcast(128), writes=[("pv", j)])
        ACT(P, pv[:, 1, :], pv[:, 1, :], AF.Exp, [("pv", 1)], [("pv", 1)])
        TS(P, "dve", pv[:, 1, :], pv[:, 1, :], -1.0, ALU.mult, [("pv", 1)], [("pv", 1)])
        MEMSET(P, "pool", h32[:], 0.0, ["h32"])
        MEMSET(P, "pool", hbf[:], 0.0, ["hbf"])
        dtb_bc, negA, dskip = pv[:, 0, :], pv[:, 1, :], pv[:, 2, :]

        def load(tt):
            b = tt % 2
            r = slice(tt * 128, (tt + 1) * 128)
            P.dma(dtr[b][:], T["dt_s"][r, :], writes=[("dtr", b)])
            P.dma(xs[b][:].rearrange("p h d -> p (h d)"), T["xs_s"][r, :], writes=[("xs", b)])
            P.dma(zt[b][:], T["z_s"][r, :], writes=[("zt", b)])
            P.dma(Bt[b][:], T["B_s"][r, :], writes=[("Bt", b)])

        WK = set(["x1", "nx", "mn", "ee", "ll", "rr", "dt", "a", "dd", "dS", "w2", "sm", "ead", "atri", "Xdt", "Xdd",
                  "t1", "y1", "t2", "sz", "yz", "sq", "rs", "yn"])
        cur = {"b": 0}
        _op = P.op

        def op2(eng, fn, reads=(), writes=()):
            bb = cur["b"]

            def kx(x):
                if isinstance(x, str) and x in WK:
                    return (x, bb)
                if isinstance(x, tuple) and x[0] in ("LT", "cbL"):
                    return x + (bb,)
                return x
            _op(eng, fn, [kx(x) for x in reads], [kx(x) for x in writes])

        P.op = op2
        bigc = {"i": 0}
        ctail = []

        def nbig():
            i = bigc["i"] % 2
            bigc["i"] += 1
            return i

        load(0)
        for tt in range(ntiles):
            b = tt % 2
            if tt + 1 < ntiles:
                load(tt + 1)
            tk = slice(tt * 128, (tt + 1) * 128)
            sc = sc_[b]; sm = sm_[b]; ead = ead_[b]; atri = atri_[b]; LT = LT_[b]; cbL = cbL_[b]; Xdt = Xdt_[b]; Xdd = Xdd_[b]; t1 = t1_[b]; y1 = y1_[b]; t2 = t2_[b]; sz = sz_[b]; yz = yz_[b]; sq = sq_[b]; rs = rs_[b]; yn = yn_[b]
            cur["b"] = b
            x1, nx, mn, ee, ll, rr, dt, a_, dd, dS, w2 = (sc[:, i, :] for i in range(11))
            TT(P, "dve", x1, dtr[b][:], dtb_bc, ALU.add, [("dtr", b), ("pv", 0)], ["x1"])
            TS(P, "dve", nx, x1, -1.0, ALU.mult, ["x1"], ["nx"])
            TT(P, "dve", mn, x1, nx, ALU.min, ["x1", "nx"], ["mn"])
            ACT(P, ee, mn, AF.Exp, ["mn"], ["ee"])
            ACT(P, ll, ee, AF.Ln, ["ee"], ["ll"], bias=1.0)
            TS(P, "dve", rr, x1, 0.0, ALU.max, ["x1"], ["rr"])
            TT(P, "dve", dt, rr, ll, ALU.add, ["rr", "ll"], ["dt"])
            TT(P, "dve", a_, dt, negA, ALU.mult, ["dt", ("pv", 1)], ["a"])
            MM(P, small[:, 0:16], tri, a_, True, True, ["cst", "a"], ["small"])
            MM(P, small[:, 16:32], ones, a_, True, True, ["cst", "a"], ["small"])
            CP(P, "dve", sm[:], small[:], ["small"], ["sm"])
            TT(P, "dve", dd, sm[:, 16:32], sm[:, 0:16], ALU.subtract, ["sm"], ["dd"])
            ACT(P, ead[:], sm[:], AF.Exp, ["sm"], ["ead"])
            ACT(P, dS, dd, AF.Exp, ["dd"], ["dS"])
            TT(P, "dve", w2, dt, dS, ALU.mult, ["dt", "dS"], ["w2"])
            ea, dec = ead[:, 0:16], ead[:, 16:32]
            TT(P, "dve", atri[:], a_.unsqueeze(2).to_broadcast([128, 16, 128]),
               tri.unsqueeze(1).to_broadcast([128, 16, 128]), ALU.mult, ["a", "cst"], ["atri"])
            for half in range(2):
                bi = nbig()
                for j in range(2):
                    h0 = half * 8 + j * 4
                    o = big[bi][:, j * 512:(j + 1) * 512]
                    MM(P, o, ones, atri[:, h0:h0 + 4, :], True, False, ["cst", "atri"], [("big", bi)])
                    MM(P, o, negtri, a_[:, h0:h0 + 4].unsqueeze(2).to_broadcast([128, 4, 128]), False, False,
                       ["cst", "a"], [("big", bi)])
                    MM(P, o, identF, maskT.unsqueeze(1).to_broadcast([128, 4, 128]), False, True,
                       ["cst"], [("big", bi)])
                ACT(P, LT[:, half * 8:(half + 1) * 8, :].rearrange("p h l -> p (h l)"), big[bi][:], AF.Exp,
                    [("big", bi)], [("LT", half)])
            for g in range(4):
                MM(P, cb[:, g, :], BT[:, g, tk], CT[:, g, tk], True, True, ["BT", "CT"], ["cb"])
            for g in range(4):
                TT(P, "dve", cbL[:, 4 * g:4 * g + 4, :], cb[:, g, :].unsqueeze(1).to_broadcast([128, 4, 128]),
                   LT[:, 4 * g:4 * g + 4, :], ALU.mult, ["cb", ("LT", g // 2)], [("cbL", g)])
            TT(P, "pool", Xdt[:], xs[b][:], dt.unsqueeze(2).to_broadcast([128, 16, 64]), ALU.mult,
               [("xs", b), "dt"], ["Xdt"])
            TT(P, "pool", Xdd[:], xs[b][:], w2.unsqueeze(2).to_broadcast([128, 16, 64]), ALU.mult,
               [("xs", b), "w2"], ["Xdd"])
            while ctail:
                ctail.pop(0)()
            bo = nbig()
            for g in range(4):
                MM(P, big[bo][:, g * 256:(g + 1) * 256], CT[:, g, tk], hbf[:, g * 256:(g + 1) * 256], True, True,
                   ["CT", "hbf"], [("big", bo)])
            TT(P, "dve", t1[:], big[bo][:].rearrange("p (h d) -> p h d", d=64),
               ea.unsqueeze(2).to_broadcast([128, 16, 64]), ALU.mult, [("big", bo), "ead"], ["t1"])
            by = nbig()
            for h in range(16):
                MM(P, big[by][:, h * 64:(h + 1) * 64], cbL[:, h, :], Xdt[:, h, :], True, True,
                   [("cbL", h // 4), "Xdt"], [("big", by)])
            TT(P, "dve", y1[:], big[by][:].rearrange("p (h d) -> p h d", d=64), t1[:], ALU.add,
               [("big", by), "t1"], ["y1"])
            bs = nbig()
            for g in range(4):
                MM(P, big[bs][:, g * 256:(g + 1) * 256], Bt[b][:, g * 128:(g + 1) * 128],
                   Xdd[:, 4 * g:4 * g + 4, :], True, True, [("Bt", b), "Xdd"], [("big", bs)])
            TT(P, "pool", h32[:], h32[:], dec.unsqueeze(2).to_broadcast([128, 16, 64]), ALU.mult,
               ["h32", "ead"], ["h32"])
            TT(P, "dve", h32[:], big[bs][:].rearrange("p (h d) -> p h d", d=64), h32[:], ALU.add,
               [("big", bs), "h32"], ["h32"])
            CP(P, "pool", hbf[:], h32[:].rearrange("p h d -> p (h d)"), ["h32"], ["hbf"])
            TT(P, "pool", t2[:], xs[b][:], dskip.unsqueeze(2).to_broadcast([128, 16, 64]), ALU.mult,
               [("xs", b), ("pv", 2)], ["t2"])
            TT(P, "dve", y1[:], y1[:], t2[:], ALU.add, ["y1", "t2"], ["y1"])
            ACT(P, sz[:], zt[b][:], AF.Silu, [("zt", b)], ["sz"])
            TT(P, "dve", yz[:], y1[:].rearrange("p h d -> p (h d)"), sz[:], ALU.mult, ["y1", "sz"], ["yz"])
            TT(P, "pool", sq[:], yz[:], yz[:], ALU.mult, ["yz"], ["sq"])
            RSUM(P, "dve", rs[:, 0:1], sq[:], ["sq"], ["rs"])
            ACT(P, rs[:, 1:2], rs[:, 0:1], AF.Ln, ["rs"], ["rs"], bias=EPS, scale=1.0 / D)
            ACT(P, rs[:, 2:3], rs[:, 1:2], AF.Exp, ["rs"], ["rs"], scale=-0.5)
            STT(P, "dve", yn[:], yz[:], rs[:, 2:3], gssd[:], ALU.mult, ALU.mult, ["yz", "rs", "gssd"], ["yn"])
            def tail(tt=tt, b=b, yn=yn):
                save = cur["b"]
                cur["b"] = b
                for k in range(8):
                    TR(P, ptr[:, k, :], yn[:, k * 128:(k + 1) * 128], identb[:], ["yn", "identb"], ["ptr"])
                sg = (tt // 4) % 2
                q4 = tt % 4
                CP(P, "act", stage[sg][:, :, q4 * 128:(q4 + 1) * 128], ptr[:], ["ptr"], [("stage", sg)])
                if q4 == 3 or tt == ntiles - 1:
                    t0 = (tt - q4) * 128
                    nn = (q4 + 1) * 128
                    P.dma(T["ybT_s"][:, t0:t0 + nn].rearrange("(k p) t -> p k t", p=128), stage[sg][:, :, 0:nn],
                          reads=[("stage", sg)], writes=[("ybT", tt)])
                cur["b"] = save

            ctail.append(tail)
        while ctail:
            ctail.pop(0)()
        P.emit()


def ssd_consts():
    t = np.arange(128)
    tri = (t[:, None] <= t[None, :]).astype(np.float32)
    ones = np.ones((128, 128), np.float32)
    negtri = -tri
    identF = np.eye(128, dtype=np.float32)
    maskT = np.where(t[None, :] >= t[:, None], 0.0, -30000.0).astype(np.float32)
    return np.ascontiguousarray(np.stack([tri, ones, negtri, identF, maskT], axis=1))


def phase_d(nc, T):
    with ExitStack() as es:
        P = Prog(nc, es, "D")
        A = Alloc(nc, es)
        identb = A.sb("d_identb", [128, 128], BF16)
        wtmp = [A.sb(f"d_wtmp{i}", [128, 8, 256], F32) for i in range(2)]
        W = [A.sb(f"d_W{i}", [128, 8, D], BF16) for i in range(3)]
        gffn = A.sb("d_gffn", [128, D], F32)
        inb = [[A.sb(f"d_in{j}{i}", [128, 8, 512], BF16) for i in range(2)] for j in range(4)]
        m1 = [A.sb(f"d_m1{i}", [128, 512], F32) for i in range(2)]
        m2 = [A.sb(f"d_m2{i}", [128, 512], F32) for i in range(2)]
        mT = A.sb("d_mT", [128, 8, 512], BF16)
        xt = [A.sb(f"d_xt{i}", [128, D], F32) for i in range(2)]
        x1 = [A.sb(f"d_x1{i}", [128, D], F32) for i in range(2)]
        sq = A.sb("d_sq", [128, D], F32)
        rs = [A.sb(f"d_rs{i}", [128, 4], F32) for i in range(2)]
        h2 = [A.sb(f"d_h2{i}", [128, D], BF16) for i in range(2)]
        stage = [A.sb(f"d_st{i}", [128, 8, 512], BF16) for i in range(2)]
        pm = [A.ps(f"d_pm{i}", [128, 512], F32) for i in range(6)]
        ptr = A.ps("d_ptr", [128, 8, 128], BF16)

        P.dma(identb[:], T["ident"], writes=["identb"])
        P.dma(gffn[:], T["g_ffn"].partition_broadcast(128), writes=["gffn"])
        ci = 0
        for wi, nm in enumerate(["w_a", "w_b", "w_o"]):
            for c in range(4):
                b = ci % 2
                ci += 1
                P.dma(wtmp[b][:], T[nm][:, :, c * 256:(c + 1) * 256], writes=[("wtmp", b)])
                CP(P, "pool" if ci % 2 else "dve", W[wi][:, :, c * 256:(c + 1) * 256], wtmp[b][:], [("wtmp", b)], [("W", wi)])
        srcs = [("attT_s", 0), ("ybT_s", 0), ("g_s", 0), ("g_s", 1024)]

        def load(tb):
            s = tb % 2
            for j, (nm, r0) in enumerate(srcs):
                P.dma(inb[j][s][:], T[nm][r0:r0 + 1024, tb * 512:(tb + 1) * 512].rearrange("(k p) t -> p k t", p=128),
                      writes=[("in", j, s)])

        pc = {"i": 0}

        def npm():
            i = pc["i"] % 6
            pc["i"] += 1
            return i

        load(0)
        dtail = []
        for tb in range(8):
            s = tb % 2
            if tb + 1 < 8:
                load(tb + 1)
            for cc in range(8):
                pa, pb = npm(), npm()
                for k in range(8):
                    MM(P, pm[pa][:], W[0][:, k, cc * 128:(cc + 1) * 128], inb[0][s][:, k, :], k == 0, k == 7,
                       [("W", 0), ("in", 0, s)], [("pm", pa)])
                for k in range(8):
                    MM(P, pm[pb][:], W[1][:, k, cc * 128:(cc + 1) * 128], inb[1][s][:, k, :], k == 0, k == 7,
                       [("W", 1), ("in", 1, s)], [("pm", pb)])
                r = cc % 2
                TT(P, "dve", m1[r][:], pm[pa][:], inb[2][s][:, cc, :], ALU.mult, [("pm", pa), ("in", 2, s)], [("m1", r)])
                TT(P, "dve", m2[r][:], pm[pb][:], inb[3][s][:, cc, :], ALU.mult, [("pm", pb), ("in", 3, s)], [("m2", r)])
                TT(P, "pool", mT[:, cc, :], m1[r][:], m2[r][:], ALU.add, [("m1", r), ("m2", r)], [("mT", cc)])
            for q4 in range(4):
                tt = tb * 4 + q4
                b = tt % 2
                P.dma(xt[b][:], T["x"][tt * 128:(tt + 1) * 128, :], writes=[("xt", b)])
                for half in range(2):
                    pi = npm()
                    for k in range(8):
                        MM(P, pm[pi][:], mT[:, k, q4 * 128:(q4 + 1) * 128], W[2][:, k, half * 512:(half + 1) * 512],
                           k == 0, k == 7, [("mT", k), ("W", 2)], [("pm", pi)])
                    TT(P, "dve", x1[b][:, half * 512:(half + 1) * 512], pm[pi][:], xt[b][:, half * 512:(half + 1) * 512],
                       ALU.add, [("pm", pi), ("xt", b)], [("x1", b)])
                while dtail:
                    dtail.pop(0)()
                P.dma(T["x1_s"][tt * 128:(tt + 1) * 128, :], x1[b][:], reads=[("x1", b)], writes=[("x1s", tt)])
                TT(P, "pool", sq[:], x1[b][:], x1[b][:], ALU.mult, [("x1", b)], ["sq"])
                RSUM(P, "dve", rs[b][:, 0:1], sq[:], ["sq"], [("rs", b)])
                ACT(P, rs[b][:, 1:2], rs[b][:, 0:1], AF.Ln, [("rs", b)], [("rs", b)], bias=EPS, scale=1.0 / D)
                ACT(P, rs[b][:, 2:3], rs[b][:, 1:2], AF.Exp, [("rs", b)], [("rs", b)], scale=-0.5)
                STT(P, "dve", h2[b][:], x1[b][:], rs[b][:, 2:3], gffn[:], ALU.mult, ALU.mult,
                    [("x1", b), ("rs", b), "gffn"], [("h2", b)])
                def tail(b=b, s=s, q4=q4):
                    for k in range(8):
                        TR(P, ptr[:, k, :], h2[b][:, k * 128:(k + 1) * 128], identb[:], [("h2", b), "identb"], ["ptr"])
                    CP(P, "act", stage[s][:, :, q4 * 128:(q4 + 1) * 128], ptr[:], ["ptr"], [("stage", s)])

                dtail.append(tail)
            while dtail:
                dtail.pop(0)()
            P.dma(T["h2T_s"][:, tb * 512:(tb + 1) * 512].rearrange("(k p) t -> p k t", p=128), stage[s][:],
                  reads=[("stage", s)], writes=[("h2T", tb)])
        P.emit()


def e0_setup(P, A, T):
    CJ0 = 2
    uf = [A.sb(f"e0_uf{i}", [128, 8, CJ0, 128], F32) for i in range(2)]
    ub = [A.sb(f"e0_ub{i}", [128, 8, CJ0, 128], BF16) for i in range(2)]
    vf = [A.sb(f"e0_vf{i}", [128, CJ0, D], F32) for i in range(2)]
    vb = [A.sb(f"e0_vb{i}", [128, CJ0, D], BF16) for i in range(2)]
    st = {"c": 0}

    def load(c):
        b = c % 2
        j0 = c * CJ0
        P.dma(uf[b][:], T["UT_h"][:, :, j0:j0 + CJ0, :], writes=[("uf", b)])
        P.dma(vf[b][:], T["V_h"][:, j0:j0 + CJ0, :], writes=[("vf", b)])

    def step():
        c = st["c"]
        if c >= 128 // CJ0:
            return False
        if c == 0:
            load(0)
        st["c"] += 1
        b = c % 2
        j0 = c * CJ0
        if c + 1 < 128 // CJ0:
            load(c + 1)
        CP(P, "pool", ub[b][:], uf[b][:], [("uf", b)], [("ub", b)])
        CP(P, "act", vb[b][:], vf[b][:], [("vf", b)], [("vb", b)])
        P.dma(T["UTb_s"][:, :, j0:j0 + CJ0, :], ub[b][:], reads=[("ub", b)], writes=[("UTb", c)])
        P.dma(T["Vb_s"][:, j0:j0 + CJ0, :], vb[b][:], reads=[("vb", b)], writes=[("Vb", c)])
        return True

    return step


def phase_e1(nc, T, nblocks=8):
    with ExitStack() as es:
        P = Prog(nc, es, "E1")
        P.skip_dist = 8
        A = Alloc(nc, es)
        e0_step = e0_setup(P, A, T)
        identF = A.sb("e1_identF", [128, 128], F32)
        iota16 = A.sb("e1_iota16", [128, 16], F32)
        thr15 = A.sb("e1_thr15", [128, 15], F32)
        wtmp = [A.sb("e1_wtmp0", [128, 8, 256], F32)] * 2
        Wq = A.sb("e1_Wq", [128, 8, 2048], BF16)
        skf = A.sb("e1_skf", [128, 16, 128], F32)
        skT = A.sb("e1_skT", [128, 16, 128], BF16)
        h2b = [A.sb(f"e1_h2b{i}", [128, 8, 512], BF16) for i in range(2)]
        qT = A.sb("e1_qT", [128, 16, 512], BF16)
        scs_ = [A.sb(f"e1_scs{i}", [128, 16, 128], F32) for i in range(2)]
        scs2 = A.sb("e1_scs2", [128, 16, 128], F32)
        v = A.sb("e1_v", [128, 16, 16], F32)
        iu = A.sb("e1_iu", [128, 16, 16], U32)
        if_ = A.sb("e1_if", [128, 16, 16], F32)
        cand = A.sb("e1_cand", [128, 8, 256], F32)
        cand2 = A.sb("e1_cand2", [128, 8, 256], F32)
        top = A.sb("e1_top", [128, 8, 16], F32)
        pu = A.sb("e1_pu", [128, 8, 16], U32)
        pf = A.sb("e1_pf", [128, 8, 16], F32)
        ge = A.sb("e1_ge", [128, 8, 16, 16], F32)
        ai = A.sb("e1_ai", [128, 8, 16], F32)
        bi = A.sb("e1_bi", [128, 8, 16], F32)
        dd = A.sb("e1_dd", [128, 8, 16], F32)
        zz = A.sb("e1_zz", [128, 16], F32)
        rt = A.sb("e1_rt", [128, 3, 128], F32)
        rstage = [A.sb(f"e1_rst{i}", [128, 3, 512], F32) for i in range(2)]
        pq = [A.ps(f"e1_pq{i}", [128, 512], F32) for i in range(2)]
        scp = [A.ps(f"e1_scp{i}", [128, 512], F32) for i in range(4)]
        ptr = A.ps("e1_ptr", [128, 3, 128], F32)

        P.dma(identF[:], T["ssd_cst"][:, 3, :], writes=["identF"])
        P.dma(iota16[:], T["iota128"][:, 0:16], writes=["iota16"])
        P.dma(thr15[:], T["thr15"], writes=["thr15"])
        P.dma(skf[:], T["skT_h"], writes=["skf"])
        CP(P, "dve", skT[:], skf[:], ["skf"], ["skT"])
        for c in range(8):
            b = 0
            P.dma(wtmp[b][:], T["wq_h"][:, :, c * 256:(c + 1) * 256], writes=[("wtmp", b)])
            CP(P, "pool" if c % 2 else "dve", Wq[:, :, c * 256:(c + 1) * 256], wtmp[b][:], [("wtmp", b)], ["Wq"])

        def load(tb):
            P.dma(h2b[tb % 2][:], T["h2T_s"][:, tb * 512:(tb + 1) * 512].rearrange("(k p) t -> p k t", p=128),
                  writes=[("h2b", tb % 2)])

        def vop(fn, reads, writes):
            P.op("dve", fn, reads, writes)

        load(0)

        def stageS(n):
            tb, q4 = divmod(n, 4)
            s = tb % 2
            if q4 == 0:
                if tb + 1 < nblocks:
                    load(tb + 1)
                for c in range(16):
                    pi = c % 2
                    for k in range(8):
                        MM(P, pq[pi][:], Wq[:, k, c * 128:(c + 1) * 128], h2b[s][:, k, :], k == 0, k == 7,
                           ["Wq", ("h2b", s)], [("pq", pi)])
                    CP(P, "act" if c % 2 else "dve", qT[:, c, :], pq[pi][:], [("pq", pi)], [("qT", c)])
            for _ in range(64 // (4 * nblocks) + 1):
                e0_step()
            for c in range(16):
                MM(P, scp[c // 4][:, (c % 4) * 128:(c % 4 + 1) * 128], qT[:, c, q4 * 128:(q4 + 1) * 128],
                   skT[:, c, :], True, True, [("qT", c), "skT"], [("scp", c // 4)])
            par = n % 2
            for g4 in range(4):
                CP(P, "act", scs_[par][:, g4 * 4:(g4 + 1) * 4, :].rearrange("p c k -> p (c k)"), scp[g4][:],
                   [("scp", g4)], [("scs", par, g4)])

        def stageD(n):
            tb, q4 = divmod(n, 4)
            s = tb % 2
            for _one in (0,):
                par = n % 2
                scs = scs_[par]
                for c in range(16):
                    kr = ("scs", par, c // 4)
                    vop(lambda e, c=c, scs=scs: e.max(out=v[:, c, 0:8], in_=scs[:, c, :]), [kr], [("v", c)])
                for c in range(16):
                    kr = ("scs", par, c // 4)
                    vop(lambda e, c=c, scs=scs: e.max_index(out=iu[:, c, 0:8], in_max=v[:, c, 0:8], in_values=scs[:, c, :]),
                        [kr, ("v", c)], [("iu", c)])
                for c in range(16):
                    kr = ("scs", par, c // 4)
                    vop(lambda e, c=c, scs=scs: e.match_replace(out=scs2[:, c, :], in_to_replace=v[:, c, 0:8],
                                                        in_values=scs[:, c, :], imm_value=-1e30),
                        [kr, ("v", c)], [("scs2", c)])
                for c in range(16):
                    vop(lambda e, c=c: e.max(out=v[:, c, 8:16], in_=scs2[:, c, :]), [("scs2", c)], [("v2", c)])
                for c in range(16):
                    vop(lambda e, c=c: e.max_index(out=iu[:, c, 8:16], in_max=v[:, c, 8:16], in_values=scs2[:, c, :]),
                        [("scs2", c), ("v2", c)], [("iu2", c)])
                allv = [("v", c) for c in range(16)] + [("v2", c) for c in range(16)]
                alli = [("iu", c) for c in range(16)] + [("iu2", c) for c in range(16)]
                CP(P, "dve", if_[:], iu[:], alli, ["if"])
                v4 = v[:].rearrange("p (n h) a -> p n h a", h=2)
                if4 = if_[:].rearrange("p (n h) a -> p n h a", h=2)
                TT(P, "dve", cand[:].rearrange("p n (a b) -> p n a b", b=16),
                   v4[:, :, 0, :].unsqueeze(3).to_broadcast([128, 8, 16, 16]),
                   v4[:, :, 1, :].unsqueeze(2).to_broadcast([128, 8, 16, 16]), ALU.add, allv, ["cand"])
                for n in range(8):
                    vop(lambda e, n=n: e.max(out=top[:, n, 0:8], in_=cand[:, n, :]), ["cand"], [("top", n)])
                for n in range(8):
                    vop(lambda e, n=n: e.max_index(out=pu[:, n, 0:8], in_max=top[:, n, 0:8], in_values=cand[:, n, :]),
                        ["cand", ("top", n)], [("pu", n)])
                for n in range(8):
                    vop(lambda e, n=n: e.match_replace(out=cand2[:, n, :], in_to_replace=top[:, n, 0:8],
                                                        in_values=cand[:, n, :], imm_value=-1e30),
                        ["cand", ("top", n)], [("cand2", n)])
                for n in range(8):
                    vop(lambda e, n=n: e.max(out=top[:, n, 8:16], in_=cand2[:, n, :]), [("cand2", n)], [("top2", n)])
                for n in range(8):
                    vop(lambda e, n=n: e.max_index(out=pu[:, n, 8:16], in_max=top[:, n, 8:16], in_values=cand2[:, n, :]),
                        [("cand2", n), ("top2", n)], [("pu2", n)])
                allt = [("top", n) for n in range(8)] + [("top2", n) for n in range(8)]
                allp = [("pu", n) for n in range(8)] + [("pu2", n) for n in range(8)]
                CP(P, "dve", pf[:], pu[:], allp, ["pf"])
                TT(P, "dve", ge[:, :, :, 0:15], pf[:].unsqueeze(3).to_broadcast([128, 8, 16, 15]),
                   thr15[:].unsqueeze(1).unsqueeze(1).to_broadcast([128, 8, 16, 15]), ALU.is_ge, ["pf", "thr15"], ["ge"])
                RSUM(P, "dve", ai[:], ge[:, :, :, 0:15], ["ge"], ["ai"])
                STT(P, "dve", bi[:], ai[:], -16.0, pf[:], ALU.mult, ALU.add, ["ai", "pf"], ["bi"])
                io4 = iota16[:].unsqueeze(1).unsqueeze(1).to_broadcast([128, 8, 16, 16])
                for x, (sel, half) in enumerate([(ai, 0), (bi, 1)]):
                    TT(P, "dve", ge[:], io4, sel[:].unsqueeze(3).to_broadcast([128, 8, 16, 16]), ALU.is_equal,
                       ["ai", "bi", "iota16"], ["ge"])
                    TT(P, "dve", ge[:], ge[:], if4[:, :, half, :].unsqueeze(2).to_broadcast([128, 8, 16, 16]),
                       ALU.mult, ["ge", "if"], ["ge"])
                    RSUM(P, "dve", rt[:, 1 + x, :].rearrange("p (n r) -> p n r", r=16), ge[:], ["ge"], [("rt", 1 + x)])
                TT(P, "dve", dd[:], top[:], top[:, :, 0:1].to_broadcast([128, 8, 16]), ALU.subtract, allt, ["dd"])
                ACT(P, dd[:], dd[:], AF.Exp, ["dd"], ["dd"])
                RSUM(P, "dve", zz[:, 0:8], dd[:], ["dd"], ["zz"])
                P.op("dve", lambda e: e.reciprocal(out=zz[:, 8:16], in_=zz[:, 0:8]), ["zz"], ["zz"])
                TT(P, "dve", rt[:, 0, :].rearrange("p (n r) -> p n r", r=16), dd[:],
                   zz[:, 8:16].unsqueeze(2).to_broadcast([128, 8, 16]), ALU.mult, ["dd", "zz"], [("rt", 0)])
                for x in range(3):
                    TR(P, ptr[:, x, :], rt[:, x, :], identF[:], [("rt", x), "identF"], ["ptr"])
                CP(P, "act", rstage[s][:, :, q4 * 128:(q4 + 1) * 128], ptr[:], ["ptr"], [("rstage", s)])
            if q4 == 3:
                P.dma(T["route_s"][:, :, tb * 512:(tb + 1) * 512].rearrange("x p t -> p x t"), rstage[s][:],
                      reads=[("rstage", s)], writes=[("route", tb)])

        ntl = nblocks * 4
        stageS(0)
        for n in range(ntl):
            if n + 1 < ntl:
                stageS(n + 1)
            stageD(n)
        while e0_step():
            pass
        P.emit()


def phase_e2(nc, T, ngroups=16):
    G = 256
    JH = 64
    with ExitStack() as es:
        P = Prog(nc, es, "E2")
        A = Alloc(nc, es)
        iota = A.sb("e2_iota", [128, 128], F32)
        gfin = A.sb("e2_gfin", [128, D], F32)
        Gsq = [A.sb(f"e2_Gs{i}", [128, G, JH], BF16) for i in range(2)]
        SBK = 16
        ohc = {"n": 0}
        OHI = [A.sb(f"e2_OHI{i}", [128, SBK, 128], BF16) for i in range(2)]
        OHJ = [A.sb(f"e2_OHJ{i}", [128, SBK, JH], BF16) for i in range(2)]
        OHJg = [A.sb(f"e2_OHJg{i}", [128, SBK, JH], BF16) for i in range(2)]
        CJ, NBUF = 4, 4
        NCH = 128 // CJ
        UTc = [A.sb(f"e2_UTc{i}", [128, 8, CJ, 128], BF16) for i in range(NBUF)]
        Vc = [A.sb(f"e2_Vc{i}", [128, CJ, D], BF16) for i in range(NBUF)]
        h2g = [A.sb(f"e2_h2g{i}", [128, 8, G], BF16) for i in range(2)]
        rtg = [A.sb(f"e2_rtg{i}", [128, 3, G], F32) for i in range(2)]
        x1g = A.sb("e2_x1g", [128, 2, D], F32)
        ga = [A.sb(f"e2_ga{i}", [128, G], BF16) for i in range(3)]
        Wj = [A.sb(f"e2_Wj{i}", [128, G], BF16) for i in range(3)]
        x2 = A.sb("e2_x2", [128, D], F32)
        sq = A.sb("e2_sq", [128, D], F32)
        rs = A.sb("e2_rs", [128, 4], F32)
        ot = [A.sb(f"e2_ot{i}", [128, D], F32) for i in range(2)]
        oacc = [[A.ps(f"e2_oacc{a}{b}", [128, 512], F32) for b in range(2)] for a in range(2)]
        pbank = [A.ps(f"e2_pb{i}", [128, 512], F32) for i in range(3)]
        gps = A.ps("e2_gps", [128, 512], F32)

        P.dma(iota[:], T["iota128"], writes=["iota"])
        iotab = A.sb("e2_iotab", [128, 128], BF16)
        CP(P, "dve", iotab[:], iota[:], ["iota"], ["iotab"])
        P.dma(gfin[:], T["g_final"].partition_broadcast(128), writes=["gfin"])

        def load_group(g):
            s = g % 2
            P.dma(h2g[s][:], T["h2T_s"][:, g * G:(g + 1) * G].rearrange("(k p) t -> p k t", p=128), writes=[("h2g", s)])
            P.dma(rtg[s][:], T["route_s"][:, :, g * G:(g + 1) * G].rearrange("x p t -> p x t"), writes=[("rtg", s)])

        def load_chunk(gc):
            b = gc % NBUF
            j0 = (gc % NCH) * CJ
            P.dma(UTc[b][:], T["UTb_s"][:, :, j0:j0 + CJ, :], writes=[("UTc", b)])
            P.dma(Vc[b][:], T["Vb_s"][:, j0:j0 + CJ, :], writes=[("Vc", b)])

        def oh_build(q, sb_):
            g_, p_ = divmod(q, 2)
            s_ = g_ % 2
            t0 = sb_ * SBK
            j0 = p_ * JH
            ob = sb_ % 2
            for t in range(SBK):
                TS(P, "dve", OHI[ob][:, t, :], iotab[:], rtg[s_][:, 1, t0 + t:t0 + t + 1], ALU.is_equal,
                   ["iotab", ("rtg", s_)], [("OHI", ob, t)])
                TS(P, "dve", OHJg[ob][:, t, :], iotab[:, j0:j0 + JH], rtg[s_][:, 2, t0 + t:t0 + t + 1], ALU.is_equal,
                   ["iotab", ("rtg", s_)], [("OHJg", ob, t)], s2=rtg[s_][:, 0, t0 + t:t0 + t + 1], op1=ALU.mult)

        def oh_mm(q, sb_):
            t0 = sb_ * SBK
            ob = sb_ % 2
            for t in range(SBK):
                MM(P, gps[:, (t % 8) * JH:(t % 8 + 1) * JH], OHI[ob][:, t, :], OHJg[ob][:, t, :], True, True,
                   [("OHI", ob, t), ("OHJg", ob, t)], ["gps"])
                if t % 8 == 7:
                    CP(P, "act", Gsq[q % 2][:, t0 + t - 7:t0 + t + 1, :].rearrange("p t j -> p (t j)"), gps[:],
                       ["gps"], [("Gs", q % 2)])

        def oh_subblock(q, sb_):
            oh_build(q, sb_)
            oh_mm(q, sb_)

        load_group(0)
        for gc in range(NBUF):
            load_chunk(gc)
        for sb_ in range(G // SBK):
            oh_subblock(0, sb_)
        NP = 2 * ngroups
        fin = 0
        for g in range(ngroups):
            s = g % 2
            if g + 1 < ngroups:
                load_group(g + 1)
            P.dma(x1g[:], T["x1_s"][g * G:(g + 1) * G, :].rearrange("(a p) c -> p a c", p=128), writes=["x1g"])

            def stage1(j):
                gc = g * NCH + j // CJ
                b = gc % NBUF
                jj = j % CJ
                r = j % 3
                q = 2 * g + j // JH
                for k in range(8):
                    MM(P, pbank[r][:, 0:G], UTc[b][:, k, jj, :], h2g[s][:, k, :], k == 0, k == 7,
                       [("UTc", b), ("h2g", s)], [("pbank", r)])
                ACT(P, ga[r][:], pbank[r][:, 0:G], AF.Gelu, [("pbank", r)], [("ga", r)])
                TT(P, "pool", Wj[r][:], ga[r][:], Gsq[q % 2][:, :, j % JH], ALU.mult, [("ga", r), ("Gs", q % 2)], [("Wj", r)])

            def stage2(j):
                b = (g * NCH + j // CJ) % NBUF
                jj = j % CJ
                r = j % 3
                for tl in range(2):
                    for half in range(2):
                        MM(P, oacc[tl][half][:], Wj[r][:, tl * 128:(tl + 1) * 128],
                           Vc[b][:, jj, half * 512:(half + 1) * 512], j == 0, j == 127,
                           [("Wj", r), ("Vc", b)], [("oacc", tl, half)])
                gc = g * NCH + j // CJ
                if j % CJ == CJ - 1 and gc + NBUF < ngroups * NCH:
                    load_chunk(gc + NBUF)

            LOOK = 2
            for x in range(128 + LOOK):
                if x < 128:
                    stage1(x)
                    qn = 2 * g + x // JH + 1
                    xl = x % JH
                    if qn < NP:
                        if xl % 4 == 0:
                            oh_build(qn, xl // 4)
                        if (xl % 4 == 1 and xl >= 5):
                            oh_mm(qn, (xl - 5) // 4)
                        if xl == JH - 1:
                            oh_mm(qn, 15)
                if x >= LOOK:
                    stage2(x - LOOK)
            for tl in range(2):
                f = fin % 2
                fin += 1
                for half in range(2):
                    TT(P, "dve", x2[:, half * 512:(half + 1) * 512], oacc[tl][half][:],
                       x1g[:, tl, half * 512:(half + 1) * 512], ALU.add, [("oacc", tl, half), "x1g"], ["x2"])
                TT(P, "pool", sq[:], x2[:], x2[:], ALU.mult, ["x2"], ["sq"])
                RSUM(P, "dve", rs[:, 0:1], sq[:], ["sq"], ["rs"])
                ACT(P, rs[:, 1:2], rs[:, 0:1], AF.Ln, ["rs"], ["rs"], bias=EPS, scale=1.0 / D)
                ACT(P, rs[:, 2:3], rs[:, 1:2], AF.Exp, ["rs"], ["rs"], scale=-0.5)
                STT(P, "dve", ot[f][:], x2[:], rs[:, 2:3], gfin[:], ALU.mult, ALU.mult, ["x2", "rs", "gfin"], [("ot", f)])
                r0 = g * G + tl * 128
                P.dma(T["out"][r0:r0 + 128, :], ot[f][:], reads=[("ot", f)], writes=[("out", r0)])
        P.emit()
```

```python
from contextlib import ExitStack
import math
import numpy as np
import ml_dtypes
import concourse.bass as bass
import concourse.mybir as mybir
from concourse.bass_utils import run_bass_kernel_spmd

F32 = mybir.dt.float32
BF16 = mybir.dt.bfloat16
U32 = mybir.dt.uint32
ALU = mybir.AluOpType
AF = mybir.ActivationFunctionType
AX = mybir.AxisListType

S = 4096
D = 1024
NT = S // 128
EPS = 1e-6
INW = 8208
COMPUTE = ("pe", "act", "dve", "pool")
NDMA_SEMS = 24


class SemPool:
    def __init__(self, nc, es):
        self.nc, self.es = nc, es
        self.dsem = [es.enter_context(nc.semaphore(f"dma{i}")) for i in range(NDMA_SEMS)]
        self.dval = [0] * NDMA_SEMS
        self.drr = 0
        self.n = 0

    def new(self, tag):
        self.n += 1
        return self.es.enter_context(self.nc.semaphore(f"c{self.n}_{tag}"))


SEM_ROLL = 30000


class Prog:
    def __init__(self, nc, es, tag):
        self.nc = nc
        self.tag = tag
        self.G = POOLS[id(nc)]
        self.q = {e: [] for e in COMPUTE + ("sp",)}
        self.cnt = {e: 0 for e in COMPUTE}
        self.epoch = {e: 0 for e in COMPUTE}
        self.semobj = {}
        for e in COMPUTE:
            self.semobj[("c", e, 0)] = self.G.new(f"{tag}_{e}")
        for i in range(NDMA_SEMS):
            self.semobj[("d", i)] = self.G.dsem[i]
        self.last_w = {}
        self.readers = {}
        self.known = {e: {} for e in COMPUTE + ("sp",)}
        self.skip_dist = None

    def _deps(self, eng, reads, writes, extra=()):
        deps = {}
        for r in reads:
            for k, v in self.last_w.get(r, {}).items():
                if deps.get(k, 0) < v:
                    deps[k] = v
        for w in writes:
            for k, v in self.last_w.get(w, {}).items():
                if deps.get(k, 0) < v:
                    deps[k] = v
            for k, v in self.readers.get(w, {}).items():
                if deps.get(k, 0) < v:
                    deps[k] = v
        for k, v in extra:
            if deps.get(k, 0) < v:
                deps[k] = v
        waits = []
        kn = self.known[eng]
        for k, v in deps.items():
            if eng == "pe" and k[0] == "c" and k[1] == "pe":
                continue
            if (self.skip_dist is not None and k[0] == "c" and k[1] == eng and k[2] == self.epoch[eng]
                    and self.cnt[eng] + 1 - v >= self.skip_dist):
                continue
            if kn.get(k, 0) >= v:
                continue
            kn[k] = v
            waits.append((k, v))
        return waits

    def _commit(self, tok, reads, writes):
        k, v = tok
        for r in reads:
            d = self.readers.setdefault(r, {})
            if d.get(k, 0) < v:
                d[k] = v
        for w in writes:
            self.last_w[w] = {k: v}
            self.readers[w] = {}

    def op(self, eng, fn, reads=(), writes=()):
        waits = self._deps(eng, reads, writes)
        if self.cnt[eng] >= SEM_ROLL:
            self.epoch[eng] += 1
            self.cnt[eng] = 0
            self.semobj[("c", eng, self.epoch[eng])] = self.G.new(f"{self.tag}_{eng}{self.epoch[eng]}")
        self.cnt[eng] += 1
        key = ("c", eng, self.epoch[eng])
        tok = (key, self.cnt[eng])
        self.q[eng].append((waits, fn, (key, 1)))
        self._commit(tok, reads, writes)

    def dma(self, out, in_, reads=(), writes=(), queue="sp", **kw):
        G = self.G
        i = G.drr
        G.drr = (G.drr + 1) % NDMA_SEMS
        extra = [(("d", i), G.dval[i])] if G.dval[i] else []
        waits = self._deps(queue, reads, writes, extra=extra)
        G.dval[i] += 16
        tok = (("d", i), G.dval[i])
        self.q[queue].append((waits, lambda e: e.dma_start(out=out, in_=in_, **kw), (("d", i), 16)))
        self._commit(tok, reads, writes)

    def merge(self, keys, newkey):
        d = {}
        for key in keys:
            for k, v in self.last_w.get(key, {}).items():
                if d.get(k, 0) < v:
                    d[k] = v
        self.last_w[newkey] = d
        self.readers.setdefault(newkey, {})

    def emit(self):
        nc = self.nc
        toks = []
        for e in COMPUTE:
            for ep in range(self.epoch[e] + 1):
                v = self.cnt[e] if ep == self.epoch[e] else SEM_ROLL
                if v:
                    toks.append((("c", e, ep), v))
        toks += [(("d", i), self.G.dval[i]) for i in range(NDMA_SEMS) if self.G.dval[i]]
        for eng in COMPUTE + ("sp",):
            kn = self.known[eng]
            waits = [(k, v) for k, v in toks if kn.get(k, 0) < v]
            if waits:
                self.q[eng].append((waits, None, None))
        engmap = {"pe": "tensor", "act": "scalar", "dve": "vector", "pool": "gpsimd", "sp": "sync"}
        with nc.Block() as block:
            for e, bname in engmap.items():
                lst = self.q[e]

                def body(eng, lst=lst):
                    for waits, fn, inc in lst:
                        for k, v in waits:
                            eng.wait_ge(self.semobj[k], v)
                        if fn is not None:
                            fn(eng).then_inc(self.semobj[inc[0]], inc[1])

                getattr(block, bname)(body)


def MM(P, out, lhsT, rhs, start, stop, reads, writes, **kw):
    P.op("pe", lambda e: e.matmul(out, lhsT=lhsT, rhs=rhs, start=start, stop=stop, **kw), reads, writes)


def TR(P, out, in_, ident, reads, writes):
    P.op("pe", lambda e: e.transpose(out=out, in_=in_, identity=ident), reads, writes)


def ACT(P, out, in_, func, reads, writes, bias=None, scale=None, eng="act"):
    kw = {}
    if bias is not None:
        kw["bias"] = bias
    if scale is not None:
        kw["scale"] = scale
    P.op("act", lambda e: e.activation(out=out, in_=in_, func=func, **kw), reads, writes)


def TT(P, eng, out, in0, in1, op, reads, writes):
    P.op(eng, lambda e: e.tensor_tensor(out=out, in0=in0, in1=in1, op=op), reads, writes)


def TS(P, eng, out, in0, s1, op0, reads, writes, s2=None, op1=None):
    if op1 is None:
        P.op(eng, lambda e: e.tensor_scalar(out=out, in0=in0, scalar1=s1, scalar2=None, op0=op0), reads, writes)
    else:
        P.op(eng, lambda e: e.tensor_scalar(out=out, in0=in0, scalar1=s1, scalar2=s2, op0=op0, op1=op1), reads, writes)


def STT(P, eng, out, in0, scalar, in1, op0, op1, reads, writes):
    P.op(eng, lambda e: e.scalar_tensor_tensor(out=out, in0=in0, scalar=scalar, in1=in1, op0=op0, op1=op1), reads, writes)


def CP(P, eng, out, in_, reads, writes):
    if eng == "act":
        P.op("act", lambda e: e.copy(out=out, in_=in_), reads, writes)
    else:
        P.op(eng, lambda e: e.tensor_copy(out=out, in_=in_), reads, writes)


def MEMSET(P, eng, ap, val, writes):
    P.op(eng, lambda e: e.memset(ap, val), (), writes)


def RSUM(P, eng, out, in_, reads, writes):
    P.op(eng, lambda e: e.reduce_sum(out=out, in_=in_, axis=AX.X), reads, writes)


class Alloc:
    def __init__(self, nc, es):
        self.nc, self.es = nc, es

    def sb(self, name, shape, dt):
        return self.es.enter_context(self.nc.sbuf_tensor(name, shape, dt))

    def ps(self, name, shape, dt):
        return self.es.enter_context(self.nc.psum_tensor(name, shape, dt))


def phase_a(nc, T):
    with ExitStack() as es:
        P = Prog(nc, es, "A")
        A = Alloc(nc, es)
        hT = A.sb("a_hT", [128, 8, S + 3], BF16)
        ident = A.sb("a_ident", [128, 128], BF16)
        gmt = A.sb("a_gmt", [128, 8], F32)
        ones_row = A.sb("a_ones", [1, 128], BF16)
        cbrow_f = A.sb("a_cbrowf", [1, 2048], F32)
        cbrow = A.sb("a_cbrow", [1, 2048], BF16)
        cbcol = A.sb("a_cbcol", [128, 16], F32)
        xt = [A.sb(f"a_xt{i}", [128, D], F32) for i in range(2)]
        sq = A.sb("a_sq", [128, D], F32)
        hb = [A.sb(f"a_hb{i}", [128, D], BF16) for i in range(2)]
        ss = [A.sb(f"a_ss{i}", [128, 4], F32) for i in range(2)]
        wf = [A.sb(f"a_wf{i}", [128, 8, 256], F32) for i in range(2)]
        wb = [A.sb(f"a_wb{i}", [128, 8, 256], BF16) for i in range(2)]
        pre = [A.sb(f"a_pre{i}", [128, S + 3], BF16) for i in range(2)]
        cacc = A.sb("a_cacc", [128, S], F32)
        cwcol = A.sb("a_cwcol", [128, 16, 4], F32)
        st_tmc = [A.sb(f"a_sttmc{i}", [128, NT, 128], BF16) for i in range(2)]
        st_fm = [A.sb(f"a_stfm{i}", [128, S], BF16) for i in range(2)]
        st_tm = [A.sb(f"a_sttm{i}", [128, 4, 256], BF16) for i in range(2)]
        st_dt = A.sb("a_stdt", [128, NT, 16], F32)
        pt = A.ps("a_pt", [128, 8, 128], BF16)
        pm = [A.ps(f"a_pm{i}", [128, 512], F32) for i in range(4)]

        P.dma(ident[:], T["ident"], writes=["ident"])
        P.dma(gmt[:], T["gm"], writes=["gmt"])
        P.dma(cbrow_f[:], T["conv_b_row"], writes=["cbrow_f"])
        P.dma(cbcol[:], T["conv_b_col"], writes=["cbcol"])
        MEMSET(P, "pool", hT[:, :, 0:3], 0.0, ["hT_pad"])
        P.dma(cwcol[:], T["conv_w_col"], writes=["cwcol"])
        for i in range(2):
            MEMSET(P, "pool", pre[i][:, 0:3], 0.0, [("prepad", i)])
        MEMSET(P, "pool", ones_row[:], 1.0, ["ones"])
        CP(P, "dve", cbrow[:], cbrow_f[:], ["cbrow_f"], ["cbrow"])

        for tt in range(NT):
            b = tt % 2
            P.dma(xt[b][:], T["x"][tt * 128:(tt + 1) * 128, :], writes=[("xt", b)])
            TT(P, "dve", sq[:], xt[b][:], xt[b][:], ALU.mult, [("xt", b)], ["sq"])
            RSUM(P, "dve", ss[b][:, 0:1], sq[:], ["sq"], [("ss", b)])
            ACT(P, ss[b][:, 1:2], ss[b][:, 0:1], AF.Ln, [("ss", b)], [("ss", b)], bias=EPS, scale=1.0 / D)
            ACT(P, ss[b][:, 2:3], ss[b][:, 1:2], AF.Exp, [("ss", b)], [("ss", b)], scale=-0.5)
            TS(P, "dve", hb[b][:], xt[b][:], ss[b][:, 2:3], ALU.mult, [("ss", b), ("xt", b)], [("hb", b)])
            for k in range(8):
                TR(P, pt[:, k, :], hb[b][:, k * 128:(k + 1) * 128], ident[:], [("hb", b), "ident"], ["pt"])
            CP(P, "act" if tt % 2 else "dve", hT[:, :, 3 + tt * 128: 3 + (tt + 1) * 128], pt[:], ["pt"], [("hT", tt)])
        P.merge([("hT", tt) for tt in range(NT)] + ["hT_pad"], "hT")

        gm_bc = gmt[:].unsqueeze(2).to_broadcast([128, 8, 256])
        segs = [("q", 0, 1024), ("k", 1024, 2048), ("v", 2048, 3072), ("z", 3072, 4096),
                ("xs", 4096, 5120), ("B", 5120, 5632), ("C", 5632, 6144), ("dt", 6144, 6160),
                ("g", 6160, 8208)]
        blocks = []
        for name, c0, c1 in segs:
            for c in range(c0, c1, 256):
                blocks.append((name, c, min(256, c1 - c)))
        state = {"pm": 0, "ev": 0, "fm": 0, "tm": 0}

        def next_pm():
            i = state["pm"]
            state["pm"] = (i + 1) % 4
            return i

        def load_w(bi):
            name, c0, ncol = blocks[bi]
            b = bi % 2
            P.dma(wf[b][:, :, 0:ncol], T["w_in"][:, :, c0:c0 + ncol], writes=[("wf", b)])

        load_w(0)
        for bi, (name, c0, ncol) in enumerate(blocks):
            b = bi % 2
            if bi + 1 < len(blocks):
                load_w(bi + 1)
            conv = False
            TT(P, "pool", wb[b][:, :, 0:ncol], wf[b][:, :, 0:ncol], gm_bc[:, :, 0:ncol], ALU.mult,
               [("wf", b), "gmt"], [("wb", b)])
            wkeys = [("wb", b)]

            def w_of(tap):
                return wb[b]

            taps = range(1)
            if name in ("xs", "B", "C"):
                for cc in range(ncol // 128):
                    j = (c0 - 4096) // 128 + cc
                    pr = pre[j % 2]
                    for tb in range(8):
                        pi = next_pm()
                        for k in range(8):
                            MM(P, pm[pi][:, :], wb[b][:, k, cc * 128:(cc + 1) * 128],
                               hT[:, k, 3 + tb * 512: 3 + (tb + 1) * 512], k == 0, k == 7, wkeys + ["hT"], [("pm", pi)])
                        CP(P, "dve" if state["ev"] % 2 else "act", pr[:, 3 + tb * 512: 3 + (tb + 1) * 512], pm[pi][:, :],
                           [("pm", pi)], [("pre", j % 2, tb)])
                        state["ev"] += 1
                    P.merge([("pre", j % 2, tb) for tb in range(8)] + [("prepad", j % 2)], ("preall", j % 2))
                    ce = "dve"
                    TS(P, ce, cacc[:], pr[:, 3:3 + S], cwcol[:, j, 3:4], ALU.mult, [("preall", j % 2), "cwcol"], ["cacc"])
                    for tap in (2, 1, 0):
                        STT(P, ce, cacc[:], pr[:, tap:tap + S], cwcol[:, j, tap:tap + 1], cacc[:], ALU.mult, ALU.add,
                            [("preall", j % 2), "cwcol", "cacc"], ["cacc"])
                    for tb in range(8):
                        P.readers.setdefault(("pre", j % 2, tb), {}).update(P.readers.get(("preall", j % 2), {}))
                    sfi = state["fm"] % 2
                    state["fm"] += 1
                    sf = st_fm[sfi]
                    ACT(P, sf[:, :], cacc[:], AF.Silu, ["cacc", "cbcol"], [("stfm", sfi)], bias=cbcol[:, j:j + 1])
                    if name in ("B", "C"):
                        r0 = (c0 - (5120 if name == "B" else 5632)) + cc * 128
                        dram = (T["BT_s"] if name == "B" else T["CT_s"])[r0:r0 + 128, :]
                        P.dma(dram, sf[:, :], reads=[("stfm", sfi)], writes=[("dram_fm", name, r0)])
                    if name in ("xs", "B"):
                        x = state["tm"] % 2
                        state["tm"] += 1
                        for tt in range(NT):
                            TR(P, pt[:, tt % 8, :], sf[:, tt * 128:(tt + 1) * 128], ident[:], [("stfm", sfi), "ident"], ["pt"])
                            if tt % 8 == 7:
                                CP(P, "dve" if (tt // 8) % 2 else "act", st_tmc[x][:, tt - 7:tt + 1, :], pt[:],
                                   ["pt"], [("sttmc", x)])
                        col0 = (c0 - (4096 if name == "xs" else 5120)) + cc * 128
                        dram = (T["xs_s"] if name == "xs" else T["B_s"])[:, col0:col0 + 128]
                        P.dma(dram.rearrange("(t p) c -> p t c", p=128), st_tmc[x][:], reads=[("sttmc", x)],
                              writes=[("dram_tmc", name, col0)])
                continue
            if name in ("q", "k", "g", "B", "C"):
                for cc in range(ncol // 128):
                    sfi = state["fm"] % 2
                    state["fm"] += 1
                    sf = st_fm[sfi]
                    for tb in range(8):
                        pi = next_pm()
                        n_mm = len(taps) * 8
                        i = 0
                        for tap in taps:
                            sh = tap if conv else 3
                            for k in range(8):
                                MM(P, pm[pi][:, :], w_of(tap)[:, k, cc * 128:(cc + 1) * 128],
                                   hT[:, k, tb * 512 + sh: tb * 512 + sh + 512], i == 0, i == n_mm - 1,
                                   wkeys + ["hT"], [("pm", pi)])
                                i += 1
                        dst = sf[:, tb * 512:(tb + 1) * 512]
                        if name == "q":
                            P.op("act", lambda e, o=dst, i_=pm[pi][:, :]: e.mul(out=o, in_=i_, mul=0.125),
                                 [("pm", pi)], [("stfm", sfi)])
                        elif name == "k":
                            CP(P, "dve" if state["ev"] % 2 else "act", dst, pm[pi][:, :], [("pm", pi)], [("stfm", sfi)])
                            state["ev"] += 1
                        elif name == "g":
                            ACT(P, dst, pm[pi][:, :], AF.Sigmoid, [("pm", pi)], [("stfm", sfi)])
                        else:
                            j = (c0 - 4096 + cc * 128) // 128
                            ACT(P, dst, pm[pi][:, :], AF.Silu, [("pm", pi), "cbcol"], [("stfm", sfi)],
                                bias=cbcol[:, j:j + 1])
                    if name == "q":
                        r0 = c0 + cc * 128
                        dram = T["qk_s"][r0:r0 + 128, :]
                    elif name == "k":
                        r0 = 1024 + (c0 - 1024) + cc * 128
                        dram = T["qk_s"][r0:r0 + 128, :]
                    elif name == "g":
                        r0 = c0 - 6160 + cc * 128
                        dram = T["g_s"][r0:r0 + 128, :]
                    elif name == "B":
                        r0 = c0 - 5120 + cc * 128
                        dram = T["BT_s"][r0:r0 + 128, :]
                    else:
                        r0 = c0 - 5632 + cc * 128
                        dram = T["CT_s"][r0:r0 + 128, :]
                    P.dma(dram, sf[:, :], reads=[("stfm", sfi)], writes=[("dram_fm", name, r0)])
            if name in ("v", "z", "xs", "B", "dt"):
                for tt in range(NT):
                    pi = next_pm()
                    n_mm = len(taps) * 8 + (1 if conv else 0)
                    i = 0
                    for tap in taps:
                        sh = tap if conv else 3
                        for k in range(8):
                            MM(P, pm[pi][:, 0:ncol], hT[:, k, tt * 128 + sh: tt * 128 + sh + 128],
                               w_of(tap)[:, k, 0:ncol], i == 0, i == n_mm - 1, wkeys + ["hT"], [("pm", pi)])
                            i += 1
                    if conv:
                        cx0 = c0 - 4096
                        MM(P, pm[pi][:, 0:ncol], ones_row[0:1, :], cbrow[0:1, cx0:cx0 + ncol], False, True,
                           ["ones", "cbrow"], [("pm", pi)])
                    if name == "dt":
                        CP(P, "dve", st_dt[:, tt, :], pm[pi][:, 0:16], [("pm", pi)], ["stdt"])
                        continue
                    q4 = tt % 4
                    si = (state["tm"] // 4) % 2
                    state["tm"] += 1
                    dst = st_tm[si][:, q4, :]
                    if conv:
                        ACT(P, dst, pm[pi][:, 0:ncol], AF.Silu, [("pm", pi)], [("sttm", si)])
                    else:
                        CP(P, "dve" if state["ev"] % 2 else "act", dst, pm[pi][:, 0:ncol], [("pm", pi)], [("sttm", si)])
                        state["ev"] += 1
                    if q4 == 3:
                        t0 = (tt - 3) * 128
                        if name == "v":
                            dram = T["v_s"][t0:t0 + 512, c0 - 2048:c0 - 2048 + 256]
                        elif name == "z":
                            dram = T["z_s"][t0:t0 + 512, c0 - 3072:c0 - 3072 + 256]
                        elif name == "xs":
                            dram = T["xs_s"][t0:t0 + 512, c0 - 4096:c0 - 4096 + 256]
                        else:
                            dram = T["B_s"][t0:t0 + 512, c0 - 5120:c0 - 5120 + 256]
                        P.dma(dram.rearrange("(a p) c -> p a c", p=128), st_tm[si][:], reads=[("sttm", si)],
                              writes=[("dram_tm", name, c0, tt)])
                if name == "dt":
                    P.dma(T["dt_s"].rearrange("(a p) c -> p a c", p=128), st_dt[:], reads=["stdt"], writes=["dram_dt"])
        P.emit()


POOLS = {}
_SEM_ES = []


def declare_tensors(nc, debug):
    T = {}

    def inp(name, shape, dt):
        T[name] = nc.dram_tensor(name, shape, dt, kind="ExternalInput").ap()

    def scr(name, shape, dt):
        kind = "ExternalOutput" if debug else "Internal"
        T[name] = nc.dram_tensor(name, shape, dt, kind=kind).ap()

    inp("x", [S, D], F32)
    inp("w_in", [128, 8, INW], F32)
    inp("gm", [128, 8], F32)
    inp("conv_w_col", [128, 16, 4], F32)
    inp("conv_b_row", [1, 2048], F32)
    inp("conv_b_col", [128, 16], F32)
    inp("ident", [128, 128], BF16)
    scr("qk_s", [2048, S], BF16)
    scr("v_s", [S, 1024], BF16)
    scr("z_s", [S, 1024], BF16)
    scr("xs_s", [S, 1024], BF16)
    scr("B_s", [S, 512], BF16)
    scr("BT_s", [512, S], BF16)
    scr("CT_s", [512, S], BF16)
    scr("dt_s", [S, 16], F32)
    scr("g_s", [2048, S], BF16)
    inp("qaug", [8, 2, S], BF16)
    inp("kbias", [128, 8, 36], F32)
    inp("dbias", [8, 128, 4, 512], F32)
    for nm in ("lam_q1", "lam_k1", "lam_q2", "lam_k2"):
        inp(nm, [1, 64], F32)
    inp("g_subln", [1, 128], F32)
    scr("attT_s", [1024, S], BF16)
    inp("ssd_cst", [128, 5, 128], F32)
    for nm in ("dt_bias", "a_log", "d_skip"):
        inp(nm, [1, 16], F32)
    inp("g_ssd", [1, D], F32)
    scr("ybT_s", [1024, S], BF16)
    for nm in ("w_a", "w_b", "w_o"):
        inp(nm, [128, 8, D], F32)
    inp("g_ffn", [1, D], F32)
    scr("x1_s", [S, D], F32)
    scr("h2T_s", [1024, S], BF16)
    inp("UT_h", [128, 8, 128, 128], F32)
    inp("V_h", [128, 128, D], F32)
    inp("wq_h", [128, 8, 2048], F32)
    inp("skT_h", [128, 16, 128], F32)
    inp("iota128", [128, 128], F32)
    inp("thr15", [128, 15], F32)
    inp("g_final", [1, D], F32)
    scr("UTb_s", [128, 8, 128, 128], BF16)
    scr("Vb_s", [128, 128, D], BF16)
    scr("route_s", [3, 128, S], F32)
    T["out"] = nc.dram_tensor("out", [S, D], F32, kind="ExternalOutput").ap()
    return T


def build_program(debug=False, phases="ABCD012", **kw):
    nc = bass.Bass("TRN2", target_bir_lowering=False)
    T = declare_tensors(nc, debug)
    _SEM_ES.append(ExitStack())
    POOLS[id(nc)] = SemPool(nc, _SEM_ES[-1])
    if "A" in phases:
        phase_a(nc, T)
    if "B" in phases:
        phase_b(nc, T, **kw.get("b", {}))
    if "C" in phases:
        phase_c(nc, T, **kw.get("c", {}))
    if "D" in phases:
        phase_d(nc, T)
    if "1" in phases:
        phase_e1(nc, T, **kw.get("e1", {}))
    if "2" in phases:
        phase_e2(nc, T, **kw.get("e2", {}))
    return nc


def host_inputs(inp, b):
    f = np.float32
    d = {}
    d["x"] = np.ascontiguousarray(inp["x"][b], dtype=f)
    d["w_in"] = np.ascontiguousarray(inp["w_in"][0].reshape(8, 128, INW).transpose(1, 0, 2), dtype=f)
    d["gm"] = np.ascontiguousarray(inp["g_mix"][0].reshape(8, 128).T, dtype=f)
    d["conv_w_col"] = np.ascontiguousarray(inp["conv_w"][0].reshape(4, 16, 128).transpose(2, 1, 0), dtype=f)
    d["conv_b_row"] = np.ascontiguousarray(inp["conv_b"][0].reshape(1, 2048), dtype=f)
    d["conv_b_col"] = np.ascontiguousarray(inp["conv_b"][0].reshape(16, 128).T, dtype=f)
    d["ident"] = np.eye(128).astype(ml_dtypes.bfloat16)
    qaug, kbias, dbias = attn_consts()
    d["qaug"], d["kbias"], d["dbias"] = qaug, kbias, dbias
    for nm in ("lam_q1", "lam_k1", "lam_q2", "lam_k2"):
        d[nm] = np.ascontiguousarray(inp[nm][0].reshape(1, 64), dtype=f)
    d["g_subln"] = np.ascontiguousarray(inp["g_subln"][0].reshape(1, 128), dtype=f)
    d["ssd_cst"] = ssd_consts()
    for nm in ("dt_bias", "a_log", "d_skip"):
        d[nm] = np.ascontiguousarray(inp[nm][0].reshape(1, 16), dtype=f)
    d["g_ssd"] = np.ascontiguousarray(inp["g_ssd"][0].reshape(1, D), dtype=f)
    for nm, src in (("w_a", "w_branch_a"), ("w_b", "w_branch_b"), ("w_o", "w_out")):
        d[nm] = np.ascontiguousarray(inp[src][0].reshape(8, 128, D).transpose(1, 0, 2), dtype=f)
    d["g_ffn"] = np.ascontiguousarray(inp["g_ffn"][0].reshape(1, D), dtype=f)
    d.update(shared_peer_inputs(inp))
    return d


_SHARED = {}


def shared_peer_inputs(inp):
    key = id(inp["expert_u"])
    if key in _SHARED:
        return _SHARED[key]
    f = np.float32
    d = {}
    U = np.asarray(inp["expert_u"][0], dtype=f)
    d["UT_h"] = np.ascontiguousarray(U.reshape(128, 128, 8, 128).transpose(3, 2, 1, 0))
    d["V_h"] = np.ascontiguousarray(np.asarray(inp["expert_v"][0], dtype=f).reshape(128, 128, D))
    d["wq_h"] = np.ascontiguousarray(inp["w_query"][0].reshape(8, 128, 2048).transpose(1, 0, 2), dtype=f)
    sk = np.asarray(inp["sub_keys"][0], dtype=f)
    d["skT_h"] = np.ascontiguousarray(sk.reshape(16, 128, 128).transpose(2, 0, 1))
    d["iota128"] = np.ascontiguousarray(np.broadcast_to(np.arange(128, dtype=f), (128, 128)))
    d["thr15"] = np.ascontiguousarray(np.broadcast_to(16.0 * np.arange(1, 16, dtype=f), (128, 15)))
    d["g_final"] = np.ascontiguousarray(inp["g_final"].reshape(1, D), dtype=f)
    _SHARED.clear()
    _SHARED[key] = d
    return d


def kernel(**inputs):
    nc = build_program()
    in_maps = [host_inputs(inputs, b) for b in range(8)]
    res = run_bass_kernel_spmd(nc, in_maps, core_ids=list(range(8)))
    return np.stack([r["out"] for r in res.results], axis=0)


LAM0 = 0.8 - 0.6 * math.exp(-0.3 * 0)
SLOPES = [2.0 ** (-(i + 1)) for i in range(8)]
BAND_CUT = 64.0


def phase_b(nc, T, heads=range(8)):
    with ExitStack() as es:
        P = Prog(nc, es, "B")
        A = Alloc(nc, es)
        ident = A.sb("b_ident", [128, 128], BF16)
        qa = [[A.sb(f"b_qa{s}{m}", [66, S], BF16) for m in range(2)] for s in range(2)]
        ka = [[A.sb(f"b_ka{s}{m}", [66, S], BF16) for m in range(2)] for s in range(2)]
        va = [A.sb(f"b_va{s}", [128, NT, 129], BF16) for s in range(2)]
        dbias = [A.sb(f"b_db{s}", [128, 4, 512], F32) for s in range(2)]
        kbias = A.sb("b_kbias", [128, 8, 36], F32)
        lamv = A.sb("b_lamv", [128, 4, 64], F32)
        lamt = A.sb("b_lamt", [128, 8], F32)
        gs = A.sb("b_gs", [128, 128], F32)
        pT = [A.sb(f"b_pT{r}", [128, 512], BF16) for r in range(4)]
        sbias = [A.sb(f"b_sb{r}", [128, 512], F32) for r in range(2)]
        rec = [A.sb(f"b_rec{r}", [128, 8], F32) for r in range(2)]
        o1 = [A.sb(f"b_o1{r}", [128, 128], F32) for r in range(2)]
        oo = [A.sb(f"b_oo{r}", [128, 128], F32) for r in range(2)]
        osq = A.sb("b_osq", [128, 128], F32)
        on = [A.sb(f"b_on{r}", [128, 128], BF16) for r in range(2)]
        stage = [A.sb(f"b_st{r}", [128, 512], BF16) for r in range(2)]
        psS = [A.ps(f"b_ps{i}", [128, 512], F32) for i in range(4)]
        accb = [A.ps(f"b_acc{i}", [128, 512], F32) for i in range(3)]
        ptr = A.ps("b_ptr", [128, 4, 128], BF16)

        def acc(m, i):
            s = m * 4 + i
            return accb[s // 3][:, (s % 3) * 129:(s % 3) * 129 + 129]

        P.dma(ident[:], T["ident"], writes=["ident"])
        P.dma(kbias[:], T["kbias"], writes=["kbias"])
        for j, nm in enumerate(["lam_q1", "lam_k1", "lam_q2", "lam_k2"]):
            P.dma(lamv[:, j, :], T[nm].partition_broadcast(128), writes=[("lamv", j)])
        P.dma(gs[:], T["g_subln"].partition_broadcast(128), writes=["gs"])
        TT(P, "dve", lamv[:, 0, :], lamv[:, 0, :], lamv[:, 1, :], ALU.mult, [("lamv", 0), ("lamv", 1)], [("lamv", 0)])
        TT(P, "dve", lamv[:, 2, :], lamv[:, 2, :], lamv[:, 3, :], ALU.mult, [("lamv", 2), ("lamv", 3)], [("lamv", 2)])
        RSUM(P, "dve", lamt[:, 0:1], lamv[:, 0, :], [("lamv", 0)], ["lamt"])
        RSUM(P, "dve", lamt[:, 1:2], lamv[:, 2, :], [("lamv", 2)], ["lamt"])
        ACT(P, lamt[:, 2:4], lamt[:, 0:2], AF.Exp, ["lamt"], ["lamt"])
        TT(P, "dve", lamt[:, 4:5], lamt[:, 3:4], lamt[:, 2:3], ALU.subtract, ["lamt"], ["lamt"])
        TS(P, "dve", lamt[:, 4:5], lamt[:, 4:5], -LAM0, ALU.add, ["lamt"], ["lamt"])
        TS(P, "dve", gs[:], gs[:], 1.0 - LAM0, ALU.mult, ["gs"], ["gs"])
        for s in range(2):
            for m in range(2):
                MEMSET(P, "pool", ka[s][m][64:66, :], 1.0, [("ka1", s, m)])
            MEMSET(P, "pool", va[s][:, :, 128:129], 1.0, [("va1", s)])

        heads = list(heads)

        def load_head(idx):
            h = heads[idx]
            s = idx % 2
            for m in range(2):
                r0 = (h * 2 + m) * 64
                P.dma(qa[s][m][0:64, :], T["qk_s"][r0:r0 + 64, :], writes=[("qa", s, m)])
                P.dma(qa[s][m][64:66, :], T["qaug"][h], writes=[("qa", s, m)])
                P.dma(ka[s][m][0:64, :], T["qk_s"][1024 + r0:1024 + r0 + 64, :], writes=[("ka", s, m)])
            P.dma(va[s][:, :, 0:128], T["v_s"][:, h * 128:(h + 1) * 128].rearrange("(t p) c -> p t c", p=128),
                  writes=[("va", s)])
            P.dma(dbias[s][:], T["dbias"][h], writes=[("db", s)])

        load_head(0)
        st = {"ps": 0, "pt": 0, "fin": 0, "stg": 0}
        for idx, h in enumerate(heads):
            s = idx % 2
            if idx + 1 < len(heads):
                load_head(idx + 1)
            slope = SLOPES[h]
            for qb in range(8):
                for b3 in range(3):
                    MEMSET(P, "dve", accb[b3][:, 0:387], 0.0, [("acc", b3)])
                units = []
                for kt in range(4 * qb + 4):
                    j = kt - 4 * qb
                    if j < 0:
                        mind = qb * 512 - (kt * 128 + 127)
                        if slope * mind > BAND_CUT:
                            continue
                    for m in range(2):
                        units.append((kt, j, m))

                def stage1(u, kt, j, m):
                    c0 = 128 * j if j > 0 else 0
                    dd = (4 * qb - kt) + 3
                    pi = u % 4
                    MM(P, psS[pi][:, c0:512], ka[s][m][0:66, kt * 128:(kt + 1) * 128],
                       qa[s][m][0:66, qb * 512 + c0:(qb + 1) * 512], True, True,
                       [("ka", s, m), ("ka1", s, m), ("qa", s, m)], [("ps", pi)])
                    if j >= 0:
                        TT(P, "dve", sbias[m][:, c0:512], psS[pi][:, c0:512], dbias[s][:, j, c0:512], ALU.add,
                           [("ps", pi), ("db", s)], [("sbias", m)])
                        ACT(P, pT[pi][:, c0:512], sbias[m][:, c0:512], AF.Exp, [("sbias", m), "kbias"],
                            [("pT", pi)], bias=kbias[:, h, dd:dd + 1])
                    else:
                        ACT(P, pT[pi][:, :], psS[pi][:, :], AF.Exp, [("ps", pi), "kbias"], [("pT", pi)],
                            bias=kbias[:, h, dd:dd + 1])

                def stage2(u, kt, j, m):
                    pi = u % 4
                    for i in range(max(j, 0), 4):
                        sl = m * 4 + i
                        MM(P, acc(m, i), pT[pi][:, i * 128:(i + 1) * 128], va[s][:, kt, :], False, False,
                           [("pT", pi), ("va", s), ("va1", s)], [("acc", sl // 3)], skip_group_check=True)

                LOOK = 2
                ub = st["ps"]
                for x in range(len(units) + LOOK):
                    if x < len(units):
                        stage1(ub + x, *units[x])
                    if x >= LOOK:
                        stage2(ub + x - LOOK, *units[x - LOOK])
                st["ps"] = ub + len(units)
                sg = st["stg"] % 2
                st["stg"] += 1
                for i in range(4):
                    f = st["fin"] % 2
                    st["fin"] += 1
                    a0, a1 = acc(0, i), acc(1, i)
                    k0, k1 = ("acc", (0 * 4 + i) // 3), ("acc", (4 + i) // 3)
                    P.op("dve", lambda e, o=rec[f][:, 0:1], i_=a0[:, 128:129]: e.reciprocal(out=o, in_=i_), [k0], [("rec", f)])
                    P.op("dve", lambda e, o=rec[f][:, 1:2], i_=a1[:, 128:129]: e.reciprocal(out=o, in_=i_), [k1], [("rec", f)])
                    TS(P, "dve", rec[f][:, 2:3], rec[f][:, 1:2], lamt[:, 4:5], ALU.mult, [("rec", f), "lamt"], [("rec", f)])
                    TS(P, "dve", o1[f][:], a0[:, 0:128], rec[f][:, 0:1], ALU.mult, [k0, ("rec", f)], [("o1", f)])
                    STT(P, "dve", oo[f][:], a1[:, 0:128], rec[f][:, 2:3], o1[f][:], ALU.mult, ALU.add,
                        [k1, ("rec", f), ("o1", f)], [("oo", f)])
                    TT(P, "dve", osq[:], oo[f][:], oo[f][:], ALU.mult, [("oo", f)], ["osq"])
                    RSUM(P, "dve", rec[f][:, 3:4], osq[:], ["osq"], [("rec", f)])
                    ACT(P, rec[f][:, 4:5], rec[f][:, 3:4], AF.Ln, [("rec", f)], [("rec", f)], bias=EPS, scale=1.0 / 128)
                    ACT(P, rec[f][:, 5:6], rec[f][:, 4:5], AF.Exp, [("rec", f)], [("rec", f)], scale=-0.5)
                    STT(P, "dve", on[f][:], oo[f][:], rec[f][:, 5:6], gs[:], ALU.mult, ALU.mult,
                        [("oo", f), ("rec", f), "gs"], [("on", f)])
                    TR(P, ptr[:, i, :], on[f][:], ident[:], [("on", f), "ident"], [("ptr", i)])
                    CP(P, "act", stage[sg][:, i * 128:(i + 1) * 128], ptr[:, i, :], [("ptr", i)], [("stage", sg)])
                P.dma(T["attT_s"][h * 128:(h + 1) * 128, qb * 512:(qb + 1) * 512], stage[sg][:],
                      reads=[("stage", sg)], writes=[("attT", h, qb)])
        P.emit()


def attn_consts():
    bf = ml_dtypes.bfloat16
    pos = np.arange(S)
    qrel = pos % 512
    qaug = np.zeros((8, 2, S), np.float32)
    kbias = np.zeros((128, 8, 36), np.float32)
    dbias = np.zeros((8, 128, 4, 512), np.float32)
    ki = np.arange(128)
    qi = np.arange(512)
    for h in range(8):
        sl = SLOPES[h]
        qaug[h, 0] = -sl * (qrel % 256)
        qaug[h, 1] = -sl * 256.0 * (qrel // 256)
        for dd in range(36):
            kbias[:, h, dd] = sl * (ki - 128.0 * (dd - 3))
        for j in range(4):
            k = (128 * j + ki)[:, None]
            q = qi[None, :]
            masked = (k // 64) > (q // 64)
            fut = (k > q) & ~masked
            dbias[h, :, j, :] = np.where(masked, -30000.0, np.where(fut, -2.0 * sl * (k - q), 0.0))
    return qaug.astype(bf), kbias, dbias


def phase_c(nc, T, ntiles=NT):
    with ExitStack() as es:
        P = Prog(nc, es, "C")
        A = Alloc(nc, es)
        identb = A.sb("c_identb", [128, 128], BF16)
        cst = A.sb("c_cst", [128, 5, 128], F32)
        BT = A.sb("c_BT", [128, 4, S], BF16)
        CT = A.sb("c_CT", [128, 4, S], BF16)
        gssd = A.sb("c_gssd", [128, D], F32)
        pv = A.sb("c_pv", [128, 3, 16], F32)
        dtr = [A.sb(f"c_dtr{i}", [128, 16], F32) for i in range(2)]
        xs = [A.sb(f"c_xs{i}", [128, 16, 64], BF16) for i in range(2)]
        zt = [A.sb(f"c_zt{i}", [128, D], BF16) for i in range(2)]
        Bt = [A.sb(f"c_Bt{i}", [128, 512], BF16) for i in range(2)]
        sc_ = [A.sb(f"c_sc{i}", [128, 12, 16], F32) for i in range(2)]
        sm_ = [A.sb(f"c_sm{i}", [128, 32], F32) for i in range(2)]
        ead_ = [A.sb(f"c_ead{i}", [128, 32], F32) for i in range(2)]
        atri_ = [A.sb(f"c_atri{i}", [128, 16, 128], F32) for i in range(2)]
        LT_ = [A.sb(f"c_LT{i}", [128, 16, 128], BF16) for i in range(2)]
        cbL_ = [A.sb(f"c_cbL{i}", [128, 16, 128], BF16) for i in range(2)]
        Xdt_ = [A.sb(f"c_Xdt{i}", [128, 16, 64], BF16) for i in range(2)]
        Xdd_ = [A.sb(f"c_Xdd{i}", [128, 16, 64], BF16) for i in range(2)]
        t1_ = [A.sb(f"c_t1{i}", [128, 16, 64], F32) for i in range(2)]
        y1_ = [A.sb(f"c_y1{i}", [128, 16, 64], F32) for i in range(2)]
        t2_ = [A.sb(f"c_t2{i}", [128, 16, 64], F32) for i in range(2)]
        h32 = A.sb("c_h32", [128, 16, 64], F32)
        hbf = A.sb("c_hbf", [128, D], BF16)
        sz_ = [A.sb(f"c_sz{i}", [128, D], F32) for i in range(2)]
        yz_ = [A.sb(f"c_yz{i}", [128, D], F32) for i in range(2)]
        sq_ = [A.sb(f"c_sq{i}", [128, D], F32) for i in range(2)]
        rs_ = [A.sb(f"c_rs{i}", [128, 4], F32) for i in range(2)]
        yn_ = [A.sb(f"c_yn{i}", [128, D], BF16) for i in range(2)]
        stage = [A.sb(f"c_st{i}", [128, 8, 512], BF16) for i in range(2)]
        big = [A.ps(f"c_big{i}", [128, 1024], F32) for i in range(2)]
        cb = A.ps("c_cb", [128, 4, 128], F32)
        small = A.ps("c_small", [128, 32], F32)
        ptr = A.ps("c_ptr", [128, 8, 128], BF16)

        tri, ones, negtri, identF, maskT = (cst[:, i, :] for i in range(5))
        P.dma(identb[:], T["ident"], writes=["identb"])
        P.dma(cst[:], T["ssd_cst"], writes=["cst"])
        P.dma(BT[:], T["BT_s"].rearrange("(g n) t -> n g t", n=128), writes=["BT"])
        P.dma(CT[:], T["CT_s"].rearrange("(g n) t -> n g t", n=128), writes=["CT"])
        P.dma(gssd[:], T["g_ssd"].partition_broadcast(128), writes=["gssd"])
        for j, nm in enumerate(["dt_bias", "a_log", "d_skip"]):
            P.dma(pv[:, j, :], T[nm].partition_broadcast(128), writes=[("pv", j)])
        ACT(P, pv[:, 1, :], pv[:, 1, :], AF.Exp, [("pv", 1)], [("pv", 1)])
        TS(P, "dve", pv[:, 1, :], pv[:, 1, :], -1.0, ALU.mult, [("pv", 1)], [("pv", 1)])
        MEMSET(P, "pool", h32[:], 0.0, ["h32"])
        MEMSET(P, "pool", hbf[:], 0.0, ["hbf"])
        dtb_bc, negA, dskip = pv[:, 0, :], pv[:, 1, :], pv[:, 2, :]

        def load(tt):
            b = tt % 2
            r = slice(tt * 128, (tt + 1) * 128)
            P.dma(dtr[b][:], T["dt_s"][r, :], writes=[("dtr", b)])
            P.dma(xs[b][:].rearrange("p h d -> p (h d)"), T["xs_s"][r, :], writes=[("xs", b)])
            P.dma(zt[b][:], T["z_s"][r, :], writes=[("zt", b)])
            P.dma(Bt[b][:], T["B_s"][r, :], writes=[("Bt", b)])

        WK = set(["x1", "nx", "mn", "ee", "ll", "rr", "dt", "a", "dd", "dS", "w2", "sm", "ead", "atri", "Xdt", "Xdd",
                  "t1", "y1", "t2", "sz", "yz", "sq", "rs", "yn"])
        cur = {"b": 0}
        _op = P.op

        def op2(eng, fn, reads=(), writes=()):
            bb = cur["b"]

            def kx(x):
                if isinstance(x, str) and x in WK:
                    return (x, bb)
                if isinstance(x, tuple) and x[0] in ("LT", "cbL"):
                    return x + (bb,)
                return x
            _op(eng, fn, [kx(x) for x in reads], [kx(x) for x in writes])

        P.op = op2
        bigc = {"i": 0}
        ctail = []

        def nbig():
            i = bigc["i"] % 2
            bigc["i"] += 1
            return i

        load(0)
        for tt in range(ntiles):
            b = tt % 2
            if tt + 1 < ntiles:
                load(tt + 1)
            tk = slice(tt * 128, (tt + 1) * 128)
            sc = sc_[b]; sm = sm_[b]; ead = ead_[b]; atri = atri_[b]; LT = LT_[b]; cbL = cbL_[b]; Xdt = Xdt_[b]; Xdd = Xdd_[b]; t1 = t1_[b]; y1 = y1_[b]; t2 = t2_[b]; sz = sz_[b]; yz = yz_[b]; sq = sq_[b]; rs = rs_[b]; yn = yn_[b]
            cur["b"] = b
            x1, nx, mn, ee, ll, rr, dt, a_, dd, dS, w2 = (sc[:, i, :] for i in range(11))
            TT(P, "dve", x1, dtr[b][:], dtb_bc, ALU.add, [("dtr", b), ("pv", 0)], ["x1"])
            TS(P, "dve", nx, x1, -1.0, ALU.mult, ["x1"], ["nx"])
            TT(P, "dve", mn, x1, nx, ALU.min, ["x1", "nx"], ["mn"])
            ACT(P, ee, mn, AF.Exp, ["mn"], ["ee"])
            ACT(P, ll, ee, AF.Ln, ["ee"], ["ll"], bias=1.0)
            TS(P, "dve", rr, x1, 0.0, ALU.max, ["x1"], ["rr"])
            TT(P, "dve", dt, rr, ll, ALU.add, ["rr", "ll"], ["dt"])
            TT(P, "dve", a_, dt, negA, ALU.mult, ["dt", ("pv", 1)], ["a"])
            MM(P, small[:, 0:16], tri, a_, True, True, ["cst", "a"], ["small"])
            MM(P, small[:, 16:32], ones, a_, True, True, ["cst", "a"], ["small"])
            CP(P, "dve", sm[:], small[:], ["small"], ["sm"])
            TT(P, "dve", dd, sm[:, 16:32], sm[:, 0:16], ALU.subtract, ["sm"], ["dd"])
            ACT(P, ead[:], sm[:], AF.Exp, ["sm"], ["ead"])
            ACT(P, dS, dd, AF.Exp, ["dd"], ["dS"])
            TT(P, "dve", w2, dt, dS, ALU.mult, ["dt", "dS"], ["w2"])
            ea, dec = ead[:, 0:16], ead[:, 16:32]
            TT(P, "dve", atri[:], a_.unsqueeze(2).to_broadcast([128, 16, 128]),
               tri.unsqueeze(1).to_broadcast([128, 16, 128]), ALU.mult, ["a", "cst"], ["atri"])
            for half in range(2):
                bi = nbig()
                for j in range(2):
                    h0 = half * 8 + j * 4
                    o = big[bi][:, j * 512:(j + 1) * 512]
                    MM(P, o, ones, atri[:, h0:h0 + 4, :], True, False, ["cst", "atri"], [("big", bi)])
                    MM(P, o, negtri, a_[:, h0:h0 + 4].unsqueeze(2).to_broadcast([128, 4, 128]), False, False,
                       ["cst", "a"], [("big", bi)])
                    MM(P, o, identF, maskT.unsqueeze(1).to_broadcast([128, 4, 128]), False, True,
                       ["cst"], [("big", bi)])
                ACT(P, LT[:, half * 8:(half + 1) * 8, :].rearrange("p h l -> p (h l)"), big[bi][:], AF.Exp,
                    [("big", bi)], [("LT", half)])
            for g in range(4):
                MM(P, cb[:, g, :], BT[:, g, tk], CT[:, g, tk], True, True, ["BT", "CT"], ["cb"])
            for g in range(4):
                TT(P, "dve", cbL[:, 4 * g:4 * g + 4, :], cb[:, g, :].unsqueeze(1).to_broadcast([128, 4, 128]),
                   LT[:, 4 * g:4 * g + 4, :], ALU.mult, ["cb", ("LT", g // 2)], [("cbL", g)])
            TT(P, "pool", Xdt[:], xs[b][:], dt.unsqueeze(2).to_broadcast([128, 16, 64]), ALU.mult,
               [("xs", b), "dt"], ["Xdt"])
            TT(P, "pool", Xdd[:], xs[b][:], w2.unsqueeze(2).to_broadcast([128, 16, 64]), ALU.mult,
               [("xs", b), "w2"], ["Xdd"])
            while ctail:
                ctail.pop(0)()
            bo = nbig()
            for g in range(4):
                MM(P, big[bo][:, g * 256:(g + 1) * 256], CT[:, g, tk], hbf[:, g * 256:(g + 1) * 256], True, True,
                   ["CT", "hbf"], [("big", bo)])
            TT(P, "dve", t1[:], big[bo][:].rearrange("p (h d) -> p h d", d=64),
               ea.unsqueeze(2).to_broadcast([128, 16, 64]), ALU.mult, [("big", bo), "ead"], ["t1"])
            by = nbig()
            for h in range(16):
                MM(P, big[by][:, h * 64:(h + 1) * 64], cbL[:, h, :], Xdt[:, h, :], True, True,
                   [("cbL", h // 4), "Xdt"], [("big", by)])
            TT(P, "dve", y1[:], big[by][:].rearrange("p (h d) -> p h d", d=64), t1[:], ALU.add,
               [("big", by), "t1"], ["y1"])
            bs = nbig()
            for g in range(4):
                MM(P, big[bs][:, g * 256:(g + 1) * 256], Bt[b][:, g * 128:(g + 1) * 128],
                   Xdd[:, 4 * g:4 * g + 4, :], True, True, [("Bt", b), "Xdd"], [("big", bs)])
            TT(P, "pool", h32[:], h32[:], dec.unsqueeze(2).to_broadcast([128, 16, 64]), ALU.mult,
               ["h32", "ead"], ["h32"])
            TT(P, "dve", h32[:], big[bs][:].rearrange("p (h d) -> p h d", d=64), h32[:], ALU.add,
               [("big", bs), "h32"], ["h32"])
            CP(P, "pool", hbf[:], h32[:].rearrange("p h d -> p (h d)"), ["h32"], ["hbf"])
            TT(P, "pool", t2[:], xs[b][:], dskip.unsqueeze(2).to_broadcast([128, 16, 64]), ALU.mult,
               [("xs", b), ("pv", 2)], ["t2"])
            TT(P, "dve", y1[:], y1[:], t2[:], ALU.add, ["y1", "t2"], ["y1"])
            ACT(P, sz[:], zt[b][:], AF.Silu, [("zt", b)], ["sz"])
            TT(P, "dve", yz[:], y1[:].rearrange("p h d -> p (h d)"), sz[:], ALU.mult, ["y1", "sz"], ["yz"])
            TT(P, "pool", sq[:], yz[:], yz[:], ALU.mult, ["yz"], ["sq"])
            RSUM(P, "dve", rs[:, 0:1], sq[:], ["sq"], ["rs"])
            ACT(P, rs[:, 1:2], rs[:, 0:1], AF.Ln, ["rs"], ["rs"], bias=EPS, scale=1.0 / D)
            ACT(P, rs[:, 2:3], rs[:, 1:2], AF.Exp, ["rs"], ["rs"], scale=-0.5)
            STT(P, "dve", yn[:], yz[:], rs[:, 2:3], gssd[:], ALU.mult, ALU.mult, ["yz", "rs", "gssd"], ["yn"])
            def tail(tt=tt, b=b, yn=yn):
                save = cur["b"]
                cur["b"] = b
                for k in range(8):
                    TR(P, ptr[:, k, :], yn[:, k * 128:(k + 1) * 128], identb[:], ["yn", "identb"], ["ptr"])
                sg = (tt // 4) % 2
                q4 = tt % 4
                CP(P, "act", stage[sg][:, :, q4 * 128:(q4 + 1) * 128], ptr[:], ["ptr"], [("stage", sg)])
                if q4 == 3 or tt == ntiles - 1:
                    t0 = (tt - q4) * 128
                    nn = (q4 + 1) * 128
                    P.dma(T["ybT_s"][:, t0:t0 + nn].rearrange("(k p) t -> p k t", p=128), stage[sg][:, :, 0:nn],
                          reads=[("stage", sg)], writes=[("ybT", tt)])
                cur["b"] = save

            ctail.append(tail)
        while ctail:
            ctail.pop(0)()
        P.emit()


def ssd_consts():
    t = np.arange(128)
    tri = (t[:, None] <= t[None, :]).astype(np.float32)
    ones = np.ones((128, 128), np.float32)
    negtri = -tri
    identF = np.eye(128, dtype=np.float32)
    maskT = np.where(t[None, :] >= t[:, None], 0.0, -30000.0).astype(np.float32)
    return np.ascontiguousarray(np.stack([tri, ones, negtri, identF, maskT], axis=1))


def phase_d(nc, T):
    with ExitStack() as es:
        P = Prog(nc, es, "D")
        A = Alloc(nc, es)
        identb = A.sb("d_identb", [128, 128], BF16)
        wtmp = [A.sb(f"d_wtmp{i}", [128, 8, 256], F32) for i in range(2)]
        W = [A.sb(f"d_W{i}", [128, 8, D], BF16) for i in range(3)]
        gffn = A.sb("d_gffn", [128, D], F32)
        inb = [[A.sb(f"d_in{j}{i}", [128, 8, 512], BF16) for i in range(2)] for j in range(4)]
        m1 = [A.sb(f"d_m1{i}", [128, 512], F32) for i in range(2)]
        m2 = [A.sb(f"d_m2{i}", [128, 512], F32) for i in range(2)]
        mT = A.sb("d_mT", [128, 8, 512], BF16)
        xt = [A.sb(f"d_xt{i}", [128, D], F32) for i in range(2)]
        x1 = [A.sb(f"d_x1{i}", [128, D], F32) for i in range(2)]
        sq = A.sb("d_sq", [128, D], F32)
        rs = [A.sb(f"d_rs{i}", [128, 4], F32) for i in range(2)]
        h2 = [A.sb(f"d_h2{i}", [128, D], BF16) for i in range(2)]
        stage = [A.sb(f"d_st{i}", [128, 8, 512], BF16) for i in range(2)]
        pm = [A.ps(f"d_pm{i}", [128, 512], F32) for i in range(6)]
        ptr = A.ps("d_ptr", [128, 8, 128], BF16)

        P.dma(identb[:], T["ident"], writes=["identb"])
        P.dma(gffn[:], T["g_ffn"].partition_broadcast(128), writes=["gffn"])
        ci = 0
        for wi, nm in enumerate(["w_a", "w_b", "w_o"]):
            for c in range(4):
                b = ci % 2
                ci += 1
                P.dma(wtmp[b][:], T[nm][:, :, c * 256:(c + 1) * 256], writes=[("wtmp", b)])
                CP(P, "pool" if ci % 2 else "dve", W[wi][:, :, c * 256:(c + 1) * 256], wtmp[b][:], [("wtmp", b)], [("W", wi)])
        srcs = [("attT_s", 0), ("ybT_s", 0), ("g_s", 0), ("g_s", 1024)]

        def load(tb):
            s = tb % 2
            for j, (nm, r0) in enumerate(srcs):
                P.dma(inb[j][s][:], T[nm][r0:r0 + 1024, tb * 512:(tb + 1) * 512].rearrange("(k p) t -> p k t", p=128),
                      writes=[("in", j, s)])

        pc = {"i": 0}

        def npm():
            i = pc["i"] % 6
            pc["i"] += 1
            return i

        load(0)
        dtail = []
        for tb in range(8):
            s = tb % 2
            if tb + 1 < 8:
                load(tb + 1)
            for cc in range(8):
                pa, pb = npm(), npm()
                for k in range(8):
                    MM(P, pm[pa][:], W[0][:, k, cc * 128:(cc + 1) * 128], inb[0][s][:, k, :], k == 0, k == 7,
                       [("W", 0), ("in", 0, s)], [("pm", pa)])
                for k in range(8):
                    MM(P, pm[pb][:], W[1][:, k, cc * 128:(cc + 1) * 128], inb[1][s][:, k, :], k == 0, k == 7,
                       [("W", 1), ("in", 1, s)], [("pm", pb)])
                r = cc % 2
                TT(P, "dve", m1[r][:], pm[pa][:], inb[2][s][:, cc, :], ALU.mult, [("pm", pa), ("in", 2, s)], [("m1", r)])
                TT(P, "dve", m2[r][:], pm[pb][:], inb[3][s][:, cc, :], ALU.mult, [("pm", pb), ("in", 3, s)], [("m2", r)])
                TT(P, "pool", mT[:, cc, :], m1[r][:], m2[r][:], ALU.add, [("m1", r), ("m2", r)], [("mT", cc)])
            for q4 in range(4):
                tt = tb * 4 + q4
                b = tt % 2
                P.dma(xt[b][:], T["x"][tt * 128:(tt + 1) * 128, :], writes=[("xt", b)])
                for half in range(2):
                    pi = npm()
                    for k in range(8):
                        MM(P, pm[pi][:], mT[:, k, q4 * 128:(q4 + 1) * 128], W[2][:, k, half * 512:(half + 1) * 512],
                           k == 0, k == 7, [("mT", k), ("W", 2)], [("pm", pi)])
                    TT(P, "dve", x1[b][:, half * 512:(half + 1) * 512], pm[pi][:], xt[b][:, half * 512:(half + 1) * 512],
                       ALU.add, [("pm", pi), ("xt", b)], [("x1", b)])
                while dtail:
                    dtail.pop(0)()
                P.dma(T["x1_s"][tt * 128:(tt + 1) * 128, :], x1[b][:], reads=[("x1", b)], writes=[("x1s", tt)])
                TT(P, "pool", sq[:], x1[b][:], x1[b][:], ALU.mult, [("x1", b)], ["sq"])
                RSUM(P, "dve", rs[b][:, 0:1], sq[:], ["sq"], [("rs", b)])
                ACT(P, rs[b][:, 1:2], rs[b][:, 0:1], AF.Ln, [("rs", b)], [("rs", b)], bias=EPS, scale=1.0 / D)
                ACT(P, rs[b][:, 2:3], rs[b][:, 1:2], AF.Exp, [("rs", b)], [("rs", b)], scale=-0.5)
                STT(P, "dve", h2[b][:], x1[b][:], rs[b][:, 2:3], gffn[:], ALU.mult, ALU.mult,
                    [("x1", b), ("rs", b), "gffn"], [("h2", b)])
                def tail(b=b, s=s, q4=q4):
                    for k in range(8):
                        TR(P, ptr[:, k, :], h2[b][:, k * 128:(k + 1) * 128], identb[:], [("h2", b), "identb"], ["ptr"])
                    CP(P, "act", stage[s][:, :, q4 * 128:(q4 + 1) * 128], ptr[:], ["ptr"], [("stage", s)])

                dtail.append(tail)
            while dtail:
                dtail.pop(0)()
            P.dma(T["h2T_s"][:, tb * 512:(tb + 1) * 512].rearrange("(k p) t -> p k t", p=128), stage[s][:],
                  reads=[("stage", s)], writes=[("h2T", tb)])
        P.emit()


def e0_setup(P, A, T):
    CJ0 = 2
    uf = [A.sb(f"e0_uf{i}", [128, 8, CJ0, 128], F32) for i in range(2)]
    ub = [A.sb(f"e0_ub{i}", [128, 8, CJ0, 128], BF16) for i in range(2)]
    vf = [A.sb(f"e0_vf{i}", [128, CJ0, D], F32) for i in range(2)]
    vb = [A.sb(f"e0_vb{i}", [128, CJ0, D], BF16) for i in range(2)]
    st = {"c": 0}

    def load(c):
        b = c % 2
        j0 = c * CJ0
        P.dma(uf[b][:], T["UT_h"][:, :, j0:j0 + CJ0, :], writes=[("uf", b)])
        P.dma(vf[b][:], T["V_h"][:, j0:j0 + CJ0, :], writes=[("vf", b)])

    def step():
        c = st["c"]
        if c >= 128 // CJ0:
            return False
        if c == 0:
            load(0)
        st["c"] += 1
        b = c % 2
        j0 = c * CJ0
        if c + 1 < 128 // CJ0:
            load(c + 1)
        CP(P, "pool", ub[b][:], uf[b][:], [("uf", b)], [("ub", b)])
        CP(P, "act", vb[b][:], vf[b][:], [("vf", b)], [("vb", b)])
        P.dma(T["UTb_s"][:, :, j0:j0 + CJ0, :], ub[b][:], reads=[("ub", b)], writes=[("UTb", c)])
        P.dma(T["Vb_s"][:, j0:j0 + CJ0, :], vb[b][:], reads=[("vb", b)], writes=[("Vb", c)])
        return True

    return step


def phase_e1(nc, T, nblocks=8):
    with ExitStack() as es:
        P = Prog(nc, es, "E1")
        P.skip_dist = 8
        A = Alloc(nc, es)
        e0_step = e0_setup(P, A, T)
        identF = A.sb("e1_identF", [128, 128], F32)
        iota16 = A.sb("e1_iota16", [128, 16], F32)
        thr15 = A.sb("e1_thr15", [128, 15], F32)
        wtmp = [A.sb("e1_wtmp0", [128, 8, 256], F32)] * 2
        Wq = A.sb("e1_Wq", [128, 8, 2048], BF16)
        skf = A.sb("e1_skf", [128, 16, 128], F32)
        skT = A.sb("e1_skT", [128, 16, 128], BF16)
        h2b = [A.sb(f"e1_h2b{i}", [128, 8, 512], BF16) for i in range(2)]
        qT = A.sb("e1_qT", [128, 16, 512], BF16)
        scs_ = [A.sb(f"e1_scs{i}", [128, 16, 128], F32) for i in range(2)]
        scs2 = A.sb("e1_scs2", [128, 16, 128], F32)
        v = A.sb("e1_v", [128, 16, 16], F32)
        iu = A.sb("e1_iu", [128, 16, 16], U32)
        if_ = A.sb("e1_if", [128, 16, 16], F32)
        cand = A.sb("e1_cand", [128, 8, 256], F32)
        cand2 = A.sb("e1_cand2", [128, 8, 256], F32)
        top = A.sb("e1_top", [128, 8, 16], F32)
        pu = A.sb("e1_pu", [128, 8, 16], U32)
        pf = A.sb("e1_pf", [128, 8, 16], F32)
        ge = A.sb("e1_ge", [128, 8, 16, 16], F32)
        ai = A.sb("e1_ai", [128, 8, 16], F32)
        bi = A.sb("e1_bi", [128, 8, 16], F32)
        dd = A.sb("e1_dd", [128, 8, 16], F32)
        zz = A.sb("e1_zz", [128, 16], F32)
        rt = A.sb("e1_rt", [128, 3, 128], F32)
        rstage = [A.sb(f"e1_rst{i}", [128, 3, 512], F32) for i in range(2)]
        pq = [A.ps(f"e1_pq{i}", [128, 512], F32) for i in range(2)]
        scp = [A.ps(f"e1_scp{i}", [128, 512], F32) for i in range(4)]
        ptr = A.ps("e1_ptr", [128, 3, 128], F32)

        P.dma(identF[:], T["ssd_cst"][:, 3, :], writes=["identF"])
        P.dma(iota16[:], T["iota128"][:, 0:16], writes=["iota16"])
        P.dma(thr15[:], T["thr15"], writes=["thr15"])
        P.dma(skf[:], T["skT_h"], writes=["skf"])
        CP(P, "dve", skT[:], skf[:], ["skf"], ["skT"])
        for c in range(8):
            b = 0
            P.dma(wtmp[b][:], T["wq_h"][:, :, c * 256:(c + 1) * 256], writes=[("wtmp", b)])
            CP(P, "pool" if c % 2 else "dve", Wq[:, :, c * 256:(c + 1) * 256], wtmp[b][:], [("wtmp", b)], ["Wq"])

        def load(tb):
            P.dma(h2b[tb % 2][:], T["h2T_s"][:, tb * 512:(tb + 1) * 512].rearrange("(k p) t -> p k t", p=128),
                  writes=[("h2b", tb % 2)])

        def vop(fn, reads, writes):
            P.op("dve", fn, reads, writes)

        load(0)

        def stageS(n):
            tb, q4 = divmod(n, 4)
            s = tb % 2
            if q4 == 0:
                if tb + 1 < nblocks:
                    load(tb + 1)
                for c in range(16):
                    pi = c % 2
                    for k in range(8):
                        MM(P, pq[pi][:], Wq[:, k, c * 128:(c + 1) * 128], h2b[s][:, k, :], k == 0, k == 7,
                           ["Wq", ("h2b", s)], [("pq", pi)])
                    CP(P, "act" if c % 2 else "dve", qT[:, c, :], pq[pi][:], [("pq", pi)], [("qT", c)])
            for _ in range(64 // (4 * nblocks) + 1):
                e0_step()
            for c in range(16):
                MM(P, scp[c // 4][:, (c % 4) * 128:(c % 4 + 1) * 128], qT[:, c, q4 * 128:(q4 + 1) * 128],
                   skT[:, c, :], True, True, [("qT", c), "skT"], [("scp", c // 4)])
            par = n % 2
            for g4 in range(4):
                CP(P, "act", scs_[par][:, g4 * 4:(g4 + 1) * 4, :].rearrange("p c k -> p (c k)"), scp[g4][:],
                   [("scp", g4)], [("scs", par, g4)])

        def stageD(n):
            tb, q4 = divmod(n, 4)
            s = tb % 2
            for _one in (0,):
                par = n % 2
                scs = scs_[par]
                for c in range(16):
                    kr = ("scs", par, c // 4)
                    vop(lambda e, c=c, scs=scs: e.max(out=v[:, c, 0:8], in_=scs[:, c, :]), [kr], [("v", c)])
                for c in range(16):
                    kr = ("scs", par, c // 4)
                    vop(lambda e, c=c, scs=scs: e.max_index(out=iu[:, c, 0:8], in_max=v[:, c, 0:8], in_values=scs[:, c, :]),
                        [kr, ("v", c)], [("iu", c)])
                for c in range(16):
                    kr = ("scs", par, c // 4)
                    vop(lambda e, c=c, scs=scs: e.match_replace(out=scs2[:, c, :], in_to_replace=v[:, c, 0:8],
                                                        in_values=scs[:, c, :], imm_value=-1e30),
                        [kr, ("v", c)], [("scs2", c)])
                for c in range(16):
                    vop(lambda e, c=c: e.max(out=v[:, c, 8:16], in_=scs2[:, c, :]), [("scs2", c)], [("v2", c)])
                for c in range(16):
                    vop(lambda e, c=c: e.max_index(out=iu[:, c, 8:16], in_max=v[:, c, 8:16], in_values=scs2[:, c, :]),
                        [("scs2", c), ("v2", c)], [("iu2", c)])
                allv = [("v", c) for c in range(16)] + [("v2", c) for c in range(16)]
                alli = [("iu", c) for c in range(16)] + [("iu2", c) for c in range(16)]
                CP(P, "dve", if_[:], iu[:], alli, ["if"])
                v4 = v[:].rearrange("p (n h) a -> p n h a", h=2)
                if4 = if_[:].rearrange("p (n h) a -> p n h a", h=2)
                TT(P, "dve", cand[:].rearrange("p n (a b) -> p n a b", b=16),
                   v4[:, :, 0, :].unsqueeze(3).to_broadcast([128, 8, 16, 16]),
                   v4[:, :, 1, :].unsqueeze(2).to_broadcast([128, 8, 16, 16]), ALU.add, allv, ["cand"])
                for n in range(8):
                    vop(lambda e, n=n: e.max(out=top[:, n, 0:8], in_=cand[:, n, :]), ["cand"], [("top", n)])
                for n in range(8):
                    vop(lambda e, n=n: e.max_index(out=pu[:, n, 0:8], in_max=top[:, n, 0:8], in_values=cand[:, n, :]),
                        ["cand", ("top", n)], [("pu", n)])
                for n in range(8):
                    vop(lambda e, n=n: e.match_replace(out=cand2[:, n, :], in_to_replace=top[:, n, 0:8],
                                                        in_values=cand[:, n, :], imm_value=-1e30),
                        ["cand", ("top", n)], [("cand2", n)])
                for n in range(8):
                    vop(lambda e, n=n: e.max(out=top[:, n, 8:16], in_=cand2[:, n, :]), [("cand2", n)], [("top2", n)])
                for n in range(8):
                    vop(lambda e, n=n: e.max_index(out=pu[:, n, 8:16], in_max=top[:, n, 8:16], in_values=cand2[:, n, :]),
                        [("cand2", n), ("top2", n)], [("pu2", n)])
                allt = [("top", n) for n in range(8)] + [("top2", n) for n in range(8)]
                allp = [("pu", n) for n in range(8)] + [("pu2", n) for n in range(8)]
                CP(P, "dve", pf[:], pu[:], allp, ["pf"])
                TT(P, "dve", ge[:, :, :, 0:15], pf[:].unsqueeze(3).to_broadcast([128, 8, 16, 15]),
                   thr15[:].unsqueeze(1).unsqueeze(1).to_broadcast([128, 8, 16, 15]), ALU.is_ge, ["pf", "thr15"], ["ge"])
                RSUM(P, "dve", ai[:], ge[:, :, :, 0:15], ["ge"], ["ai"])
                STT(P, "dve", bi[:], ai[:], -16.0, pf[:], ALU.mult, ALU.add, ["ai", "pf"], ["bi"])
                io4 = iota16[:].unsqueeze(1).unsqueeze(1).to_broadcast([128, 8, 16, 16])
                for x, (sel, half) in enumerate([(ai, 0), (bi, 1)]):
                    TT(P, "dve", ge[:], io4, sel[:].unsqueeze(3).to_broadcast([128, 8, 16, 16]), ALU.is_equal,
                       ["ai", "bi", "iota16"], ["ge"])
                    TT(P, "dve", ge[:], ge[:], if4[:, :, half, :].unsqueeze(2).to_broadcast([128, 8, 16, 16]),
                       ALU.mult, ["ge", "if"], ["ge"])
                    RSUM(P, "dve", rt[:, 1 + x, :].rearrange("p (n r) -> p n r", r=16), ge[:], ["ge"], [("rt", 1 + x)])
                TT(P, "dve", dd[:], top[:], top[:, :, 0:1].to_broadcast([128, 8, 16]), ALU.subtract, allt, ["dd"])
                ACT(P, dd[:], dd[:], AF.Exp, ["dd"], ["dd"])
                RSUM(P, "dve", zz[:, 0:8], dd[:], ["dd"], ["zz"])
                P.op("dve", lambda e: e.reciprocal(out=zz[:, 8:16], in_=zz[:, 0:8]), ["zz"], ["zz"])
                TT(P, "dve", rt[:, 0, :].rearrange("p (n r) -> p n r", r=16), dd[:],
                   zz[:, 8:16].unsqueeze(2).to_broadcast([128, 8, 16]), ALU.mult, ["dd", "zz"], [("rt", 0)])
                for x in range(3):
                    TR(P, ptr[:, x, :], rt[:, x, :], identF[:], [("rt", x), "identF"], ["ptr"])
                CP(P, "act", rstage[s][:, :, q4 * 128:(q4 + 1) * 128], ptr[:], ["ptr"], [("rstage", s)])
            if q4 == 3:
                P.dma(T["route_s"][:, :, tb * 512:(tb + 1) * 512].rearrange("x p t -> p x t"), rstage[s][:],
                      reads=[("rstage", s)], writes=[("route", tb)])

        ntl = nblocks * 4
        stageS(0)
        for n in range(ntl):
            if n + 1 < ntl:
                stageS(n + 1)
            stageD(n)
        while e0_step():
            pass
        P.emit()


def phase_e2(nc, T, ngroups=16):
    G = 256
    JH = 64
    with ExitStack() as es:
        P = Prog(nc, es, "E2")
        A = Alloc(nc, es)
        iota = A.sb("e2_iota", [128, 128], F32)
        gfin = A.sb("e2_gfin", [128, D], F32)
        Gsq = [A.sb(f"e2_Gs{i}", [128, G, JH], BF16) for i in range(2)]
        SBK = 16
        ohc = {"n": 0}
        OHI = [A.sb(f"e2_OHI{i}", [128, SBK, 128], BF16) for i in range(2)]
        OHJ = [A.sb(f"e2_OHJ{i}", [128, SBK, JH], BF16) for i in range(2)]
        OHJg = [A.sb(f"e2_OHJg{i}", [128, SBK, JH], BF16) for i in range(2)]
        CJ, NBUF = 4, 4
        NCH = 128 // CJ
        UTc = [A.sb(f"e2_UTc{i}", [128, 8, CJ, 128], BF16) for i in range(NBUF)]
        Vc = [A.sb(f"e2_Vc{i}", [128, CJ, D], BF16) for i in range(NBUF)]
        h2g = [A.sb(f"e2_h2g{i}", [128, 8, G], BF16) for i in range(2)]
        rtg = [A.sb(f"e2_rtg{i}", [128, 3, G], F32) for i in range(2)]
        x1g = A.sb("e2_x1g", [128, 2, D], F32)
        ga = [A.sb(f"e2_ga{i}", [128, G], BF16) for i in range(3)]
        Wj = [A.sb(f"e2_Wj{i}", [128, G], BF16) for i in range(3)]
        x2 = A.sb("e2_x2", [128, D], F32)
        sq = A.sb("e2_sq", [128, D], F32)
        rs = A.sb("e2_rs", [128, 4], F32)
        ot = [A.sb(f"e2_ot{i}", [128, D], F32) for i in range(2)]
        oacc = [[A.ps(f"e2_oacc{a}{b}", [128, 512], F32) for b in range(2)] for a in range(2)]
        pbank = [A.ps(f"e2_pb{i}", [128, 512], F32) for i in range(3)]
        gps = A.ps("e2_gps", [128, 512], F32)

        P.dma(iota[:], T["iota128"], writes=["iota"])
        iotab = A.sb("e2_iotab", [128, 128], BF16)
        CP(P, "dve", iotab[:], iota[:], ["iota"], ["iotab"])
        P.dma(gfin[:], T["g_final"].partition_broadcast(128), writes=["gfin"])

        def load_group(g):
            s = g % 2
            P.dma(h2g[s][:], T["h2T_s"][:, g * G:(g + 1) * G].rearrange("(k p) t -> p k t", p=128), writes=[("h2g", s)])
            P.dma(rtg[s][:], T["route_s"][:, :, g * G:(g + 1) * G].rearrange("x p t -> p x t"), writes=[("rtg", s)])

        def load_chunk(gc):
            b = gc % NBUF
            j0 = (gc % NCH) * CJ
            P.dma(UTc[b][:], T["UTb_s"][:, :, j0:j0 + CJ, :], writes=[("UTc", b)])
            P.dma(Vc[b][:], T["Vb_s"][:, j0:j0 + CJ, :], writes=[("Vc", b)])

        def oh_build(q, sb_):
            g_, p_ = divmod(q, 2)
            s_ = g_ % 2
            t0 = sb_ * SBK
            j0 = p_ * JH
            ob = sb_ % 2
            for t in reversed(range(SBK)):
                TS(P, "dve", OHI[ob][:, t, :], iotab[:], rtg[s_][:, 1, t0 + t:t0 + t + 1], ALU.is_equal,
                   ["iotab", ("rtg", s_)], [("OHI", ob, t)])
                TS(P, "dve", OHJg[ob][:, t, :], iotab[:, j0:j0 + JH], rtg[s_][:, 2, t0 + t:t0 + t + 1], ALU.is_equal,
                   ["iotab", ("rtg", s_)], [("OHJg", ob, t)], s2=rtg[s_][:, 0, t0 + t:t0 + t + 1], op1=ALU.mult)

        def oh_mm(q, sb_):
            t0 = sb_ * SBK
            ob = sb_ % 2
            for t in range(SBK):
                MM(P, gps[:, (t % 8) * JH:(t % 8 + 1) * JH], OHI[ob][:, t, :], OHJg[ob][:, t, :], True, True,
                   [("OHI", ob, t), ("OHJg", ob, t)], ["gps"])
                if t % 8 == 7:
                    CP(P, "act", Gsq[q % 2][:, t0 + t - 7:t0 + t + 1, :].rearrange("p t j -> p (t j)"), gps[:],
                       ["gps"], [("Gs", q % 2)])

        def oh_subblock(q, sb_):
            oh_build(q, sb_)
            oh_mm(q, sb_)

        load_group(0)
        for gc in range(NBUF):
            load_chunk(gc)
        for sb_ in range(G // SBK):
            oh_subblock(0, sb_)
        NP = 2 * ngroups
        fin = 0
        for g in range(ngroups):
            s = g % 2
            if g + 1 < ngroups:
                load_group(g + 1)
            P.dma(x1g[:], T["x1_s"][g * G:(g + 1) * G, :].rearrange("(a p) c -> p a c", p=128), writes=["x1g"])

            def stage1(j):
                gc = g * NCH + j // CJ
                b = gc % NBUF
                jj = j % CJ
                r = j % 3
                q = 2 * g + j // JH
                for k in range(8):
                    MM(P, pbank[r][:, 0:G], UTc[b][:, k, jj, :], h2g[s][:, k, :], k == 0, k == 7,
                       [("UTc", b), ("h2g", s)], [("pbank", r)])
                ACT(P, ga[r][:], pbank[r][:, 0:G], AF.Gelu, [("pbank", r)], [("ga", r)])
                TT(P, "pool", Wj[r][:], ga[r][:], Gsq[q % 2][:, :, j % JH], ALU.mult, [("ga", r), ("Gs", q % 2)], [("Wj", r)])

            def stage2(j):
                b = (g * NCH + j // CJ) % NBUF
                jj = j % CJ
                r = j % 3
                for tl in range(2):
                    for half in range(2):
                        MM(P, oacc[tl][half][:], Wj[r][:, tl * 128:(tl + 1) * 128],
                           Vc[b][:, jj, half * 512:(half + 1) * 512], j == 0, j == 127,
                           [("Wj", r), ("Vc", b)], [("oacc", tl, half)])
                gc = g * NCH + j // CJ
                if j % CJ == CJ - 1 and gc + NBUF < ngroups * NCH:
                    load_chunk(gc + NBUF)

            LOOK = 2
            for x in range(128 + LOOK):
                if x < 128:
                    stage1(x)
                    qn = 2 * g + x // JH + 1
                    xl = x % JH
                    if qn < NP:
                        if xl % 4 == 0:
                            oh_build(qn, xl // 4)
                        if (xl % 4 == 1 and xl >= 5):
                            oh_mm(qn, (xl - 5) // 4)
                        if xl == JH - 1:
                            oh_mm(qn, 15)
                if x >= LOOK:
                    stage2(x - LOOK)
            for tl in range(2):
                f = fin % 2
                fin += 1
                for half in range(2):
                    TT(P, "dve", x2[:, half * 512:(half + 1) * 512], oacc[tl][half][:],
                       x1g[:, tl, half * 512:(half + 1) * 512], ALU.add, [("oacc", tl, half), "x1g"], ["x2"])
                TT(P, "pool", sq[:], x2[:], x2[:], ALU.mult, ["x2"], ["sq"])
                RSUM(P, "dve", rs[:, 0:1], sq[:], ["sq"], ["rs"])
                ACT(P, rs[:, 1:2], rs[:, 0:1], AF.Ln, ["rs"], ["rs"], bias=EPS, scale=1.0 / D)
                ACT(P, rs[:, 2:3], rs[:, 1:2], AF.Exp, ["rs"], ["rs"], scale=-0.5)
                STT(P, "dve", ot[f][:], x2[:], rs[:, 2:3], gfin[:], ALU.mult, ALU.mult, ["x2", "rs", "gfin"], [("ot", f)])
                r0 = g * G + tl * 128
                P.dma(T["out"][r0:r0 + 128, :], ot[f][:], reads=[("ot", f)], writes=[("out", r0)])
        P.emit()
```

```python
from contextlib import ExitStack
import math
import numpy as np
import ml_dtypes
import concourse.bass as bass
import concourse.mybir as mybir
from concourse.bass_utils import run_bass_kernel_spmd

F32 = mybir.dt.float32
BF16 = mybir.dt.bfloat16
U32 = mybir.dt.uint32
ALU = mybir.AluOpType
AF = mybir.ActivationFunctionType
AX = mybir.AxisListType

S = 4096
D = 1024
NT = S // 128
EPS = 1e-6
INW = 8208
COMPUTE = ("pe", "act", "dve", "pool")
NDMA_SEMS = 24


class SemPool:
    def __init__(self, nc, es):
        self.nc, self.es = nc, es
        self.dsem = [es.enter_context(nc.semaphore(f"dma{i}")) for i in range(NDMA_SEMS)]
        self.dval = [0] * NDMA_SEMS
        self.drr = 0
        self.n = 0

    def new(self, tag):
        self.n += 1
        return self.es.enter_context(self.nc.semaphore(f"c{self.n}_{tag}"))


SEM_ROLL = 30000


class Prog:
    def __init__(self, nc, es, tag):
        self.nc = nc
        self.tag = tag
        self.G = POOLS[id(nc)]
        self.q = {e: [] for e in COMPUTE + ("sp",)}
        self.cnt = {e: 0 for e in COMPUTE}
        self.epoch = {e: 0 for e in COMPUTE}
        self.semobj = {}
        for e in COMPUTE:
            self.semobj[("c", e, 0)] = self.G.new(f"{tag}_{e}")
        for i in range(NDMA_SEMS):
            self.semobj[("d", i)] = self.G.dsem[i]
        self.last_w = {}
        self.readers = {}
        self.known = {e: {} for e in COMPUTE + ("sp",)}
        self.skip_dist = 8

    def _deps(self, eng, reads, writes, extra=()):
        deps = {}
        for r in reads:
            for k, v in self.last_w.get(r, {}).items():
                if deps.get(k, 0) < v:
                    deps[k] = v
        for w in writes:
            for k, v in self.last_w.get(w, {}).items():
                if deps.get(k, 0) < v:
                    deps[k] = v
            for k, v in self.readers.get(w, {}).items():
                if deps.get(k, 0) < v:
                    deps[k] = v
        for k, v in extra:
            if deps.get(k, 0) < v:
                deps[k] = v
        waits = []
        kn = self.known[eng]
        for k, v in deps.items():
            if eng == "pe" and k[0] == "c" and k[1] == "pe":
                continue
            if (self.skip_dist is not None and k[0] == "c" and k[1] == eng and k[2] == self.epoch[eng]
                    and self.cnt[eng] + 1 - v >= self.skip_dist):
                continue
            if kn.get(k, 0) >= v:
                continue
            kn[k] = v
            waits.append((k, v))
        return waits

    def _commit(self, tok, reads, writes):
        k, v = tok
        for r in reads:
            d = self.readers.setdefault(r, {})
            if d.get(k, 0) < v:
                d[k] = v
        for w in writes:
            self.last_w[w] = {k: v}
            self.readers[w] = {}

    def op(self, eng, fn, reads=(), writes=()):
        waits = self._deps(eng, reads, writes)
        if self.cnt[eng] >= SEM_ROLL:
            self.epoch[eng] += 1
            self.cnt[eng] = 0
            self.semobj[("c", eng, self.epoch[eng])] = self.G.new(f"{self.tag}_{eng}{self.epoch[eng]}")
        self.cnt[eng] += 1
        key = ("c", eng, self.epoch[eng])
        tok = (key, self.cnt[eng])
        self.q[eng].append((waits, fn, (key, 1)))
        self._commit(tok, reads, writes)

    def dma(self, out, in_, reads=(), writes=(), queue="sp", **kw):
        G = self.G
        i = G.drr
        G.drr = (G.drr + 1) % NDMA_SEMS
        extra = [(("d", i), G.dval[i])] if G.dval[i] else []
        waits = self._deps(queue, reads, writes, extra=extra)
        G.dval[i] += 16
        tok = (("d", i), G.dval[i])
        self.q[queue].append((waits, lambda e: e.dma_start(out=out, in_=in_, **kw), (("d", i), 16)))
        self._commit(tok, reads, writes)

    def merge(self, keys, newkey):
        d = {}
        for key in keys:
            for k, v in self.last_w.get(key, {}).items():
                if d.get(k, 0) < v:
                    d[k] = v
        self.last_w[newkey] = d
        self.readers.setdefault(newkey, {})

    def emit(self):
        nc = self.nc
        toks = []
        for e in COMPUTE:
            for ep in range(self.epoch[e] + 1):
                v = self.cnt[e] if ep == self.epoch[e] else SEM_ROLL
                if v:
                    toks.append((("c", e, ep), v))
        toks += [(("d", i), self.G.dval[i]) for i in range(NDMA_SEMS) if self.G.dval[i]]
        for eng in COMPUTE + ("sp",):
            kn = self.known[eng]
            waits = [(k, v) for k, v in toks if kn.get(k, 0) < v]
            if waits:
                self.q[eng].append((waits, None, None))
        engmap = {"pe": "tensor", "act": "scalar", "dve": "vector", "pool": "gpsimd", "sp": "sync"}
        with nc.Block() as block:
            for e, bname in engmap.items():
                lst = self.q[e]

                def body(eng, lst=lst):
                    for waits, fn, inc in lst:
                        for k, v in waits:
                            eng.wait_ge(self.semobj[k], v)
                        if fn is not None:
                            fn(eng).then_inc(self.semobj[inc[0]], inc[1])

                getattr(block, bname)(body)


def MM(P, out, lhsT, rhs, start, stop, reads, writes, **kw):
    P.op("pe", lambda e: e.matmul(out, lhsT=lhsT, rhs=rhs, start=start, stop=stop, **kw), reads, writes)


def TR(P, out, in_, ident, reads, writes):
    P.op("pe", lambda e: e.transpose(out=out, in_=in_, identity=ident), reads, writes)


def ACT(P, out, in_, func, reads, writes, bias=None, scale=None, eng="act"):
    kw = {}
    if bias is not None:
        kw["bias"] = bias
    if scale is not None:
        kw["scale"] = scale
    P.op("act", lambda e: e.activation(out=out, in_=in_, func=func, **kw), reads, writes)


def TT(P, eng, out, in0, in1, op, reads, writes):
    P.op(eng, lambda e: e.tensor_tensor(out=out, in0=in0, in1=in1, op=op), reads, writes)


def TS(P, eng, out, in0, s1, op0, reads, writes, s2=None, op1=None):
    if op1 is None:
        P.op(eng, lambda e: e.tensor_scalar(out=out, in0=in0, scalar1=s1, scalar2=None, op0=op0), reads, writes)
    else:
        P.op(eng, lambda e: e.tensor_scalar(out=out, in0=in0, scalar1=s1, scalar2=s2, op0=op0, op1=op1), reads, writes)


def STT(P, eng, out, in0, scalar, in1, op0, op1, reads, writes):
    P.op(eng, lambda e: e.scalar_tensor_tensor(out=out, in0=in0, scalar=scalar, in1=in1, op0=op0, op1=op1), reads, writes)


def CP(P, eng, out, in_, reads, writes):
    if eng == "act":
        P.op("act", lambda e: e.copy(out=out, in_=in_), reads, writes)
    else:
        P.op(eng, lambda e: e.tensor_copy(out=out, in_=in_), reads, writes)


def MEMSET(P, eng, ap, val, writes):
    P.op(eng, lambda e: e.memset(ap, val), (), writes)


def RSUM(P, eng, out, in_, reads, writes):
    P.op(eng, lambda e: e.reduce_sum(out=out, in_=in_, axis=AX.X), reads, writes)


class Alloc:
    def __init__(self, nc, es):
        self.nc, self.es = nc, es

    def sb(self, name, shape, dt):
        return self.es.enter_context(self.nc.sbuf_tensor(name, shape, dt))

    def ps(self, name, shape, dt):
        return self.es.enter_context(self.nc.psum_tensor(name, shape, dt))


def phase_a(nc, T):
    with ExitStack() as es:
        P = Prog(nc, es, "A")
        A = Alloc(nc, es)
        hT = A.sb("a_hT", [128, 8, S + 3], BF16)
        ident = A.sb("a_ident", [128, 128], BF16)
        gmt = A.sb("a_gmt", [128, 8], F32)
        ones_row = A.sb("a_ones", [1, 128], BF16)
        cbrow_f = A.sb("a_cbrowf", [1, 2048], F32)
        cbrow = A.sb("a_cbrow", [1, 2048], BF16)
        cbcol = A.sb("a_cbcol", [128, 16], F32)
        xt = [A.sb(f"a_xt{i}", [128, D], F32) for i in range(2)]
        sq = A.sb("a_sq", [128, D], F32)
        hb = [A.sb(f"a_hb{i}", [128, D], BF16) for i in range(2)]
        ss = [A.sb(f"a_ss{i}", [128, 4], F32) for i in range(2)]
        wf = [A.sb(f"a_wf{i}", [128, 8, 256], F32) for i in range(2)]
        wb = [A.sb(f"a_wb{i}", [128, 8, 256], BF16) for i in range(2)]
        pre = [A.sb(f"a_pre{i}", [128, S + 3], BF16) for i in range(2)]
        cacc = A.sb("a_cacc", [128, S], F32)
        cwcol = A.sb("a_cwcol", [128, 16, 4], F32)
        st_tmc = [A.sb(f"a_sttmc{i}", [128, NT, 128], BF16) for i in range(2)]
        st_fm = [A.sb(f"a_stfm{i}", [128, S], BF16) for i in range(2)]
        st_tm = [A.sb(f"a_sttm{i}", [128, 4, 256], BF16) for i in range(2)]
        st_dt = A.sb("a_stdt", [128, NT, 16], F32)
        pt = A.ps("a_pt", [128, 8, 128], BF16)
        pm = [A.ps(f"a_pm{i}", [128, 512], F32) for i in range(4)]

        P.dma(ident[:], T["ident"], writes=["ident"])
        P.dma(gmt[:], T["gm"], writes=["gmt"])
        P.dma(cbrow_f[:], T["conv_b_row"], writes=["cbrow_f"])
        P.dma(cbcol[:], T["conv_b_col"], writes=["cbcol"])
        MEMSET(P, "pool", hT[:, :, 0:3], 0.0, ["hT_pad"])
        P.dma(cwcol[:], T["conv_w_col"], writes=["cwcol"])
        for i in range(2):
            MEMSET(P, "pool", pre[i][:, 0:3], 0.0, [("prepad", i)])
        MEMSET(P, "pool", ones_row[:], 1.0, ["ones"])
        CP(P, "dve", cbrow[:], cbrow_f[:], ["cbrow_f"], ["cbrow"])

        for tt in range(NT):
            b = tt % 2
            P.dma(xt[b][:], T["x"][tt * 128:(tt + 1) * 128, :], writes=[("xt", b)])
            TT(P, "dve", sq[:], xt[b][:], xt[b][:], ALU.mult, [("xt", b)], ["sq"])
            RSUM(P, "dve", ss[b][:, 0:1], sq[:], ["sq"], [("ss", b)])
            ACT(P, ss[b][:, 1:2], ss[b][:, 0:1], AF.Ln, [("ss", b)], [("ss", b)], bias=EPS, scale=1.0 / D)
            ACT(P, ss[b][:, 2:3], ss[b][:, 1:2], AF.Exp, [("ss", b)], [("ss", b)], scale=-0.5)
            TS(P, "dve", hb[b][:], xt[b][:], ss[b][:, 2:3], ALU.mult, [("ss", b), ("xt", b)], [("hb", b)])
            for k in range(8):
                TR(P, pt[:, k, :], hb[b][:, k * 128:(k + 1) * 128], ident[:], [("hb", b), "ident"], ["pt"])
            CP(P, "act" if tt % 2 else "dve", hT[:, :, 3 + tt * 128: 3 + (tt + 1) * 128], pt[:], ["pt"], [("hT", tt)])
        P.merge([("hT", tt) for tt in range(NT)] + ["hT_pad"], "hT")

        gm_bc = gmt[:].unsqueeze(2).to_broadcast([128, 8, 256])
        segs = [("q", 0, 1024), ("k", 1024, 2048), ("v", 2048, 3072), ("z", 3072, 4096),
                ("xs", 4096, 5120), ("B", 5120, 5632), ("C", 5632, 6144), ("dt", 6144, 6160),
                ("g", 6160, 8208)]
        blocks = []
        for name, c0, c1 in segs:
            for c in range(c0, c1, 256):
                blocks.append((name, c, min(256, c1 - c)))
        state = {"pm": 0, "ev": 0, "fm": 0, "tm": 0}

        def next_pm():
            i = state["pm"]
            state["pm"] = (i + 1) % 4
            return i

        def load_w(bi):
            name, c0, ncol = blocks[bi]
            b = bi % 2
            P.dma(wf[b][:, :, 0:ncol], T["w_in"][:, :, c0:c0 + ncol], writes=[("wf", b)])

        load_w(0)
        for bi, (name, c0, ncol) in enumerate(blocks):
            b = bi % 2
            if bi + 1 < len(blocks):
                load_w(bi + 1)
            conv = False
            TT(P, "pool", wb[b][:, :, 0:ncol], wf[b][:, :, 0:ncol], gm_bc[:, :, 0:ncol], ALU.mult,
               [("wf", b), "gmt"], [("wb", b)])
            wkeys = [("wb", b)]

            def w_of(tap):
                return wb[b]

            taps = range(1)
            if name in ("xs", "B", "C"):
                for cc in range(ncol // 128):
                    j = (c0 - 4096) // 128 + cc
                    pr = pre[j % 2]
                    for tb in range(8):
                        pi = next_pm()
                        for k in range(8):
                            MM(P, pm[pi][:, :], wb[b][:, k, cc * 128:(cc + 1) * 128],
                               hT[:, k, 3 + tb * 512: 3 + (tb + 1) * 512], k == 0, k == 7, wkeys + ["hT"], [("pm", pi)])
                        CP(P, "dve" if state["ev"] % 2 else "act", pr[:, 3 + tb * 512: 3 + (tb + 1) * 512], pm[pi][:, :],
                           [("pm", pi)], [("pre", j % 2, tb)])
                        state["ev"] += 1
                    P.merge([("pre", j % 2, tb) for tb in range(8)] + [("prepad", j % 2)], ("preall", j % 2))
                    ce = "dve"
                    TS(P, ce, cacc[:], pr[:, 3:3 + S], cwcol[:, j, 3:4], ALU.mult, [("preall", j % 2), "cwcol"], ["cacc"])
                    for tap in (2, 1, 0):
                        STT(P, ce, cacc[:], pr[:, tap:tap + S], cwcol[:, j, tap:tap + 1], cacc[:], ALU.mult, ALU.add,
                            [("preall", j % 2), "cwcol", "cacc"], ["cacc"])
                    for tb in range(8):
                        P.readers.setdefault(("pre", j % 2, tb), {}).update(P.readers.get(("preall", j % 2), {}))
                    sfi = state["fm"] % 2
                    state["fm"] += 1
                    sf = st_fm[sfi]
                    ACT(P, sf[:, :], cacc[:], AF.Silu, ["cacc", "cbcol"], [("stfm", sfi)], bias=cbcol[:, j:j + 1])
                    if name in ("B", "C"):
                        r0 = (c0 - (5120 if name == "B" else 5632)) + cc * 128
                        dram = (T["BT_s"] if name == "B" else T["CT_s"])[r0:r0 + 128, :]
                        P.dma(dram, sf[:, :], reads=[("stfm", sfi)], writes=[("dram_fm", name, r0)])
                    if name in ("xs", "B"):
                        x = state["tm"] % 2
                        state["tm"] += 1
                        for tt in range(NT):
                            TR(P, pt[:, tt % 8, :], sf[:, tt * 128:(tt + 1) * 128], ident[:], [("stfm", sfi), "ident"], ["pt"])
                            if tt % 8 == 7:
                                CP(P, "dve" if (tt // 8) % 2 else "act", st_tmc[x][:, tt - 7:tt + 1, :], pt[:],
                                   ["pt"], [("sttmc", x)])
                        col0 = (c0 - (4096 if name == "xs" else 5120)) + cc * 128
                        dram = (T["xs_s"] if name == "xs" else T["B_s"])[:, col0:col0 + 128]
                        P.dma(dram.rearrange("(t p) c -> p t c", p=128), st_tmc[x][:], reads=[("sttmc", x)],
                              writes=[("dram_tmc", name, col0)])
                continue
            if name in ("q", "k", "g", "B", "C"):
                for cc in range(ncol // 128):
                    sfi = state["fm"] % 2
                    state["fm"] += 1
                    sf = st_fm[sfi]
                    for tb in range(8):
                        pi = next_pm()
                        n_mm = len(taps) * 8
                        i = 0
                        for tap in taps:
                            sh = tap if conv else 3
                            for k in range(8):
                                MM(P, pm[pi][:, :], w_of(tap)[:, k, cc * 128:(cc + 1) * 128],
                                   hT[:, k, tb * 512 + sh: tb * 512 + sh + 512], i == 0, i == n_mm - 1,
                                   wkeys + ["hT"], [("pm", pi)])
                                i += 1
                        dst = sf[:, tb * 512:(tb + 1) * 512]
                        if name == "q":
                            P.op("act", lambda e, o=dst, i_=pm[pi][:, :]: e.mul(out=o, in_=i_, mul=0.125),
                                 [("pm", pi)], [("stfm", sfi)])
                        elif name == "k":
                            CP(P, "dve" if state["ev"] % 2 else "act", dst, pm[pi][:, :], [("pm", pi)], [("stfm", sfi)])
                            state["ev"] += 1
                        elif name == "g":
                            ACT(P, dst, pm[pi][:, :], AF.Sigmoid, [("pm", pi)], [("stfm", sfi)])
                        else:
                            j = (c0 - 4096 + cc * 128) // 128
                            ACT(P, dst, pm[pi][:, :], AF.Silu, [("pm", pi), "cbcol"], [("stfm", sfi)],
                                bias=cbcol[:, j:j + 1])
                    if name == "q":
                        r0 = c0 + cc * 128
                        dram = T["qk_s"][r0:r0 + 128, :]
                    elif name == "k":
                        r0 = 1024 + (c0 - 1024) + cc * 128
                        dram = T["qk_s"][r0:r0 + 128, :]
                    elif name == "g":
                        r0 = c0 - 6160 + cc * 128
                        dram = T["g_s"][r0:r0 + 128, :]
                    elif name == "B":
                        r0 = c0 - 5120 + cc * 128
                        dram = T["BT_s"][r0:r0 + 128, :]
                    else:
                        r0 = c0 - 5632 + cc * 128
                        dram = T["CT_s"][r0:r0 + 128, :]
                    P.dma(dram, sf[:, :], reads=[("stfm", sfi)], writes=[("dram_fm", name, r0)])
            if name in ("v", "z", "xs", "B", "dt"):
                for tt in range(NT):
                    pi = next_pm()
                    n_mm = len(taps) * 8 + (1 if conv else 0)
                    i = 0
                    for tap in taps:
                        sh = tap if conv else 3
                        for k in range(8):
                            MM(P, pm[pi][:, 0:ncol], hT[:, k, tt * 128 + sh: tt * 128 + sh + 128],
                               w_of(tap)[:, k, 0:ncol], i == 0, i == n_mm - 1, wkeys + ["hT"], [("pm", pi)])
                            i += 1
                    if conv:
                        cx0 = c0 - 4096
                        MM(P, pm[pi][:, 0:ncol], ones_row[0:1, :], cbrow[0:1, cx0:cx0 + ncol], False, True,
                           ["ones", "cbrow"], [("pm", pi)])
                    if name == "dt":
                        CP(P, "dve", st_dt[:, tt, :], pm[pi][:, 0:16], [("pm", pi)], ["stdt"])
                        continue
                    q4 = tt % 4
                    si = (state["tm"] // 4) % 2
                    state["tm"] += 1
                    dst = st_tm[si][:, q4, :]
                    if conv:
                        ACT(P, dst, pm[pi][:, 0:ncol], AF.Silu, [("pm", pi)], [("sttm", si)])
                    else:
                        CP(P, "dve" if state["ev"] % 2 else "act", dst, pm[pi][:, 0:ncol], [("pm", pi)], [("sttm", si)])
                        state["ev"] += 1
                    if q4 == 3:
                        t0 = (tt - 3) * 128
                        if name == "v":
                            dram = T["v_s"][t0:t0 + 512, c0 - 2048:c0 - 2048 + 256]
                        elif name == "z":
                            dram = T["z_s"][t0:t0 + 512, c0 - 3072:c0 - 3072 + 256]
                        elif name == "xs":
                            dram = T["xs_s"][t0:t0 + 512, c0 - 4096:c0 - 4096 + 256]
                        else:
                            dram = T["B_s"][t0:t0 + 512, c0 - 5120:c0 - 5120 + 256]
                        P.dma(dram.rearrange("(a p) c -> p a c", p=128), st_tm[si][:], reads=[("sttm", si)],
                              writes=[("dram_tm", name, c0, tt)])
                if name == "dt":
                    P.dma(T["dt_s"].rearrange("(a p) c -> p a c", p=128), st_dt[:], reads=["stdt"], writes=["dram_dt"])
        P.emit()


POOLS = {}
_SEM_ES = []


def declare_tensors(nc, debug):
    T = {}

    def inp(name, shape, dt):
        T[name] = nc.dram_tensor(name, shape, dt, kind="ExternalInput").ap()

    def scr(name, shape, dt):
        kind = "ExternalOutput" if debug else "Internal"
        T[name] = nc.dram_tensor(name, shape, dt, kind=kind).ap()

    inp("x", [S, D], F32)
    inp("w_in", [128, 8, INW], F32)
    inp("gm", [128, 8], F32)
    inp("conv_w_col", [128, 16, 4], F32)
    inp("conv_b_row", [1, 2048], F32)
    inp("conv_b_col", [128, 16], F32)
    inp("ident", [128, 128], BF16)
    scr("qk_s", [2048, S], BF16)
    scr("v_s", [S, 1024], BF16)
    scr("z_s", [S, 1024], BF16)
    scr("xs_s", [S, 1024], BF16)
    scr("B_s", [S, 512], BF16)
    scr("BT_s", [512, S], BF16)
    scr("CT_s", [512, S], BF16)
    scr("dt_s", [S, 16], F32)
    scr("g_s", [2048, S], BF16)
    inp("qaug", [8, 2, S], BF16)
    inp("kbias", [128, 8, 36], F32)
    inp("dbias", [8, 128, 4, 512], F32)
    for nm in ("lam_q1", "lam_k1", "lam_q2", "lam_k2"):
        inp(nm, [1, 64], F32)
    inp("g_subln", [1, 128], F32)
    scr("attT_s", [1024, S], BF16)
    inp("ssd_cst", [128, 5, 128], F32)
    for nm in ("dt_bias", "a_log", "d_skip"):
        inp(nm, [1, 16], F32)
    inp("g_ssd", [1, D], F32)
    scr("ybT_s", [1024, S], BF16)
    for nm in ("w_a", "w_b", "w_o"):
        inp(nm, [128, 8, D], F32)
    inp("g_ffn", [1, D], F32)
    scr("x1_s", [S, D], F32)
    scr("h2T_s", [1024, S], BF16)
    inp("UT_h", [128, 8, 128, 128], F32)
    inp("V_h", [128, 128, D], F32)
    inp("wq_h", [128, 8, 2048], F32)
    inp("skT_h", [128, 16, 128], F32)
    inp("iota128", [128, 128], F32)
    inp("thr15", [128, 15], F32)
    inp("g_final", [1, D], F32)
    scr("UTb_s", [128, 8, 128, 128], BF16)
    scr("Vb_s", [128, 128, D], BF16)
    scr("route_s", [3, 128, S], F32)
    T["out"] = nc.dram_tensor("out", [S, D], F32, kind="ExternalOutput").ap()
    return T


def build_program(debug=False, phases="ABCD012", **kw):
    nc = bass.Bass("TRN2", target_bir_lowering=False)
    T = declare_tensors(nc, debug)
    _SEM_ES.append(ExitStack())
    POOLS[id(nc)] = SemPool(nc, _SEM_ES[-1])
    if "A" in phases:
        phase_a(nc, T)
    if "B" in phases:
        phase_b(nc, T, **kw.get("b", {}))
    if "C" in phases:
        phase_c(nc, T, **kw.get("c", {}))
    if "D" in phases:
        phase_d(nc, T)
    if "1" in phases:
        phase_e1(nc, T, **kw.get("e1", {}))
    if "2" in phases:
        phase_e2(nc, T, **kw.get("e2", {}))
    return nc


def host_inputs(inp, b):
    f = np.float32
    d = {}
    d["x"] = np.ascontiguousarray(inp["x"][b], dtype=f)
    d["w_in"] = np.ascontiguousarray(inp["w_in"][0].reshape(8, 128, INW).transpose(1, 0, 2), dtype=f)
    d["gm"] = np.ascontiguousarray(inp["g_mix"][0].reshape(8, 128).T, dtype=f)
    d["conv_w_col"] = np.ascontiguousarray(inp["conv_w"][0].reshape(4, 16, 128).transpose(2, 1, 0), dtype=f)
    d["conv_b_row"] = np.ascontiguousarray(inp["conv_b"][0].reshape(1, 2048), dtype=f)
    d["conv_b_col"] = np.ascontiguousarray(inp["conv_b"][0].reshape(16, 128).T, dtype=f)
    d["ident"] = np.eye(128).astype(ml_dtypes.bfloat16)
    qaug, kbias, dbias = attn_consts()
    d["qaug"], d["kbias"], d["dbias"] = qaug, kbias, dbias
    for nm in ("lam_q1", "lam_k1", "lam_q2", "lam_k2"):
        d[nm] = np.ascontiguousarray(inp[nm][0].reshape(1, 64), dtype=f)
    d["g_subln"] = np.ascontiguousarray(inp["g_subln"][0].reshape(1, 128), dtype=f)
    d["ssd_cst"] = ssd_consts()
    for nm in ("dt_bias", "a_log", "d_skip"):
        d[nm] = np.ascontiguousarray(inp[nm][0].reshape(1, 16), dtype=f)
    d["g_ssd"] = np.ascontiguousarray(inp["g_ssd"][0].reshape(1, D), dtype=f)
    for nm, src in (("w_a", "w_branch_a"), ("w_b", "w_branch_b"), ("w_o", "w_out")):
        d[nm] = np.ascontiguousarray(inp[src][0].reshape(8, 128, D).transpose(1, 0, 2), dtype=f)
    d["g_ffn"] = np.ascontiguousarray(inp["g_ffn"][0].reshape(1, D), dtype=f)
    d.update(shared_peer_inputs(inp))
    return d


_SHARED = {}


def shared_peer_inputs(inp):
    key = id(inp["expert_u"])
    if key in _SHARED:
        return _SHARED[key]
    f = np.float32
    d = {}
    U = np.asarray(inp["expert_u"][0], dtype=f)
    d["UT_h"] = np.ascontiguousarray(U.reshape(128, 128, 8, 128).transpose(3, 2, 1, 0))
    d["V_h"] = np.ascontiguousarray(np.asarray(inp["expert_v"][0], dtype=f).reshape(128, 128, D))
    d["wq_h"] = np.ascontiguousarray(inp["w_query"][0].reshape(8, 128, 2048).transpose(1, 0, 2), dtype=f)
    sk = np.asarray(inp["sub_keys"][0], dtype=f)
    d["skT_h"] = np.ascontiguousarray(sk.reshape(16, 128, 128).transpose(2, 0, 1))
    d["iota128"] = np.ascontiguousarray(np.broadcast_to(np.arange(128, dtype=f), (128, 128)))
    d["thr15"] = np.ascontiguousarray(np.broadcast_to(16.0 * np.arange(1, 16, dtype=f), (128, 15)))
    d["g_final"] = np.ascontiguousarray(inp["g_final"].reshape(1, D), dtype=f)
    _SHARED.clear()
    _SHARED[key] = d
    return d


def kernel(**inputs):
    nc = build_program()
    in_maps = [host_inputs(inputs, b) for b in range(8)]
    res = run_bass_kernel_spmd(nc, in_maps, core_ids=list(range(8)))
    return np.stack([r["out"] for r in res.results], axis=0)


LAM0 = 0.8 - 0.6 * math.exp(-0.3 * 0)
SLOPES = [2.0 ** (-(i + 1)) for i in range(8)]
BAND_CUT = 64.0


def phase_b(nc, T, heads=range(8)):
    with ExitStack() as es:
        P = Prog(nc, es, "B")
        A = Alloc(nc, es)
        ident = A.sb("b_ident", [128, 128], BF16)
        qa = [[A.sb(f"b_qa{s}{m}", [66, S], BF16) for m in range(2)] for s in range(2)]
        ka = [[A.sb(f"b_ka{s}{m}", [66, S], BF16) for m in range(2)] for s in range(2)]
        va = [A.sb(f"b_va{s}", [128, NT, 129], BF16) for s in range(2)]
        dbias = [A.sb(f"b_db{s}", [128, 4, 512], F32) for s in range(2)]
        kbias = A.sb("b_kbias", [128, 8, 36], F32)
        lamv = A.sb("b_lamv", [128, 4, 64], F32)
        lamt = A.sb("b_lamt", [128, 8], F32)
        gs = A.sb("b_gs", [128, 128], F32)
        pT = [A.sb(f"b_pT{r}", [128, 512], BF16) for r in range(4)]
        sbias = [A.sb(f"b_sb{r}", [128, 512], F32) for r in range(2)]
        rec = [A.sb(f"b_rec{r}", [128, 8], F32) for r in range(2)]
        o1 = [A.sb(f"b_o1{r}", [128, 128], F32) for r in range(2)]
        oo = [A.sb(f"b_oo{r}", [128, 128], F32) for r in range(2)]
        osq = A.sb("b_osq", [128, 128], F32)
        on = [A.sb(f"b_on{r}", [128, 128], BF16) for r in range(2)]
        stage = [A.sb(f"b_st{r}", [128, 512], BF16) for r in range(2)]
        psS = [A.ps(f"b_ps{i}", [128, 512], F32) for i in range(4)]
        accb = [A.ps(f"b_acc{i}", [128, 512], F32) for i in range(3)]
        ptr = A.ps("b_ptr", [128, 4, 128], BF16)

        def acc(m, i):
            s = m * 4 + i
            return accb[s // 3][:, (s % 3) * 129:(s % 3) * 129 + 129]

        P.dma(ident[:], T["ident"], writes=["ident"])
        P.dma(kbias[:], T["kbias"], writes=["kbias"])
        for j, nm in enumerate(["lam_q1", "lam_k1", "lam_q2", "lam_k2"]):
            P.dma(lamv[:, j, :], T[nm].partition_broadcast(128), writes=[("lamv", j)])
        P.dma(gs[:], T["g_subln"].partition_broadcast(128), writes=["gs"])
        TT(P, "dve", lamv[:, 0, :], lamv[:, 0, :], lamv[:, 1, :], ALU.mult, [("lamv", 0), ("lamv", 1)], [("lamv", 0)])
        TT(P, "dve", lamv[:, 2, :], lamv[:, 2, :], lamv[:, 3, :], ALU.mult, [("lamv", 2), ("lamv", 3)], [("lamv", 2)])
        RSUM(P, "dve", lamt[:, 0:1], lamv[:, 0, :], [("lamv", 0)], ["lamt"])
        RSUM(P, "dve", lamt[:, 1:2], lamv[:, 2, :], [("lamv", 2)], ["lamt"])
        ACT(P, lamt[:, 2:4], lamt[:, 0:2], AF.Exp, ["lamt"], ["lamt"])
        TT(P, "dve", lamt[:, 4:5], lamt[:, 3:4], lamt[:, 2:3], ALU.subtract, ["lamt"], ["lamt"])
        TS(P, "dve", lamt[:, 4:5], lamt[:, 4:5], -LAM0, ALU.add, ["lamt"], ["lamt"])
        TS(P, "dve", gs[:], gs[:], 1.0 - LAM0, ALU.mult, ["gs"], ["gs"])
        for s in range(2):
            for m in range(2):
                MEMSET(P, "pool", ka[s][m][64:66, :], 1.0, [("ka1", s, m)])
            MEMSET(P, "pool", va[s][:, :, 128:129], 1.0, [("va1", s)])

        heads = list(heads)

        def load_head(idx):
            h = heads[idx]
            s = idx % 2
            for m in range(2):
                r0 = (h * 2 + m) * 64
                P.dma(qa[s][m][0:64, :], T["qk_s"][r0:r0 + 64, :], writes=[("qa", s, m)])
                P.dma(qa[s][m][64:66, :], T["qaug"][h], writes=[("qa", s, m)])
                P.dma(ka[s][m][0:64, :], T["qk_s"][1024 + r0:1024 + r0 + 64, :], writes=[("ka", s, m)])
            P.dma(va[s][:, :, 0:128], T["v_s"][:, h * 128:(h + 1) * 128].rearrange("(t p) c -> p t c", p=128),
                  writes=[("va", s)])
            P.dma(dbias[s][:], T["dbias"][h], writes=[("db", s)])

        load_head(0)
        st = {"ps": 0, "pt": 0, "fin": 0, "stg": 0}
        for idx, h in enumerate(heads):
            s = idx % 2
            if idx + 1 < len(heads):
                load_head(idx + 1)
            slope = SLOPES[h]
            for qb in range(8):
                for b3 in range(3):
                    MEMSET(P, "dve", accb[b3][:, 0:387], 0.0, [("acc", b3)])
                units = []
                for kt in range(4 * qb + 4):
                    j = kt - 4 * qb
                    if j < 0:
                        mind = qb * 512 - (kt * 128 + 127)
                        if slope * mind > BAND_CUT:
                            continue
                    for m in range(2):
                        units.append((kt, j, m))

                def stage1(u, kt, j, m):
                    c0 = 128 * j if j > 0 else 0
                    dd = (4 * qb - kt) + 3
                    pi = u % 4
                    MM(P, psS[pi][:, c0:512], ka[s][m][0:66, kt * 128:(kt + 1) * 128],
                       qa[s][m][0:66, qb * 512 + c0:(qb + 1) * 512], True, True,
                       [("ka", s, m), ("ka1", s, m), ("qa", s, m)], [("ps", pi)])
                    if j >= 0:
                        TT(P, "dve", sbias[m][:, c0:512], psS[pi][:, c0:512], dbias[s][:, j, c0:512], ALU.add,
                           [("ps", pi), ("db", s)], [("sbias", m)])
                        ACT(P, pT[pi][:, c0:512], sbias[m][:, c0:512], AF.Exp, [("sbias", m), "kbias"],
                            [("pT", pi)], bias=kbias[:, h, dd:dd + 1])
                    else:
                        ACT(P, pT[pi][:, :], psS[pi][:, :], AF.Exp, [("ps", pi), "kbias"], [("pT", pi)],
                            bias=kbias[:, h, dd:dd + 1])

                def stage2(u, kt, j, m):
                    pi = u % 4
                    for i in range(max(j, 0), 4):
                        sl = m * 4 + i
                        MM(P, acc(m, i), pT[pi][:, i * 128:(i + 1) * 128], va[s][:, kt, :], False, False,
                           [("pT", pi), ("va", s), ("va1", s)], [("acc", sl // 3)], skip_group_check=True)

                LOOK = 3
                ub = st["ps"]
                for x in range(len(units) + LOOK):
                    if x < len(units):
                        stage1(ub + x, *units[x])
                    if x >= LOOK:
                        stage2(ub + x - LOOK, *units[x - LOOK])
                st["ps"] = ub + len(units)
                sg = st["stg"] % 2
                st["stg"] += 1
                for i in range(4):
                    f = st["fin"] % 2
                    st["fin"] += 1
                    a0, a1 = acc(0, i), acc(1, i)
                    k0, k1 = ("acc", (0 * 4 + i) // 3), ("acc", (4 + i) // 3)
                    P.op("dve", lambda e, o=rec[f][:, 0:1], i_=a0[:, 128:129]: e.reciprocal(out=o, in_=i_), [k0], [("rec", f)])
                    P.op("dve", lambda e, o=rec[f][:, 1:2], i_=a1[:, 128:129]: e.reciprocal(out=o, in_=i_), [k1], [("rec", f)])
                    TS(P, "dve", rec[f][:, 2:3], rec[f][:, 1:2], lamt[:, 4:5], ALU.mult, [("rec", f), "lamt"], [("rec", f)])
                    TS(P, "dve", o1[f][:], a0[:, 0:128], rec[f][:, 0:1], ALU.mult, [k0, ("rec", f)], [("o1", f)])
                    STT(P, "dve", oo[f][:], a1[:, 0:128], rec[f][:, 2:3], o1[f][:], ALU.mult, ALU.add,
                        [k1, ("rec", f), ("o1", f)], [("oo", f)])
                    TT(P, "dve", osq[:], oo[f][:], oo[f][:], ALU.mult, [("oo", f)], ["osq"])
                    RSUM(P, "dve", rec[f][:, 3:4], osq[:], ["osq"], [("rec", f)])
                    ACT(P, rec[f][:, 4:5], rec[f][:, 3:4], AF.Ln, [("rec", f)], [("rec", f)], bias=EPS, scale=1.0 / 128)
                    ACT(P, rec[f][:, 5:6], rec[f][:, 4:5], AF.Exp, [("rec", f)], [("rec", f)], scale=-0.5)
                    STT(P, "dve", on[f][:], oo[f][:], rec[f][:, 5:6], gs[:], ALU.mult, ALU.mult,
                        [("oo", f), ("rec", f), "gs"], [("on", f)])
                    TR(P, ptr[:, i, :], on[f][:], ident[:], [("on", f), "ident"], [("ptr", i)])
                    CP(P, "act", stage[sg][:, i * 128:(i + 1) * 128], ptr[:, i, :], [("ptr", i)], [("stage", sg)])
                P.dma(T["attT_s"][h * 128:(h + 1) * 128, qb * 512:(qb + 1) * 512], stage[sg][:],
                      reads=[("stage", sg)], writes=[("attT", h, qb)])
        P.emit()


def attn_consts():
    bf = ml_dtypes.bfloat16
    pos = np.arange(S)
    qrel = pos % 512
    qaug = np.zeros((8, 2, S), np.float32)
    kbias = np.zeros((128, 8, 36), np.float32)
    dbias = np.zeros((8, 128, 4, 512), np.float32)
    ki = np.arange(128)
    qi = np.arange(512)
    for h in range(8):
        sl = SLOPES[h]
        qaug[h, 0] = -sl * (qrel % 256)
        qaug[h, 1] = -sl * 256.0 * (qrel // 256)
        for dd in range(36):
            kbias[:, h, dd] = sl * (ki - 128.0 * (dd - 3))
        for j in range(4):
            k = (128 * j + ki)[:, None]
            q = qi[None, :]
            masked = (k // 64) > (q // 64)
            fut = (k > q) & ~masked
            dbias[h, :, j, :] = np.where(masked, -30000.0, np.where(fut, -2.0 * sl * (k - q), 0.0))
    return qaug.astype(bf), kbias, dbias


def phase_c(nc, T, ntiles=NT):
    with ExitStack() as es:
        P = Prog(nc, es, "C")
        A = Alloc(nc, es)
        identb = A.sb("c_identb", [128, 128], BF16)
        cst = A.sb("c_cst", [128, 5, 128], F32)
        BT = A.sb("c_BT", [128, 4, S], BF16)
        CT = A.sb("c_CT", [128, 4, S], BF16)
        gssd = A.sb("c_gssd", [128, D], F32)
        pv = A.sb("c_pv", [128, 3, 16], F32)
        dtr = [A.sb(f"c_dtr{i}", [128, 16], F32) for i in range(2)]
        xs = [A.sb(f"c_xs{i}", [128, 16, 64], BF16) for i in range(2)]
        zt = [A.sb(f"c_zt{i}", [128, D], BF16) for i in range(2)]
        Bt = [A.sb(f"c_Bt{i}", [128, 512], BF16) for i in range(2)]
        sc_ = [A.sb(f"c_sc{i}", [128, 12, 16], F32) for i in range(2)]
        sm_ = [A.sb(f"c_sm{i}", [128, 32], F32) for i in range(2)]
        ead_ = [A.sb(f"c_ead{i}", [128, 32], F32) for i in range(2)]
        atri_ = [A.sb(f"c_atri{i}", [128, 16, 128], F32) for i in range(2)]
        LT_ = [A.sb(f"c_LT{i}", [128, 16, 128], BF16) for i in range(2)]
        cbL_ = [A.sb(f"c_cbL{i}", [128, 16, 128], BF16) for i in range(2)]
        Xdt_ = [A.sb(f"c_Xdt{i}", [128, 16, 64], BF16) for i in range(2)]
        Xdd_ = [A.sb(f"c_Xdd{i}", [128, 16, 64], BF16) for i in range(2)]
        t1_ = [A.sb(f"c_t1{i}", [128, 16, 64], F32) for i in range(2)]
        y1_ = [A.sb(f"c_y1{i}", [128, 16, 64], F32) for i in range(2)]
        t2_ = [A.sb(f"c_t2{i}", [128, 16, 64], F32) for i in range(2)]
        h32 = A.sb("c_h32", [128, 16, 64], F32)
        hbf = A.sb("c_hbf", [128, D], BF16)
        sz_ = [A.sb(f"c_sz{i}", [128, D], F32) for i in range(2)]
        yz_ = [A.sb(f"c_yz{i}", [128, D], F32) for i in range(2)]
        sq_ = [A.sb(f"c_sq{i}", [128, D], F32) for i in range(2)]
        rs_ = [A.sb(f"c_rs{i}", [128, 4], F32) for i in range(2)]
        yn_ = [A.sb(f"c_yn{i}", [128, D], BF16) for i in range(2)]
        stage = [A.sb(f"c_st{i}", [128, 8, 512], BF16) for i in range(2)]
        big = [A.ps(f"c_big{i}", [128, 1024], F32) for i in range(2)]
        cb = A.ps("c_cb", [128, 4, 128], F32)
        small = A.ps("c_small", [128, 32], F32)
        ptr = A.ps("c_ptr", [128, 8, 128], BF16)

        tri, ones, negtri, identF, maskT = (cst[:, i, :] for i in range(5))
        P.dma(identb[:], T["ident"], writes=["identb"])
        P.dma(cst[:], T["ssd_cst"], writes=["cst"])
        P.dma(BT[:], T["BT_s"].rearrange("(g n) t -> n g t", n=128), writes=["BT"])
        P.dma(CT[:], T["CT_s"].rearrange("(g n) t -> n g t", n=128), writes=["CT"])
        P.dma(gssd[:], T["g_ssd"].partition_broadcast(128), writes=["gssd"])
        for j, nm in enumerate(["dt_bias", "a_log", "d_skip"]):
            P.dma(pv[:, j, :], T[nm].partition_broadcast(128), writes=[("pv", j)])
        ACT(P, pv[:, 1, :], pv[:, 1, :], AF.Exp, [("pv", 1)], [("pv", 1)])
        TS(P, "dve", pv[:, 1, :], pv[:, 1, :], -1.0, ALU.mult, [("pv", 1)], [("pv", 1)])
        MEMSET(P, "pool", h32[:], 0.0, ["h32"])
        MEMSET(P, "pool", hbf[:], 0.0, ["hbf"])
        dtb_bc, negA, dskip = pv[:, 0, :], pv[:, 1, :], pv[:, 2, :]

        def load(tt):
            b = tt % 2
            r = slice(tt * 128, (tt + 1) * 128)
            P.dma(dtr[b][:], T["dt_s"][r, :], writes=[("dtr", b)])
            P.dma(xs[b][:].rearrange("p h d -> p (h d)"), T["xs_s"][r, :], writes=[("xs", b)])
            P.dma(zt[b][:], T["z_s"][r, :], writes=[("zt", b)])
            P.dma(Bt[b][:], T["B_s"][r, :], writes=[("Bt", b)])

        WK = set(["x1", "nx", "mn", "ee", "ll", "rr", "dt", "a", "dd", "dS", "w2", "sm", "ead", "atri", "Xdt", "Xdd",
                  "t1", "y1", "t2", "sz", "yz", "sq", "rs", "yn"])
        cur = {"b": 0}
        _op = P.op

        def op2(eng, fn, reads=(), writes=()):
            bb = cur["b"]

            def kx(x):
                if isinstance(x, str) and x in WK:
                    return (x, bb)
                if isinstance(x, tuple) and x[0] in ("LT", "cbL"):
                    return x + (bb,)
                return x
            _op(eng, fn, [kx(x) for x in reads], [kx(x) for x in writes])

        P.op = op2
        bigc = {"i": 0}
        ctail = []

        def nbig():
            i = bigc["i"] % 2
            bigc["i"] += 1
            return i

        load(0)
        for tt in range(ntiles):
            b = tt % 2
            if tt + 1 < ntiles:
                load(tt + 1)
            tk = slice(tt * 128, (tt + 1) * 128)
            sc = sc_[b]; sm = sm_[b]; ead = ead_[b]; atri = atri_[b]; LT = LT_[b]; cbL = cbL_[b]; Xdt = Xdt_[b]; Xdd = Xdd_[b]; t1 = t1_[b]; y1 = y1_[b]; t2 = t2_[b]; sz = sz_[b]; yz = yz_[b]; sq = sq_[b]; rs = rs_[b]; yn = yn_[b]
            cur["b"] = b
            x1, nx, mn, ee, ll, rr, dt, a_, dd, dS, w2 = (sc[:, i, :] for i in range(11))
            TT(P, "dve", x1, dtr[b][:], dtb_bc, ALU.add, [("dtr", b), ("pv", 0)], ["x1"])
            TS(P, "dve", nx, x1, -1.0, ALU.mult, ["x1"], ["nx"])
            TT(P, "dve", mn, x1, nx, ALU.min, ["x1", "nx"], ["mn"])
            ACT(P, ee, mn, AF.Exp, ["mn"], ["ee"])
            ACT(P, ll, ee, AF.Ln, ["ee"], ["ll"], bias=1.0)
            TS(P, "dve", rr, x1, 0.0, ALU.max, ["x1"], ["rr"])
            TT(P, "dve", dt, rr, ll, ALU.add, ["rr", "ll"], ["dt"])
            TT(P, "dve", a_, dt, negA, ALU.mult, ["dt", ("pv", 1)], ["a"])
            MM(P, small[:, 0:16], tri, a_, True, True, ["cst", "a"], ["small"])
            MM(P, small[:, 16:32], ones, a_, True, True, ["cst", "a"], ["small"])
            CP(P, "dve", sm[:], small[:], ["small"], ["sm"])
            TT(P, "dve", dd, sm[:, 16:32], sm[:, 0:16], ALU.subtract, ["sm"], ["dd"])
            ACT(P, ead[:], sm[:], AF.Exp, ["sm"], ["ead"])
            ACT(P, dS, dd, AF.Exp, ["dd"], ["dS"])
            TT(P, "dve", w2, dt, dS, ALU.mult, ["dt", "dS"], ["w2"])
            ea, dec = ead[:, 0:16], ead[:, 16:32]
            TT(P, "dve", atri[:], a_.unsqueeze(2).to_broadcast([128, 16, 128]),
               tri.unsqueeze(1).to_broadcast([128, 16, 128]), ALU.mult, ["a", "cst"], ["atri"])
            for half in range(2):
                bi = nbig()
                for j in range(2):
                    h0 = half * 8 + j * 4
                    o = big[bi][:, j * 512:(j + 1) * 512]
                    MM(P, o, ones, atri[:, h0:h0 + 4, :], True, False, ["cst", "atri"], [("big", bi)])
                    MM(P, o, negtri, a_[:, h0:h0 + 4].unsqueeze(2).to_broadcast([128, 4, 128]), False, False,
                       ["cst", "a"], [("big", bi)])
                    MM(P, o, identF, maskT.unsqueeze(1).to_broadcast([128, 4, 128]), False, True,
                       ["cst"], [("big", bi)])
                ACT(P, LT[:, half * 8:(half + 1) * 8, :].rearrange("p h l -> p (h l)"), big[bi][:], AF.Exp,
                    [("big", bi)], [("LT", half)])
            for g in range(4):
                MM(P, cb[:, g, :], BT[:, g, tk], CT[:, g, tk], True, True, ["BT", "CT"], ["cb"])
            for g in range(4):
                TT(P, "dve", cbL[:, 4 * g:4 * g + 4, :], cb[:, g, :].unsqueeze(1).to_broadcast([128, 4, 128]),
                   LT[:, 4 * g:4 * g + 4, :], ALU.mult, ["cb", ("LT", g // 2)], [("cbL", g)])
            TT(P, "pool", Xdt[:], xs[b][:], dt.unsqueeze(2).to_broadcast([128, 16, 64]), ALU.mult,
               [("xs", b), "dt"], ["Xdt"])
            TT(P, "pool", Xdd[:], xs[b][:], w2.unsqueeze(2).to_broadcast([128, 16, 64]), ALU.mult,
               [("xs", b), "w2"], ["Xdd"])
            while ctail:
                ctail.pop(0)()
            bo = nbig()
            for g in range(4):
                MM(P, big[bo][:, g * 256:(g + 1) * 256], CT[:, g, tk], hbf[:, g * 256:(g + 1) * 256], True, True,
                   ["CT", "hbf"], [("big", bo)])
            TT(P, "dve", t1[:], big[bo][:].rearrange("p (h d) -> p h d", d=64),
               ea.unsqueeze(2).to_broadcast([128, 16, 64]), ALU.mult, [("big", bo), "ead"], ["t1"])
            by = nbig()
            for h in range(16):
                MM(P, big[by][:, h * 64:(h + 1) * 64], cbL[:, h, :], Xdt[:, h, :], True, True,
                   [("cbL", h // 4), "Xdt"], [("big", by)])
            TT(P, "dve", y1[:], big[by][:].rearrange("p (h d) -> p h d", d=64), t1[:], ALU.add,
               [("big", by), "t1"], ["y1"])
            bs = nbig()
            for g in range(4):
                MM(P, big[bs][:, g * 256:(g + 1) * 256], Bt[b][:, g * 128:(g + 1) * 128],
                   Xdd[:, 4 * g:4 * g + 4, :], True, True, [("Bt", b), "Xdd"], [("big", bs)])
            TT(P, "pool", h32[:], h32[:], dec.unsqueeze(2).to_broadcast([128, 16, 64]), ALU.mult,
               ["h32", "ead"], ["h32"])
            TT(P, "dve", h32[:], big[bs][:].rearrange("p (h d) -> p h d", d=64), h32[:], ALU.add,
               [("big", bs), "h32"], ["h32"])
            CP(P, "pool", hbf[:], h32[:].rearrange("p h d -> p (h d)"), ["h32"], ["hbf"])
            TT(P, "pool", t2[:], xs[b][:], dskip.unsqueeze(2).to_broadcast([128, 16, 64]), ALU.mult,
               [("xs", b), ("pv", 2)], ["t2"])
            TT(P, "dve", y1[:], y1[:], t2[:], ALU.add, ["y1", "t2"], ["y1"])
            ACT(P, sz[:], zt[b][:], AF.Silu, [("zt", b)], ["sz"])
            TT(P, "dve", yz[:], y1[:].rearrange("p h d -> p (h d)"), sz[:], ALU.mult, ["y1", "sz"], ["yz"])
            TT(P, "pool", sq[:], yz[:], yz[:], ALU.mult, ["yz"], ["sq"])
            RSUM(P, "dve", rs[:, 0:1], sq[:], ["sq"], ["rs"])
            ACT(P, rs[:, 1:2], rs[:, 0:1], AF.Ln, ["rs"], ["rs"], bias=EPS, scale=1.0 / D)
            ACT(P, rs[:, 2:3], rs[:, 1:2], AF.Exp, ["rs"], ["rs"], scale=-0.5)
            STT(P, "dve", yn[:], yz[:], rs[:, 2:3], gssd[:], ALU.mult, ALU.mult, ["yz", "rs", "gssd"], ["yn"])
            def tail(tt=tt, b=b, yn=yn):
                save = cur["b"]
                cur["b"] = b
                for k in range(8):
                    TR(P, ptr[:, k, :], yn[:, k * 128:(k + 1) * 128], identb[:], ["yn", "identb"], ["ptr"])
                sg = (tt // 4) % 2
                q4 = tt % 4
                CP(P, "act", stage[sg][:, :, q4 * 128:(q4 + 1) * 128], ptr[:], ["ptr"], [("stage", sg)])
                if q4 == 3 or tt == ntiles - 1:
                    t0 = (tt - q4) * 128
                    nn = (q4 + 1) * 128
                    P.dma(T["ybT_s"][:, t0:t0 + nn].rearrange("(k p) t -> p k t", p=128), stage[sg][:, :, 0:nn],
                          reads=[("stage", sg)], writes=[("ybT", tt)])
                cur["b"] = save

            ctail.append(tail)
        while ctail:
            ctail.pop(0)()
        P.emit()


def ssd_consts():
    t = np.arange(128)
    tri = (t[:, None] <= t[None, :]).astype(np.float32)
    ones = np.ones((128, 128), np.float32)
    negtri = -tri
    identF = np.eye(128, dtype=np.float32)
    maskT = np.where(t[None, :] >= t[:, None], 0.0, -30000.0).astype(np.float32)
    return np.ascontiguousarray(np.stack([tri, ones, negtri, identF, maskT], axis=1))


def phase_d(nc, T):
    with ExitStack() as es:
        P = Prog(nc, es, "D")
        A = Alloc(nc, es)
        identb = A.sb("d_identb", [128, 128], BF16)
        wtmp = [A.sb(f"d_wtmp{i}", [128, 8, 256], F32) for i in range(2)]
        W = [A.sb(f"d_W{i}", [128, 8, D], BF16) for i in range(3)]
        gffn = A.sb("d_gffn", [128, D], F32)
        inb = [[A.sb(f"d_in{j}{i}", [128, 8, 512], BF16) for i in range(2)] for j in range(4)]
        m1 = [A.sb(f"d_m1{i}", [128, 512], F32) for i in range(2)]
        m2 = [A.sb(f"d_m2{i}", [128, 512], F32) for i in range(2)]
        mT = A.sb("d_mT", [128, 8, 512], BF16)
        xt = [A.sb(f"d_xt{i}", [128, D], F32) for i in range(2)]
        x1 = [A.sb(f"d_x1{i}", [128, D], F32) for i in range(2)]
        sq = A.sb("d_sq", [128, D], F32)
        rs = [A.sb(f"d_rs{i}", [128, 4], F32) for i in range(2)]
        h2 = [A.sb(f"d_h2{i}", [128, D], BF16) for i in range(2)]
        stage = [A.sb(f"d_st{i}", [128, 8, 512], BF16) for i in range(2)]
        pm = [A.ps(f"d_pm{i}", [128, 512], F32) for i in range(6)]
        ptr = A.ps("d_ptr", [128, 8, 128], BF16)

        P.dma(identb[:], T["ident"], writes=["identb"])
        P.dma(gffn[:], T["g_ffn"].partition_broadcast(128), writes=["gffn"])
        ci = 0
        for wi, nm in enumerate(["w_a", "w_b", "w_o"]):
            for c in range(4):
                b = ci % 2
                ci += 1
                P.dma(wtmp[b][:], T[nm][:, :, c * 256:(c + 1) * 256], writes=[("wtmp", b)])
                CP(P, "pool" if ci % 2 else "dve", W[wi][:, :, c * 256:(c + 1) * 256], wtmp[b][:], [("wtmp", b)], [("W", wi)])
        srcs = [("attT_s", 0), ("ybT_s", 0), ("g_s", 0), ("g_s", 1024)]

        def load(tb):
            s = tb % 2
            for j, (nm, r0) in enumerate(srcs):
                P.dma(inb[j][s][:], T[nm][r0:r0 + 1024, tb * 512:(tb + 1) * 512].rearrange("(k p) t -> p k t", p=128),
                      writes=[("in", j, s)])

        pc = {"i": 0}

        def npm():
            i = pc["i"] % 6
            pc["i"] += 1
            return i

        load(0)
        dtail = []
        for tb in range(8):
            s = tb % 2
            if tb + 1 < 8:
                load(tb + 1)
            for cc in range(8):
                pa, pb = npm(), npm()
                for k in range(8):
                    MM(P, pm[pa][:], W[0][:, k, cc * 128:(cc + 1) * 128], inb[0][s][:, k, :], k == 0, k == 7,
                       [("W", 0), ("in", 0, s)], [("pm", pa)])
                for k in range(8):
                    MM(P, pm[pb][:], W[1][:, k, cc * 128:(cc + 1) * 128], inb[1][s][:, k, :], k == 0, k == 7,
                       [("W", 1), ("in", 1, s)], [("pm", pb)])
                r = cc % 2
                TT(P, "dve", m1[r][:], pm[pa][:], inb[2][s][:, cc, :], ALU.mult, [("pm", pa), ("in", 2, s)], [("m1", r)])
                TT(P, "dve", m2[r][:], pm[pb][:], inb[3][s][:, cc, :], ALU.mult, [("pm", pb), ("in", 3, s)], [("m2", r)])
                TT(P, "pool", mT[:, cc, :], m1[r][:], m2[r][:], ALU.add, [("m1", r), ("m2", r)], [("mT", cc)])
            for q4 in range(4):
                tt = tb * 4 + q4
                b = tt % 2
                P.dma(xt[b][:], T["x"][tt * 128:(tt + 1) * 128, :], writes=[("xt", b)])
                for half in range(2):
                    pi = npm()
                    for k in range(8):
                        MM(P, pm[pi][:], mT[:, k, q4 * 128:(q4 + 1) * 128], W[2][:, k, half * 512:(half + 1) * 512],
                           k == 0, k == 7, [("mT", k), ("W", 2)], [("pm", pi)])
                    TT(P, "dve", x1[b][:, half * 512:(half + 1) * 512], pm[pi][:], xt[b][:, half * 512:(half + 1) * 512],
                       ALU.add, [("pm", pi), ("xt", b)], [("x1", b)])
                while dtail:
                    dtail.pop(0)()
                P.dma(T["x1_s"][tt * 128:(tt + 1) * 128, :], x1[b][:], reads=[("x1", b)], writes=[("x1s", tt)])
                TT(P, "pool", sq[:], x1[b][:], x1[b][:], ALU.mult, [("x1", b)], ["sq"])
                RSUM(P, "dve", rs[b][:, 0:1], sq[:], ["sq"], [("rs", b)])
                ACT(P, rs[b][:, 1:2], rs[b][:, 0:1], AF.Ln, [("rs", b)], [("rs", b)], bias=EPS, scale=1.0 / D)
                ACT(P, rs[b][:, 2:3], rs[b][:, 1:2], AF.Exp, [("rs", b)], [("rs", b)], scale=-0.5)
                STT(P, "dve", h2[b][:], x1[b][:], rs[b][:, 2:3], gffn[:], ALU.mult, ALU.mult,
                    [("x1", b), ("rs", b), "gffn"], [("h2", b)])
                def tail(b=b, s=s, q4=q4):
                    for k in range(8):
                        TR(P, ptr[:, k, :], h2[b][:, k * 128:(k + 1) * 128], identb[:], [("h2", b), "identb"], ["ptr"])
                    CP(P, "act", stage[s][:, :, q4 * 128:(q4 + 1) * 128], ptr[:], ["ptr"], [("stage", s)])

                dtail.append(tail)
            while dtail:
                dtail.pop(0)()
            P.dma(T["h2T_s"][:, tb * 512:(tb + 1) * 512].rearrange("(k p) t -> p k t", p=128), stage[s][:],
                  reads=[("stage", s)], writes=[("h2T", tb)])
        P.emit()


def e0_setup(P, A, T):
    CJ0 = 2
    uf = [A.sb(f"e0_uf{i}", [128, 8, CJ0, 128], F32) for i in range(2)]
    ub = [A.sb(f"e0_ub{i}", [128, 8, CJ0, 128], BF16) for i in range(2)]
    vf = [A.sb(f"e0_vf{i}", [128, CJ0, D], F32) for i in range(2)]
    vb = [A.sb(f"e0_vb{i}", [128, CJ0, D], BF16) for i in range(2)]
    st = {"c": 0}

    def load(c):
        b = c % 2
        j0 = c * CJ0
        P.dma(uf[b][:], T["UT_h"][:, :, j0:j0 + CJ0, :], writes=[("uf", b)])
        P.dma(vf[b][:], T["V_h"][:, j0:j0 + CJ0, :], writes=[("vf", b)])

    def step():
        c = st["c"]
        if c >= 128 // CJ0:
            return False
        if c == 0:
            load(0)
        st["c"] += 1
        b = c % 2
        j0 = c * CJ0
        if c + 1 < 128 // CJ0:
            load(c + 1)
        CP(P, "pool", ub[b][:], uf[b][:], [("uf", b)], [("ub", b)])
        CP(P, "act", vb[b][:], vf[b][:], [("vf", b)], [("vb", b)])
        P.dma(T["UTb_s"][:, :, j0:j0 + CJ0, :], ub[b][:], reads=[("ub", b)], writes=[("UTb", c)])
        P.dma(T["Vb_s"][:, j0:j0 + CJ0, :], vb[b][:], reads=[("vb", b)], writes=[("Vb", c)])
        return True

    return step


def phase_e1(nc, T, nblocks=8):
    with ExitStack() as es:
        P = Prog(nc, es, "E1")
        P.skip_dist = 8
        A = Alloc(nc, es)
        e0_step = e0_setup(P, A, T)
        identF = A.sb("e1_identF", [128, 128], F32)
        iota16 = A.sb("e1_iota16", [128, 16], F32)
        thr15 = A.sb("e1_thr15", [128, 15], F32)
        wtmp = [A.sb("e1_wtmp0", [128, 8, 256], F32)] * 2
        Wq = A.sb("e1_Wq", [128, 8, 2048], BF16)
        skf = A.sb("e1_skf", [128, 16, 128], F32)
        skT = A.sb("e1_skT", [128, 16, 128], BF16)
        h2b = [A.sb(f"e1_h2b{i}", [128, 8, 512], BF16) for i in range(2)]
        qT = A.sb("e1_qT", [128, 16, 512], BF16)
        scs_ = [A.sb(f"e1_scs{i}", [128, 16, 128], F32) for i in range(2)]
        scs2 = A.sb("e1_scs2", [128, 16, 128], F32)
        v = A.sb("e1_v", [128, 16, 16], F32)
        iu = A.sb("e1_iu", [128, 16, 16], U32)
        if_ = A.sb("e1_if", [128, 16, 16], F32)
        cand = A.sb("e1_cand", [128, 8, 256], F32)
        cand2 = A.sb("e1_cand2", [128, 8, 256], F32)
        top = A.sb("e1_top", [128, 8, 16], F32)
        pu = A.sb("e1_pu", [128, 8, 16], U32)
        pf = A.sb("e1_pf", [128, 8, 16], F32)
        ge = A.sb("e1_ge", [128, 8, 16, 16], F32)
        ai = A.sb("e1_ai", [128, 8, 16], F32)
        bi = A.sb("e1_bi", [128, 8, 16], F32)
        dd = A.sb("e1_dd", [128, 8, 16], F32)
        zz = A.sb("e1_zz", [128, 16], F32)
        rt = A.sb("e1_rt", [128, 3, 128], F32)
        rstage = [A.sb(f"e1_rst{i}", [128, 3, 512], F32) for i in range(2)]
        pq = [A.ps(f"e1_pq{i}", [128, 512], F32) for i in range(2)]
        scp = [A.ps(f"e1_scp{i}", [128, 512], F32) for i in range(4)]
        ptr = A.ps("e1_ptr", [128, 3, 128], F32)

        P.dma(identF[:], T["ssd_cst"][:, 3, :], writes=["identF"])
        P.dma(iota16[:], T["iota128"][:, 0:16], writes=["iota16"])
        P.dma(thr15[:], T["thr15"], writes=["thr15"])
        P.dma(skf[:], T["skT_h"], writes=["skf"])
        CP(P, "dve", skT[:], skf[:], ["skf"], ["skT"])
        for c in range(8):
            b = 0
            P.dma(wtmp[b][:], T["wq_h"][:, :, c * 256:(c + 1) * 256], writes=[("wtmp", b)])
            CP(P, "pool" if c % 2 else "dve", Wq[:, :, c * 256:(c + 1) * 256], wtmp[b][:], [("wtmp", b)], ["Wq"])

        def load(tb):
            P.dma(h2b[tb % 2][:], T["h2T_s"][:, tb * 512:(tb + 1) * 512].rearrange("(k p) t -> p k t", p=128),
                  writes=[("h2b", tb % 2)])

        def vop(fn, reads, writes):
            P.op("dve", fn, reads, writes)

        load(0)

        def stageS(n):
            tb, q4 = divmod(n, 4)
            s = tb % 2
            if q4 == 0:
                if tb + 1 < nblocks:
                    load(tb + 1)
                for c in range(16):
                    pi = c % 2
                    for k in range(8):
                        MM(P, pq[pi][:], Wq[:, k, c * 128:(c + 1) * 128], h2b[s][:, k, :], k == 0, k == 7,
                           ["Wq", ("h2b", s)], [("pq", pi)])
                    CP(P, "act" if c % 2 else "dve", qT[:, c, :], pq[pi][:], [("pq", pi)], [("qT", c)])
            for _ in range(64 // (4 * nblocks) + 1):
                e0_step()
            for c in range(16):
                MM(P, scp[c // 4][:, (c % 4) * 128:(c % 4 + 1) * 128], qT[:, c, q4 * 128:(q4 + 1) * 128],
                   skT[:, c, :], True, True, [("qT", c), "skT"], [("scp", c // 4)])
            par = n % 2
            for g4 in range(4):
                CP(P, "act", scs_[par][:, g4 * 4:(g4 + 1) * 4, :].rearrange("p c k -> p (c k)"), scp[g4][:],
                   [("scp", g4)], [("scs", par, g4)])

        def stageD(n):
            tb, q4 = divmod(n, 4)
            s = tb % 2
            for _one in (0,):
                par = n % 2
                scs = scs_[par]
                for c in range(16):
                    kr = ("scs", par, c // 4)
                    vop(lambda e, c=c, scs=scs: e.max(out=v[:, c, 0:8], in_=scs[:, c, :]), [kr], [("v", c)])
                for c in range(16):
                    kr = ("scs", par, c // 4)
                    vop(lambda e, c=c, scs=scs: e.max_index(out=iu[:, c, 0:8], in_max=v[:, c, 0:8], in_values=scs[:, c, :]),
                        [kr, ("v", c)], [("iu", c)])
                for c in range(16):
                    kr = ("scs", par, c // 4)
                    vop(lambda e, c=c, scs=scs: e.match_replace(out=scs2[:, c, :], in_to_replace=v[:, c, 0:8],
                                                        in_values=scs[:, c, :], imm_value=-1e30),
                        [kr, ("v", c)], [("scs2", c)])
                for c in range(16):
                    vop(lambda e, c=c: e.max(out=v[:, c, 8:16], in_=scs2[:, c, :]), [("scs2", c)], [("v2", c)])
                for c in range(16):
                    vop(lambda e, c=c: e.max_index(out=iu[:, c, 8:16], in_max=v[:, c, 8:16], in_values=scs2[:, c, :]),
                        [("scs2", c), ("v2", c)], [("iu2", c)])
                allv = [("v", c) for c in range(16)] + [("v2", c) for c in range(16)]
                alli = [("iu", c) for c in range(16)] + [("iu2", c) for c in range(16)]
                CP(P, "dve", if_[:], iu[:], alli, ["if"])
                v4 = v[:].rearrange("p (n h) a -> p n h a", h=2)
                if4 = if_[:].rearrange("p (n h) a -> p n h a", h=2)
                TT(P, "dve", cand[:].rearrange("p n (a b) -> p n a b", b=16),
                   v4[:, :, 0, :].unsqueeze(3).to_broadcast([128, 8, 16, 16]),
                   v4[:, :, 1, :].unsqueeze(2).to_broadcast([128, 8, 16, 16]), ALU.add, allv, ["cand"])
                for n in range(8):
                    vop(lambda e, n=n: e.max(out=top[:, n, 0:8], in_=cand[:, n, :]), ["cand"], [("top", n)])
                for n in range(8):
                    vop(lambda e, n=n: e.max_index(out=pu[:, n, 0:8], in_max=top[:, n, 0:8], in_values=cand[:, n, :]),
                        ["cand", ("top", n)], [("pu", n)])
                for n in range(8):
                    vop(lambda e, n=n: e.match_replace(out=cand2[:, n, :], in_to_replace=top[:, n, 0:8],
                                                        in_values=cand[:, n, :], imm_value=-1e30),
                        ["cand", ("top", n)], [("cand2", n)])
                for n in range(8):
                    vop(lambda e, n=n: e.max(out=top[:, n, 8:16], in_=cand2[:, n, :]), [("cand2", n)], [("top2", n)])
                for n in range(8):
                    vop(lambda e, n=n: e.max_index(out=pu[:, n, 8:16], in_max=top[:, n, 8:16], in_values=cand2[:, n, :]),
                        [("cand2", n), ("top2", n)], [("pu2", n)])
                allt = [("top", n) for n in range(8)] + [("top2", n) for n in range(8)]
                allp = [("pu", n) for n in range(8)] + [("pu2", n) for n in range(8)]
                CP(P, "dve", pf[:], pu[:], allp, ["pf"])
                TT(P, "dve", ge[:, :, :, 0:15], pf[:].unsqueeze(3).to_broadcast([128, 8, 16, 15]),
                   thr15[:].unsqueeze(1).unsqueeze(1).to_broadcast([128, 8, 16, 15]), ALU.is_ge, ["pf", "thr15"], ["ge"])
                RSUM(P, "dve", ai[:], ge[:, :, :, 0:15], ["ge"], ["ai"])
                STT(P, "dve", bi[:], ai[:], -16.0, pf[:], ALU.mult, ALU.add, ["ai", "pf"], ["bi"])
                io4 = iota16[:].unsqueeze(1).unsqueeze(1).to_broadcast([128, 8, 16, 16])
                for x, (sel, half) in enumerate([(ai, 0), (bi, 1)]):
                    TT(P, "dve", ge[:], io4, sel[:].unsqueeze(3).to_broadcast([128, 8, 16, 16]), ALU.is_equal,
                       ["ai", "bi", "iota16"], ["ge"])
                    TT(P, "dve", ge[:], ge[:], if4[:, :, half, :].unsqueeze(2).to_broadcast([128, 8, 16, 16]),
                       ALU.mult, ["ge", "if"], ["ge"])
                    RSUM(P, "dve", rt[:, 1 + x, :].rearrange("p (n r) -> p n r", r=16), ge[:], ["ge"], [("rt", 1 + x)])
                TT(P, "dve", dd[:], top[:], top[:, :, 0:1].to_broadcast([128, 8, 16]), ALU.subtract, allt, ["dd"])
                ACT(P, dd[:], dd[:], AF.Exp, ["dd"], ["dd"])
                RSUM(P, "dve", zz[:, 0:8], dd[:], ["dd"], ["zz"])
                P.op("dve", lambda e: e.reciprocal(out=zz[:, 8:16], in_=zz[:, 0:8]), ["zz"], ["zz"])
                TT(P, "dve", rt[:, 0, :].rearrange("p (n r) -> p n r", r=16), dd[:],
                   zz[:, 8:16].unsqueeze(2).to_broadcast([128, 8, 16]), ALU.mult, ["dd", "zz"], [("rt", 0)])
                for x in range(3):
                    TR(P, ptr[:, x, :], rt[:, x, :], identF[:], [("rt", x), "identF"], ["ptr"])
                CP(P, "act", rstage[s][:, :, q4 * 128:(q4 + 1) * 128], ptr[:], ["ptr"], [("rstage", s)])
            if q4 == 3:
                P.dma(T["route_s"][:, :, tb * 512:(tb + 1) * 512].rearrange("x p t -> p x t"), rstage[s][:],
                      reads=[("rstage", s)], writes=[("route", tb)])

        ntl = nblocks * 4
        stageS(0)
        for n in range(ntl):
            if n + 1 < ntl:
                stageS(n + 1)
            stageD(n)
        while e0_step():
            pass
        P.emit()


def phase_e2(nc, T, ngroups=16):
    G = 256
    JH = 64
    with ExitStack() as es:
        P = Prog(nc, es, "E2")
        A = Alloc(nc, es)
        iota = A.sb("e2_iota", [128, 128], F32)
        gfin = A.sb("e2_gfin", [128, D], F32)
        Gsq = [A.sb(f"e2_Gs{i}", [128, G, JH], BF16) for i in range(2)]
        SBK = 16
        ohc = {"n": 0}
        OHI = [A.sb(f"e2_OHI{i}", [128, SBK, 128], BF16) for i in range(2)]
        OHJ = [A.sb(f"e2_OHJ{i}", [128, SBK, JH], BF16) for i in range(2)]
        OHJg = [A.sb(f"e2_OHJg{i}", [128, SBK, JH], BF16) for i in range(2)]
        CJ, NBUF = 4, 4
        NCH = 128 // CJ
        UTc = [A.sb(f"e2_UTc{i}", [128, 8, CJ, 128], BF16) for i in range(NBUF)]
        Vc = [A.sb(f"e2_Vc{i}", [128, CJ, D], BF16) for i in range(NBUF)]
        h2g = [A.sb(f"e2_h2g{i}", [128, 8, G], BF16) for i in range(2)]
        rtg = [A.sb(f"e2_rtg{i}", [128, 3, G], F32) for i in range(2)]
        x1g = A.sb("e2_x1g", [128, 2, D], F32)
        ga = [A.sb(f"e2_ga{i}", [128, G], BF16) for i in range(3)]
        Wj = [A.sb(f"e2_Wj{i}", [128, G], BF16) for i in range(3)]
        x2 = A.sb("e2_x2", [128, D], F32)
        sq = A.sb("e2_sq", [128, D], F32)
        rs = A.sb("e2_rs", [128, 4], F32)
        ot = [A.sb(f"e2_ot{i}", [128, D], F32) for i in range(2)]
        oacc = [[A.ps(f"e2_oacc{a}{b}", [128, 512], F32) for b in range(2)] for a in range(2)]
        pbank = [A.ps(f"e2_pb{i}", [128, 512], F32) for i in range(3)]
        gps = A.ps("e2_gps", [128, 512], F32)

        P.dma(iota[:], T["iota128"], writes=["iota"])
        iotab = A.sb("e2_iotab", [128, 128], BF16)
        CP(P, "dve", iotab[:], iota[:], ["iota"], ["iotab"])
        P.dma(gfin[:], T["g_final"].partition_broadcast(128), writes=["gfin"])

        def load_group(g):
            s = g % 2
            P.dma(h2g[s][:], T["h2T_s"][:, g * G:(g + 1) * G].rearrange("(k p) t -> p k t", p=128), writes=[("h2g", s)])
            P.dma(rtg[s][:], T["route_s"][:, :, g * G:(g + 1) * G].rearrange("x p t -> p x t"), writes=[("rtg", s)])

        def load_chunk(gc):
            b = gc % NBUF
            j0 = (gc % NCH) * CJ
            P.dma(UTc[b][:], T["UTb_s"][:, :, j0:j0 + CJ, :], writes=[("UTc", b)])
            P.dma(Vc[b][:], T["Vb_s"][:, j0:j0 + CJ, :], writes=[("Vc", b)])

        def oh_build(q, sb_):
            g_, p_ = divmod(q, 2)
            s_ = g_ % 2
            t0 = sb_ * SBK
            j0 = p_ * JH
            ob = sb_ % 2
            for t in reversed(range(SBK)):
                TS(P, "dve", OHI[ob][:, t, :], iotab[:], rtg[s_][:, 1, t0 + t:t0 + t + 1], ALU.is_equal,
                   ["iotab", ("rtg", s_)], [("OHI", ob, t)])
                TS(P, "dve", OHJg[ob][:, t, :], iotab[:, j0:j0 + JH], rtg[s_][:, 2, t0 + t:t0 + t + 1], ALU.is_equal,
                   ["iotab", ("rtg", s_)], [("OHJg", ob, t)], s2=rtg[s_][:, 0, t0 + t:t0 + t + 1], op1=ALU.mult)

        def oh_mm(q, sb_):
            t0 = sb_ * SBK
            ob = sb_ % 2
            for t in range(SBK):
                MM(P, gps[:, (t % 8) * JH:(t % 8 + 1) * JH], OHI[ob][:, t, :], OHJg[ob][:, t, :], True, True,
                   [("OHI", ob, t), ("OHJg", ob, t)], ["gps"])
                if t % 8 == 7:
                    CP(P, "act", Gsq[q % 2][:, t0 + t - 7:t0 + t + 1, :].rearrange("p t j -> p (t j)"), gps[:],
                       ["gps"], [("Gs", q % 2)])

        def oh_subblock(q, sb_):
            oh_build(q, sb_)
            oh_mm(q, sb_)

        load_group(0)
        for gc in range(NBUF):
            load_chunk(gc)
        for sb_ in range(G // SBK):
            oh_subblock(0, sb_)
        NP = 2 * ngroups
        fin = 0
        for g in range(ngroups):
            s = g % 2
            if g + 1 < ngroups:
                load_group(g + 1)
            P.dma(x1g[:], T["x1_s"][g * G:(g + 1) * G, :].rearrange("(a p) c -> p a c", p=128), writes=["x1g"])

            def stage1(j):
                gc = g * NCH + j // CJ
                b = gc % NBUF
                jj = j % CJ
                r = j % 3
                q = 2 * g + j // JH
                for k in range(8):
                    MM(P, pbank[r][:, 0:G], UTc[b][:, k, jj, :], h2g[s][:, k, :], k == 0, k == 7,
                       [("UTc", b), ("h2g", s)], [("pbank", r)])
                ACT(P, ga[r][:], pbank[r][:, 0:G], AF.Gelu, [("pbank", r)], [("ga", r)])
                TT(P, "pool", Wj[r][:], ga[r][:], Gsq[q % 2][:, :, j % JH], ALU.mult, [("ga", r), ("Gs", q % 2)], [("Wj", r)])

            def stage2(j):
                b = (g * NCH + j // CJ) % NBUF
                jj = j % CJ
                r = j % 3
                for tl in range(2):
                    for half in range(2):
                        MM(P, oacc[tl][half][:], Wj[r][:, tl * 128:(tl + 1) * 128],
                           Vc[b][:, jj, half * 512:(half + 1) * 512], j == 0, j == 127,
                           [("Wj", r), ("Vc", b)], [("oacc", tl, half)])
                gc = g * NCH + j // CJ
                if j % CJ == CJ - 1 and gc + NBUF < ngroups * NCH:
                    load_chunk(gc + NBUF)

            LOOK = 2
            for x in range(128 + LOOK):
                if x < 128:
                    stage1(x)
                    qn = 2 * g + x // JH + 1
                    xl = x % JH
                    if qn < NP:
                        if xl % 4 == 0:
                            oh_build(qn, xl // 4)
                        if (xl % 4 == 1 and xl >= 5):
                            oh_mm(qn, (xl - 5) // 4)
                        if xl == JH - 1:
                            oh_mm(qn, 15)
                if x >= LOOK:
                    stage2(x - LOOK)
            for tl in range(2):
                f = fin % 2
                fin += 1
                for half in range(2):
                    TT(P, "dve", x2[:, half * 512:(half + 1) * 512], oacc[tl][half][:],
                       x1g[:, tl, half * 512:(half + 1) * 512], ALU.add, [("oacc", tl, half), "x1g"], ["x2"])
                TT(P, "pool", sq[:], x2[:], x2[:], ALU.mult, ["x2"], ["sq"])
                RSUM(P, "dve", rs[:, 0:1], sq[:], ["sq"], ["rs"])
                ACT(P, rs[:, 1:2], rs[:, 0:1], AF.Ln, ["rs"], ["rs"], bias=EPS, scale=1.0 / D)
                ACT(P, rs[:, 2:3], rs[:, 1:2], AF.Exp, ["rs"], ["rs"], scale=-0.5)
                STT(P, "dve", ot[f][:], x2[:], rs[:, 2:3], gfin[:], ALU.mult, ALU.mult, ["x2", "rs", "gfin"], [("ot", f)])
                r0 = g * G + tl * 128
                P.dma(T["out"][r0:r0 + 128, :], ot[f][:], reads=[("ot", f)], writes=[("out", r0)])
        P.emit()
```
